# Optimizing a Trainium2 kernel written in Bass

```python
import math
import jax, jax.numpy as jnp
from jax import lax
import numpy as np

D_MODEL = 1024
BATCH = 16
SEQ = 4096
DEPTH = 4

N_MIXERS = 3
N_MLSTM_LAYERS = (DEPTH + 2) // 3
N_SSM_LAYERS = (DEPTH + 1) // 3
N_RWKV_LAYERS = DEPTH // 3
DEEPNORM_ALPHA = (2 * DEPTH) ** 0.25
DEEPNORM_BETA = (8 * DEPTH) ** -0.25
LN_EPS = 1e-5
RMS_EPS = 1e-6

ML_HEADS = 4
ML_DV = D_MODEL // ML_HEADS
ML_DQK = ML_DV // 2
ML_CHUNK = 64
ML_GATE_CAP = 15.0
ML_IN = 2 * ML_HEADS * ML_DQK + 2 * ML_HEADS * ML_DV + 2 * ML_HEADS

SSM_DINNER = 2 * D_MODEL
SSM_HEADDIM = 64
SSM_HEADS = SSM_DINNER // SSM_HEADDIM
SSM_STATE = 128
SSM_GROUPS = 4
SSM_CONV = 4
SSM_CHUNK = 128
SSM_CONV_CH = SSM_DINNER + 2 * SSM_GROUPS * SSM_STATE
SSM_IN = SSM_DINNER + SSM_CONV_CH + SSM_HEADS

RW_HEADDIM = 64
RW_HEADS = D_MODEL // RW_HEADDIM
RW_DECAY_LORA = 64
RW_AAA_LORA = 64
RW_GATE_LORA = 128
RW_LNX_EPS = 64e-5
RW_N_MIX = 6

FFN_HIDDEN = int(math.ceil(8 * D_MODEL / 3 / 256)) * 256

kernel_name = 'hybrid_mlstm_mamba2_rwkv7_deepnorm'


def layer_norm(x, g, b):
    xf = x.astype(jnp.float32)
    mu = jnp.mean(xf, axis=-1, keepdims=True)
    var = jnp.mean(jnp.square(xf - mu), axis=-1, keepdims=True)
    return ((xf - mu) * lax.rsqrt(var + LN_EPS) * g + b).astype(x.dtype)


def soft_cap(t):
    return ML_GATE_CAP * jnp.tanh(t / ML_GATE_CAP)


def mlstm_mixer(x, w_in, b_gate, norm_w, w_out):
    Bsz, S, _ = x.shape
    H, DK, DV, L = ML_HEADS, ML_DQK, ML_DV, ML_CHUNK
    NC = S // L
    proj = (x @ w_in).astype(jnp.float32)
    q, k, v, o, gates = jnp.split(
        proj, [H * DK, 2 * H * DK, 2 * H * DK + H * DV, 2 * H * DK + 2 * H * DV], axis=-1)
    gates = soft_cap(gates + b_gate.astype(jnp.float32))
    i_pre = gates[..., :H]
    log_f = jax.nn.log_sigmoid(gates[..., H:])

    def chunks(t, d):
        return t.reshape(Bsz, NC, L, H, d).transpose(1, 0, 3, 2, 4)

    def gchunks(t):
        return t.reshape(Bsz, NC, L, H).transpose(1, 0, 3, 2)

    causal = jnp.tril(jnp.ones((L, L), dtype=bool))

    def step(carry, inp):
        C, n, m = carry
        q_, k_, v_, i_, lf = inp
        b = jnp.cumsum(lf, axis=-1)
        dmat = jnp.where(causal, b[..., :, None] - b[..., None, :] + i_[..., None, :], -jnp.inf)
        inter = b + m[..., None]
        m_t = jnp.maximum(inter, jnp.max(dmat, axis=-1))
        wts = jnp.exp(dmat - m_t[..., None])
        sc = jnp.exp(inter - m_t)
        qk = jnp.einsum('bhtd,bhsd->bhts', q_, k_) * wts
        num = jnp.einsum('bhts,bhsv->bhtv', qk, v_) + sc[..., None] * jnp.einsum('bhtd,bhdv->bhtv', q_, C)
        den = jnp.sum(qk, axis=-1) + sc * jnp.einsum('bhtd,bhd->bht', q_, n)
        h = num / jnp.maximum(jnp.abs(den), jnp.exp(-m_t))[..., None]
        b_last = b[..., -1]
        g_s = b_last[..., None] - b + i_
        m_new = jnp.maximum(b_last + m, jnp.max(g_s, axis=-1))
        ws = jnp.exp(g_s - m_new[..., None])
        dec = jnp.exp(b_last + m - m_new)
        C_new = dec[..., None, None] * C + jnp.einsum('bhs,bhsd,bhsv->bhdv', ws, k_, v_)
        n_new = dec[..., None] * n + jnp.einsum('bhs,bhsd->bhd', ws, k_)
        return (C_new, n_new, m_new), h

    init = (jnp.zeros((Bsz, H, DK, DV), jnp.float32),
            jnp.zeros((Bsz, H, DK), jnp.float32),
            jnp.zeros((Bsz, H), jnp.float32))
    xs = (chunks(q, DK) * (DK ** -0.5), chunks(k, DK), chunks(v, DV), gchunks(i_pre), gchunks(log_f))
    _, h = lax.scan(step, init, xs)
    h = h.transpose(1, 0, 3, 2, 4).reshape(Bsz, S, H, DV)
    h = h * lax.rsqrt(jnp.mean(h * h, axis=-1, keepdims=True) + RMS_EPS)
    h = h.reshape(Bsz, S, H * DV) * norm_w * jax.nn.sigmoid(o)
    return h.astype(x.dtype) @ w_out


def causal_depthwise_conv(u, w, b):
    K, C = w.shape
    out = lax.conv_general_dilated(
        u, w[:, None, :].astype(u.dtype), window_strides=(1,), padding=[(K - 1, 0)],
        dimension_numbers=('NWC', 'WIO', 'NWC'), feature_group_count=C)
    return out + b


def ssd_chunk_scan(xs, dt, A, Bm, Cm):
    Bsz, S, _ = xs.shape
    H, P, G, N, L = SSM_HEADS, SSM_HEADDIM, SSM_GROUPS, SSM_STATE, SSM_CHUNK
    HG = H // G
    NC = S // L
    xc = xs.reshape(Bsz, NC, L, G, HG, P).transpose(1, 0, 3, 4, 2, 5)
    dtc = dt.reshape(Bsz, NC, L, G, HG).transpose(1, 0, 3, 4, 2)
    Bc = Bm.reshape(Bsz, NC, L, G, N).transpose(1, 0, 3, 2, 4)
    Cc = Cm.reshape(Bsz, NC, L, G, N).transpose(1, 0, 3, 2, 4)
    A_g = A.reshape(G, HG)[None, :, :, None]
    causal = jnp.tril(jnp.ones((L, L), dtype=bool))

    def step(state, inp):
        x_, dt_, B_, C_ = inp
        cum = jnp.cumsum(dt_ * A_g, axis=-1)
        seg = jnp.where(causal, cum[..., :, None] - cum[..., None, :], -jnp.inf)
        cb = jnp.einsum('bgtn,bgsn->bgts', C_, B_)
        mmat = cb[:, :, None] * jnp.exp(seg) * dt_[..., None, :]
        y = jnp.einsum('bghts,bghsp->bghtp', mmat, x_)
        y = y + jnp.einsum('bgtn,bghpn->bghtp', C_, state) * jnp.exp(cum)[..., None]
        w_s = jnp.exp(cum[..., -1:] - cum) * dt_
        state = (state * jnp.exp(cum[..., -1])[..., None, None]
                 + jnp.einsum('bghs,bgsn,bghsp->bghpn', w_s, B_, x_))
        return state, y

    state0 = jnp.zeros((Bsz, G, HG, P, N), jnp.float32)
    _, y = lax.scan(step, state0, (xc, dtc, Bc, Cc))
    return y.transpose(1, 0, 4, 2, 3, 5).reshape(Bsz, S, H * P)


def mamba2_mixer(x, w_in, conv_w, conv_b, dt_bias, a_log, d_skip, norm_w, w_out):
    Bsz, S, _ = x.shape
    DI, H, P, G = SSM_DINNER, SSM_HEADS, SSM_HEADDIM, SSM_GROUPS
    f32 = jnp.float32
    proj = x @ w_in
    z, xbc, dt = jnp.split(proj, [DI, DI + SSM_CONV_CH], axis=-1)
    xbc = jax.nn.silu(causal_depthwise_conv(xbc, conv_w, conv_b)).astype(f32)
    xs, Bm, Cm = jnp.split(xbc, [DI, DI + G * SSM_STATE], axis=-1)
    dt = jax.nn.softplus(dt.astype(f32) + dt_bias.astype(f32))
    A = -jnp.exp(a_log.astype(f32))
    y = ssd_chunk_scan(xs, dt, A, Bm, Cm)
    y = y + (xs.reshape(Bsz, S, H, P) * d_skip[:, None]).reshape(Bsz, S, DI)
    y = (y * jax.nn.silu(z.astype(f32))).reshape(Bsz, S, G, DI // G)
    y = y * lax.rsqrt(jnp.mean(y * y, axis=-1, keepdims=True) + RMS_EPS)
    y = y.reshape(Bsz, S, DI) * norm_w
    return y.astype(x.dtype) @ w_out


def rwkv7_scan(r, w, k, v, a, b):
    Bsz, _, H, K = r.shape

    def step(st, inp):
        r_, w_, k_, v_, a_, b_ = inp
        sa = jnp.einsum('bhvk,bhk->bhv', st, a_)
        st = st * w_[:, :, None, :] + sa[..., None] * b_[:, :, None, :] + v_[..., None] * k_[:, :, None, :]
        return st, jnp.einsum('bhvk,bhk->bhv', st, r_)

    tm = lambda t: jnp.moveaxis(t, 1, 0)
    st0 = jnp.zeros((Bsz, H, K, K), jnp.float32)
    _, y = lax.scan(step, st0, (tm(r), tm(w), tm(k), tm(v), tm(a), tm(b)))
    return jnp.moveaxis(y, 0, 1)


def rwkv7_mixer(x, mix, w_rkv, w0, w1, w2, a0, a1, a2, g1, g2, k_k, k_a, r_k, lnx_w, lnx_b, w_out):
    Bsz, S, D = x.shape
    H, K = RW_HEADS, RW_HEADDIM
    f32 = jnp.float32
    xx = jnp.pad(x, ((0, 0), (1, 0), (0, 0)))[:, :-1] - x
    xr, xw, xk, xv, xa, xg = (x + xx * mix[j] for j in range(RW_N_MIX))
    rkv = jnp.einsum('cbsd,cde->cbse', jnp.stack([xr, xk, xv]), w_rkv).astype(f32)
    r, k, v = rkv[0], rkv[1], rkv[2]
    w = -jax.nn.softplus(-(w0 + jnp.tanh(xw @ w1) @ w2).astype(f32)) - 0.5
    a = jax.nn.sigmoid((a0 + (xa @ a1) @ a2).astype(f32))
    g = (jax.nn.sigmoid(xg @ g1) @ g2).astype(f32)
    heads = lambda t: t.reshape(Bsz, S, H, K)
    kk = heads(k * k_k)
    kk = kk * lax.rsqrt(jnp.maximum(jnp.sum(kk * kk, axis=-1, keepdims=True), 1e-24))
    k = heads(k * (1.0 + (a - 1.0) * k_a))
    r_h, v_h = heads(r), heads(v)
    y = rwkv7_scan(r_h, heads(jnp.exp(-jnp.exp(w))), k, v_h, -kk, kk * heads(a))
    mu = jnp.mean(y, axis=-1, keepdims=True)
    var = jnp.mean(jnp.square(y - mu), axis=-1, keepdims=True)
    y = ((y - mu) * lax.rsqrt(var + RW_LNX_EPS)).reshape(Bsz, S, D) * lnx_w + lnx_b
    y = y + (jnp.sum(r_h * k * r_k, axis=-1, keepdims=True) * v_h).reshape(Bsz, S, D)
    return (y * g).astype(x.dtype) @ w_out


def swiglu(x, w_in, w_out):
    gate, up = jnp.split(x @ w_in, 2, axis=-1)
    return (jax.nn.silu(gate) * up) @ w_out


def setup_inputs(seed: int = 0) -> dict:
    key = jax.random.key(seed)
    ks = jax.random.split(key, 33)
    f32 = jnp.float32
    D, F = D_MODEL, FFN_HIDDEN
    nm, ns, nr = N_MLSTM_LAYERS, N_SSM_LAYERS, N_RWKV_LAYERS
    nrm = lambda k, shape, scale: jax.random.normal(k, shape, f32) * scale
    H = ML_HEADS
    gn = jax.random.normal(ks[6], (nm, 2 * H), f32)
    ml_b_gate = jnp.concatenate([0.1 * gn[:, :H], 3.0 + 0.5 * gn[:, H:]], axis=-1)
    dt0 = jnp.exp(jax.random.uniform(ks[12], (ns, SSM_HEADS), f32, math.log(1e-3), math.log(1e-1)))
    return {
        'x': nrm(ks[0], (BATCH, SEQ, D), 1.0),
        'ln_g': 1.0 + nrm(ks[1], (DEPTH, 2, D), 0.02),
        'ln_b': nrm(ks[2], (DEPTH, 2, D), 0.02),
        'ffn_w_in': nrm(ks[3], (DEPTH, D, 2 * F), D ** -0.5),
        'ffn_w_out': nrm(ks[4], (DEPTH, F, D), DEEPNORM_BETA * F ** -0.5),
        'ml_w_in': nrm(ks[5], (nm, D, ML_IN), D ** -0.5),
        'ml_b_gate': ml_b_gate,
        'ml_norm_w': 1.0 + nrm(ks[7], (nm, D), 0.02),
        'ml_w_out': nrm(ks[8], (nm, D, D), DEEPNORM_BETA * D ** -0.5),
        'ssm_w_in': nrm(ks[9], (ns, D, SSM_IN), D ** -0.5),
        'ssm_conv_w': nrm(ks[10], (ns, SSM_CONV, SSM_CONV_CH), SSM_CONV ** -0.5),
        'ssm_conv_b': nrm(ks[11], (ns, SSM_CONV_CH), 0.02),
        'ssm_dt_bias': dt0 + jnp.log(-jnp.expm1(-dt0)),
        'ssm_a_log': jnp.log(jax.random.uniform(ks[13], (ns, SSM_HEADS), f32, 1.0, 16.0)),
        'ssm_d': 1.0 + nrm(ks[14], (ns, SSM_HEADS), 0.1),
        'ssm_norm_w': 1.0 + nrm(ks[15], (ns, SSM_DINNER), 0.02),
        'ssm_w_out': nrm(ks[16], (ns, SSM_DINNER, D), DEEPNORM_BETA * SSM_DINNER ** -0.5),
        'rw_mix': jax.random.uniform(ks[17], (nr, RW_N_MIX, D), f32),
        'rw_w_rkv': nrm(ks[18], (nr, 3, D, D), D ** -0.5),
        'rw_w0': jax.random.uniform(ks[19], (nr, D), f32, -6.0, -1.0),
        'rw_w1': nrm(ks[20], (nr, D, RW_DECAY_LORA), D ** -0.5),
        'rw_w2': nrm(ks[21], (nr, RW_DECAY_LORA, D), 0.5 * RW_DECAY_LORA ** -0.5),
        'rw_a0': nrm(ks[22], (nr, D), 0.1),
        'rw_a1': nrm(ks[23], (nr, D, RW_AAA_LORA), D ** -0.5),
        'rw_a2': nrm(ks[24], (nr, RW_AAA_LORA, D), 0.5 * RW_AAA_LORA ** -0.5),
        'rw_g1': nrm(ks[25], (nr, D, RW_GATE_LORA), D ** -0.5),
        'rw_g2': nrm(ks[26], (nr, RW_GATE_LORA, D), RW_GATE_LORA ** -0.5),
        'rw_k_k': 0.85 + nrm(ks[27], (nr, D), 0.02),
        'rw_k_a': 1.0 + nrm(ks[28], (nr, D), 0.02),
        'rw_r_k': nrm(ks[29], (nr, RW_HEADS, RW_HEADDIM), 0.1),
        'rw_lnx_w': 1.0 + nrm(ks[30], (nr, D), 0.02),
        'rw_lnx_b': nrm(ks[31], (nr, D), 0.02),
        'rw_w_out': nrm(ks[32], (nr, D, D), DEEPNORM_BETA * D ** -0.5),
    }


def reference(x, ln_g, ln_b, ffn_w_in, ffn_w_out,
              ml_w_in, ml_b_gate, ml_norm_w, ml_w_out,
              ssm_w_in, ssm_conv_w, ssm_conv_b, ssm_dt_bias, ssm_a_log, ssm_d, ssm_norm_w, ssm_w_out,
              rw_mix, rw_w_rkv, rw_w0, rw_w1, rw_w2, rw_a0, rw_a1, rw_a2, rw_g1, rw_g2,
              rw_k_k, rw_k_a, rw_r_k, rw_lnx_w, rw_lnx_b, rw_w_out):
    h = x
    for i in range(DEPTH):
        kind, j = i % N_MIXERS, i // N_MIXERS
        if kind == 0:
            y = mlstm_mixer(h, ml_w_in[j], ml_b_gate[j], ml_norm_w[j], ml_w_out[j])
        elif kind == 1:
            y = mamba2_mixer(h, ssm_w_in[j], ssm_conv_w[j], ssm_conv_b[j], ssm_dt_bias[j],
                             ssm_a_log[j], ssm_d[j], ssm_norm_w[j], ssm_w_out[j])
        else:
            y = rwkv7_mixer(h, rw_mix[j], rw_w_rkv[j], rw_w0[j], rw_w1[j], rw_w2[j], rw_a0[j],
                            rw_a1[j], rw_a2[j], rw_g1[j], rw_g2[j], rw_k_k[j], rw_k_a[j],
                            rw_r_k[j], rw_lnx_w[j], rw_lnx_b[j], rw_w_out[j])
        h = layer_norm(DEEPNORM_ALPHA * h + y, ln_g[i, 0], ln_b[i, 0])
        h = layer_norm(DEEPNORM_ALPHA * h + swiglu(h, ffn_w_in[i], ffn_w_out[i]), ln_g[i, 1], ln_b[i, 1])
    return h
```

```python
import contextlib
import numpy as np
import concourse.bass as bass
import concourse.mybir as mybir
from concourse.bass_utils import run_bass_kernel_spmd

F32 = mybir.dt.float32
BF16 = mybir.dt.bfloat16
AF = mybir.ActivationFunctionType
ALU = mybir.AluOpType
AX = mybir.AxisListType

D = 1024
DEPTH = 4
FH = 2816
ALPHA = (2 * DEPTH) ** 0.25
LN_EPS = 1e-5
RMS_EPS = 1e-6
T = 512
KC = D // 128
EPOCH = 30000
WSC = float(np.exp(-0.5))
import os as _os
NOSAME = bool(int(_os.environ.get('NOSAME', '0')))


class Dom:
    def __init__(self, name, sems, unit, epoch):
        self.name, self.sems, self.unit, self.epoch = name, sems, unit, epoch
        self.count = 0

    def target(self, n):
        idx = (n - 1) // self.epoch
        return self.sems[idx], ((n - 1) % self.epoch + 1) * self.unit


class Eng(Dom):
    def __init__(self, name, be, sems, is_pe=False):
        super().__init__(name, sems, 1, EPOCH)
        self.be = be
        self.seen = {}
        self.is_pe = is_pe


class Buf:
    __slots__ = ("w", "r", "name")

    def __init__(self, name):
        self.w = None
        self.r = {}
        self.name = name


class V:
    __slots__ = ("ap", "bufs")

    def __init__(self, ap, bufs):
        self.ap = ap
        self.bufs = bufs


def _flat(lists):
    d = {}
    for l in lists:
        for b in l:
            d[id(b)] = b
    return list(d.values())


class Tile:
    def __init__(self, name, ap, split=None, bsplit=None):
        self.name = name
        self.ap = ap
        self.split = split
        n = ap.shape[split] if split is not None else 1
        self.bsplit = bsplit if bsplit is not None else [[Buf(f"{name}.{i}")] for i in range(n)]
        self.bufs = _flat(self.bsplit)

    def __getitem__(self, key):
        if not isinstance(key, tuple):
            key = (key,)
        bufs = self.bufs
        if self.split is not None and len(key) > self.split:
            k = key[self.split]
            if isinstance(k, int):
                bufs = self.bsplit[k]
            elif isinstance(k, slice):
                bufs = _flat(self.bsplit[k])
        return V(self.ap[key], bufs)

    def v(self, ap, bufs=None):
        return V(ap, self.bufs if bufs is None else bufs)

    def all(self):
        return V(self.ap, self.bufs)


class EP:
    def __init__(self, ctx, eng):
        self.ctx, self.eng = ctx, eng

    def __getattr__(self, name):
        meth = getattr(self.eng.be, name)
        ctx, eng = self.ctx, self.eng

        def call(*args, **kw):
            reads, writes = [], []

            def conv(k, v):
                if isinstance(v, V):
                    (writes if k in ("out", "accum_out") else reads).extend(v.bufs)
                    return v.ap
                return v
            args2 = [conv("out" if i == 0 else "in", a) for i, a in enumerate(args)]
            kw2 = {k: conv(k, v) for k, v in kw.items()}
            return ctx.issue(eng, lambda: meth(*args2, **kw2), reads, writes, dma=(name == "dma_start"))
        return call


class Ctx:
    def __init__(self, nc, es):
        self.nc, self.es = nc, es
        nsem_eng = {"pe": 5, "dve": 4, "act": 4, "pool": 3, "sp": 2}
        bes = {"pe": nc.tensor, "dve": nc.vector, "act": nc.scalar, "pool": nc.gpsimd, "sp": nc.sync}
        self.engs = {}
        for k, n in nsem_eng.items():
            sems = [es.enter_context(nc.semaphore(f"s_{k}{i}")) for i in range(n)]
            self.engs[k] = Eng(k, bes[k], sems, is_pe=(k == "pe"))
        self.pe, self.dve, self.act, self.pool, self.sp = (EP(self, self.engs[k]) for k in ("pe", "dve", "act", "pool", "sp"))
        self.dma_doms = [Dom(f"dma{i}", [es.enter_context(nc.semaphore(f"s_dma{i}"))], 16, 4000) for i in range(40)]
        self.dma_rr = 0
        self.ninst = 0
        self.nwait = 0
        self._rr = 0

    def sbuf(self, name, shape, dtype, split=None):
        t = self.es.enter_context(self.nc.sbuf_tensor("sb_" + name, list(shape), dtype))
        return Tile(name, t[:] if hasattr(t, "__getitem__") else t.ap(), split)

    def make_arena(self, nbytes, gran=1024):
        self.ar_tile = self.sbuf("arena", [128, nbytes // 4], F32)
        self.ar_gran = gran
        self.ar_bufs = [Buf(f"ar{i}") for i in range(nbytes // gran)]
        self.ar_size = nbytes
        self.ar_off = 0
        self.ar_peak = 0

    def areset(self):
        self.ar_off = 0

    def carve(self, name, shape, dtype, split=None):
        esz = 4 if dtype == F32 else 2
        free = 1
        for d in shape[1:]:
            free *= d
        nbytes = free * esz
        off = (self.ar_off + 63) // 64 * 64
        assert off + nbytes <= self.ar_size, (name, off, nbytes, self.ar_size)
        self.ar_off = off + nbytes
        self.ar_peak = max(self.ar_peak, self.ar_off)
        ap = self.ar_tile.ap[:, off // 4:(off + nbytes) // 4]
        if dtype != F32:
            ap = ap.bitcast(dtype)
        if len(shape) == 3:
            ap = ap.rearrange("p (a b) -> p a b", a=shape[1])
        elif len(shape) == 4:
            ap = ap.rearrange("p (a b c) -> p a b c", a=shape[1], b=shape[2])
        g = self.ar_gran

        def regs(o0, o1):
            return self.ar_bufs[o0 // g:(o1 + g - 1) // g]
        if split is None:
            bs = [regs(off, off + nbytes)]
        else:
            assert split == 1
            per = nbytes // shape[1]
            bs = [regs(off + i * per, off + (i + 1) * per) for i in range(shape[1])]
        return Tile(name, ap, split, bs)

    def issue(self, eng, fn, reads, writes, dma=False):
        deps = {}

        def need(d, n):
            if deps.get(d, 0) < n:
                deps[d] = n
        for b in reads:
            if b.w:
                need(*b.w)
        for b in writes:
            if b.w:
                need(*b.w)
            for d, n in b.r.items():
                need(d, n)
        if dma:
            dom = self.dma_doms[self.dma_rr]
            self.dma_rr = (self.dma_rr + 1) % len(self.dma_doms)
            if dom.count:
                need(dom, dom.count)
        else:
            dom = eng
        for d, n in deps.items():
            if d is eng and (eng.is_pe or (NOSAME and eng.name in ("dve", "act"))):
                continue
            if eng.seen.get(d, 0) >= n:
                continue
            sem, val = d.target(n)
            eng.be.wait_ge(sem, val)
            eng.seen[d] = n
            self.nwait += 1
        inst = fn()
        dom.count += 1
        n = dom.count
        sem, _ = dom.target(n)
        inst.then_inc(sem, dom.unit)
        self.ninst += 1
        for b in reads:
            b.r[dom] = n
        for b in writes:
            b.w = (dom, n)
            b.r = {}
        return inst

    def any2(self):
        self._rr ^= 1
        return self.dve if self._rr else self.pool

    def finish(self, bufs):
        sp = self.engs["sp"]
        for b in bufs:
            if b.w:
                d, n = b.w
                if sp.seen.get(d, 0) < n:
                    sem, val = d.target(n)
                    sp.be.wait_ge(sem, val)
                    sp.seen[d] = n


class WPlan:
    def __init__(self):
        self.blocks = {}
        self.src = []
        self.tot = 0

    def add(self, name, key, idx, kdim, cols):
        kc = max(1, kdim // 128)
        nb = sum(c1 - c0 for c0, c1 in cols)
        self.blocks[name] = (self.tot, kc, nb)
        self.src.append((name, key, idx, kdim, cols))
        self.tot += kc * nb
        if self.tot % 2:
            self.tot += 1

    def add_custom(self, name, kc, nb, fn):
        self.blocks[name] = (self.tot, kc, nb)
        self.src.append((name, None, fn, None, None))
        self.tot += kc * nb
        if self.tot % 2:
            self.tot += 1

    def pack(self, inputs):
        out = np.zeros((128, self.tot), np.float32)
        for name, key, idx, kdim, cols in self.src:
            off, kc, nb = self.blocks[name]
            if key is None:
                out[:, off:off + kc * nb] = idx(inputs)
                continue
            w = inputs[key][idx]
            wc = np.concatenate([w[:, c0:c1] for c0, c1 in cols], axis=1)
            if kdim < 128:
                out[:kdim, off:off + nb] = wc
            else:
                out[:, off:off + kc * nb] = wc.reshape(kc, 128, nb).transpose(1, 0, 2).reshape(128, kc * nb)
        return out


def layer_kind(i):
    return i % 3


def make_plan(layers, mixers=True):
    wp = WPlan()
    for i in layers:
        kind, j = i % 3, i // 3
        if kind == 0 and mixers:
            for b in range(6):
                wp.add(f"L{i}.ml_in{b}", "ml_w_in", j, D, [(b * 512, (b + 1) * 512)])
            wp.add(f"L{i}.ml_g", "ml_w_in", j, D, [(3072, 3080)])
            for b in range(2):
                wp.add(f"L{i}.ml_out{b}", "ml_w_out", j, D, [(b * 512, (b + 1) * 512)])
        if kind == 1 and mixers:
            for b in range(4):
                wp.add(f"L{i}.ssm_z{b}", "ssm_w_in", j, D, [(b * 512, (b + 1) * 512)])
            for b in range(6):
                wp.add(f"L{i}.ssm_x{b}", "ssm_w_in", j, D, [(2048 + b * 512, 2048 + (b + 1) * 512)])
            wp.add(f"L{i}.ssm_dt", "ssm_w_in", j, D, [(5120, 5152)])

            def cbrow(inputs, j=j):
                a = np.zeros((128, 1024), np.float32)
                for r in range(3):
                    a[32 * r] = inputs["ssm_conv_b"][j][r * 1024:(r + 1) * 1024]
                return a
            wp.add_custom(f"L{i}.ssm_cb", 1, 1024, cbrow)
            for cc in range(24):
                def cw(inputs, j=j, cc=cc):
                    a = np.zeros((128, 4, 128), np.float32)
                    w = inputs["ssm_conv_w"][j]
                    for tap in range(4):
                        a[np.arange(128), tap, np.arange(128)] = w[tap, cc * 128:(cc + 1) * 128]
                    return a.reshape(128, 512)
                wp.add_custom(f"L{i}.ssm_cw{cc}", 1, 512, cw)
            for b in range(4):
                wp.add(f"L{i}.ssm_out{b}", "ssm_w_out", j, 2048, [(b * 256, (b + 1) * 256)])
        if kind == 2 and mixers:
            wp.add(f"L{i}.rw_w1", "rw_w1", j, D, [(0, 64)])
            wp.add(f"L{i}.rw_w2", "rw_w2", j, 64, [(0, 1024)])
            for b in range(2):
                wp.add(f"L{i}.rw_r{b}", "rw_w_rkv", (j, 0), D, [(b * 512, (b + 1) * 512)])
            wp.add(f"L{i}.rw_a1", "rw_a1", j, D, [(0, 64)])
            wp.add(f"L{i}.rw_a2", "rw_a2", j, 64, [(0, 1024)])
            for b in range(2):
                wp.add(f"L{i}.rw_k{b}", "rw_w_rkv", (j, 1), D, [(b * 512, (b + 1) * 512)])
            for b in range(2):
                wp.add(f"L{i}.rw_v{b}", "rw_w_rkv", (j, 2), D, [(b * 512, (b + 1) * 512)])
            wp.add(f"L{i}.rw_g1", "rw_g1", j, D, [(0, 128)])
            wp.add(f"L{i}.rw_g2", "rw_g2", j, 128, [(0, 1024)])
            for b in range(2):
                wp.add(f"L{i}.rw_o{b}", "rw_w_out", j, D, [(b * 512, (b + 1) * 512)])
        for b in range(11):
            wp.add(f"L{i}.f_in{b}", "ffn_w_in", i, D, [(b * 256, (b + 1) * 256), (FH + b * 256, FH + (b + 1) * 256)])
        for b in range(8):
            wp.add(f"L{i}.f_out{b}", "ffn_w_out", i, FH, [(b * 128, (b + 1) * 128)])
    return wp


def layer_block_seq(i, mixers=True, nsub=2):
    seq = []
    for _ in range(nsub):
        seq += mixer_block_seq(i, mixers)
    seq += [f"L{i}.f_in{b}" for b in range(11)] + [f"L{i}.f_out{b}" for b in range(8)]
    return seq


def mixer_block_seq(i, mixers=True):
    kind = i % 3
    seq = []
    if kind == 0 and mixers:
        seq += [f"L{i}.ml_g"] + [f"L{i}.ml_in{b}" for b in range(6)] + [f"L{i}.ml_out{b}" for b in range(2)]
    if kind == 1 and mixers:
        seq += [f"L{i}.ssm_dt"] + [f"L{i}.ssm_z{b}" for b in range(4)] + [f"L{i}.ssm_x{b}" for b in range(6)] + [f"L{i}.ssm_cb"]
        seq += [f"L{i}.ssm_cw{cc}" for cc in range(24)] + [f"L{i}.ssm_out{b}" for b in range(4)]
    if kind == 2 and mixers:
        seq += [f"L{i}.rw_w1", f"L{i}.rw_w2", f"L{i}.rw_r0", f"L{i}.rw_r1", f"L{i}.rw_a1", f"L{i}.rw_a2", f"L{i}.rw_k0", f"L{i}.rw_k1",
                f"L{i}.rw_v0", f"L{i}.rw_v1", f"L{i}.rw_g1", f"L{i}.rw_g2", f"L{i}.rw_o0", f"L{i}.rw_o1"]
    return seq


SLOT = 4096
NSLOT = 4
ARENA = 66 * 1024


class Prog:
    def __init__(self, nseq, seqlen, layers, mixers=True):
        self.nseq, self.seqlen, self.layers, self.mixers = nseq, seqlen, layers, mixers
        self.ntile = seqlen // T
        self.TM = 256
        self.wp = make_plan(layers, mixers)
        self.pv_names = {}
        self.npv = 0

    def pv_add(self, name, n):
        self.pv_names[name] = self.npv
        self.npv += n

    def build(self):
        nc = bass.Bass("TRN2", target_bir_lowering=False)
        self.nc = nc
        nseq, seqlen = self.nseq, self.seqlen
        for i in self.layers:
            for j in range(2):
                self.pv_add(f"ln_g{i}.{j}", KC)
                self.pv_add(f"ln_b{i}.{j}", KC)
        self.bc_names = {}
        self.nbc = 0
        for i in self.layers:
            if i % 3 == 0 and self.mixers:
                self.pv_add(f"ml_nw{i}", KC)
                self.bc_names[f"ml_bg{i}"] = self.nbc
                self.nbc += 8
            if i % 3 == 1 and self.mixers:
                self.pv_add(f"ssm_cb{i}", 24)
                self.pv_add(f"ssm_nw{i}", 16)
                self.bc_names[f"ssm{i}"] = self.nbc
                self.nbc += 96
            if i % 3 == 2 and self.mixers:
                for nm, n in (("mix", 48), ("w0", 8), ("a0", 8), ("k_k", 8), ("k_a", 8), ("r_k", 8)):
                    self.pv_add(f"rw{i}_{nm}", n)
                self.bc_names[f"rw{i}"] = self.nbc
                self.nbc += 2048
        self.nbc = max(self.nbc, 8)
        bc_d = nc.dram_tensor("bc", [128, self.nbc], F32, kind="ExternalInput").ap()
        xT_d = nc.dram_tensor("xT", [nseq, D, seqlen], F32, kind="ExternalInput").ap()
        wf_d = nc.dram_tensor("wf", [128, self.wp.tot], F32, kind="ExternalInput").ap()
        pv_d = nc.dram_tensor("pv", [128, self.npv], F32, kind="ExternalInput").ap()
        yT_d = nc.dram_tensor("yT", [nseq, D, seqlen], F32, kind="ExternalOutput").ap()
        wb_d = nc.dram_tensor("wb", [128, self.wp.tot], BF16, kind="Internal").ap()
        with contextlib.ExitStack() as es:
            c = Ctx(nc, es)
            self.c = c
            self.xT_t = Tile("xT", xT_d)
            self.yT_t = Tile("yT", yT_d)
            self.wf_t = Tile("wf", wf_d)
            self.pv_dt = Tile("pvd", pv_d)
            self.wb_t = Tile("wb", wb_d)
            self.bc_dt = Tile("bcd", bc_d)
            self.alloc()
            self.prepass()
            self.consts()
            self.useq = []
            for s in range(nseq):
                for ti in range(self.ntile):
                    for i in self.layers:
                        self.useq += layer_block_seq(i, self.mixers)
            self.upos = 0
            self.uissued = 0
            for s in range(nseq):
                self.reset_state()
                for ti in range(self.ntile):
                    self.tile(s, ti)
            assert self.upos == len(self.useq)
            c.finish(self.yT_t.bufs)
            print(f"[build] instructions={c.ninst} waits={c.nwait} arena_peak={c.ar_peak}")
        return nc

    def alloc(self):
        c = self.c
        ps = self.c.es.enter_context(self.nc.psum_tensor("psum_all", [128, 8, 512], F32))
        self.ps = Tile("ps", ps[:] if hasattr(ps, "__getitem__") else ps.ap(), split=1)
        self.bank_rr = 0
        self.x = c.sbuf("x", [128, KC, T], F32, split=1)
        self.xb = c.sbuf("xb", [128, KC, T], BF16, split=1)
        self.s = c.sbuf("s", [128, KC, T], F32, split=1)
        self.wslot = [c.sbuf(f"wslot{i}", [128, SLOT], BF16) for i in range(NSLOT)]
        self.pv = c.sbuf("pv", [128, self.npv], F32)
        self.ones_b = c.sbuf("ones_b", [128, 128], BF16)
        c.make_arena(ARENA)

    def bank(self):
        b = self.bank_rr
        self.bank_rr = (self.bank_rr + 1) % 8
        return b

    def prepass(self):
        c = self.c
        tot = self.wp.tot
        i = 0
        off = 0
        engs = [c.dve, c.act]
        c.areset()
        stg_f = [c.carve(f"stgf{q}", [128, SLOT], F32) for q in range(2)]
        stg_b = [c.carve(f"stgb{q}", [128, SLOT], BF16) for q in range(2)]
        while off < tot:
            sz = min(SLOT, tot - off)
            f, b = stg_f[i % 2], stg_b[i % 2]
            c.sp.dma_start(out=f[:, 0:sz], in_=self.wf_t[:, off:off + sz])
            e = engs[i % 2]
            if e is c.act:
                e.activation(out=b[:, 0:sz], in_=f[:, 0:sz], func=AF.Copy)
            else:
                e.tensor_copy(out=b[:, 0:sz], in_=f[:, 0:sz])
            c.sp.dma_start(out=self.wb_t[:, off:off + sz], in_=b[:, 0:sz])
            off += sz
            i += 1
        c.sp.dma_start(out=self.pv.all(), in_=self.pv_dt.all())
        c.sp.dma_start(out=self.bc.all(), in_=self.bc_dt.all())

    def consts(self):
        c = self.c
        c.dve.memset(self.ones_b.all(), 1.0 / D)

    def reset_state(self):
        pass

    def _issue_load(self, u):
        name = self.useq[u]
        off, kc, nb = self.wp.blocks[name]
        slot = self.wslot[u % NSLOT]
        self.c.sp.dma_start(out=slot[:, 0:kc * nb], in_=self.wb_t[:, off:off + kc * nb])

    def weight(self, name):
        assert self.useq[self.upos] == name, (self.useq[self.upos], name)
        while self.uissued < min(len(self.useq), self.upos + NSLOT):
            self._issue_load(self.uissued)
            self.uissued += 1
        off, kc, nb = self.wp.blocks[name]
        slot = self.wslot[self.upos % NSLOT]
        self.upos += 1
        ap = slot.ap[:, 0:kc * nb].rearrange("p (k n) -> p k n", k=kc)
        return V(ap, slot.bufs)

    def tile(self, s, ti):
        c = self.c
        t0 = ti * T
        src = self.xT_t.ap[s].rearrange("(k p) t -> p k t", p=128)[:, :, t0:t0 + T]
        c.sp.dma_start(out=self.x.all(), in_=self.xT_t.v(src))
        for kc in range(KC):
            if kc % 2:
                c.act.activation(out=self.xb[:, kc, :], in_=self.x[:, kc, :], func=AF.Copy)
            else:
                c.dve.tensor_copy(out=self.xb[:, kc, :], in_=self.x[:, kc, :])
        for i in self.layers:
            if self.mixers:
                self.mixer(i, s, ti)
                self.layernorm(i, 0)
            self.ffn(i)
            self.layernorm(i, 1)
        dst = self.yT_t.ap[s].rearrange("(k p) t -> p k t", p=128)[:, :, t0:t0 + T]
        c.sp.dma_start(out=self.yT_t.v(dst), in_=self.x.all())

    def pvv(self, name, k):
        col = self.pv_names[name] + k
        return self.pv[:, col:col + 1]

    def layernorm(self, i, j):
        cx = self.c
        cx.areset()
        self.sqb = cx.carve("sqb", [128, KC, T], BF16, split=1)
        self.sb = cx.carve("sb", [128, KC, T], BF16, split=1)
        self.stat = cx.carve("stat", [128, 4, T], F32, split=1)
        for kc in range(KC):
            cx.act.activation(out=self.sqb[:, kc, :], in_=self.s[:, kc, :], func=AF.Square)
            cx.act.activation(out=self.sb[:, kc, :], in_=self.s[:, kc, :], func=AF.Copy)
        b1, b2 = self.bank(), self.bank()
        for kc in range(KC):
            cx.pe.matmul(self.ps[:, b1, :], lhsT=self.ones_b.all(), rhs=self.sb[:, kc, :], start=(kc == 0), stop=(kc == KC - 1))
        for kc in range(KC):
            cx.pe.matmul(self.ps[:, b2, :], lhsT=self.ones_b.all(), rhs=self.sqb[:, kc, :], start=(kc == 0), stop=(kc == KC - 1))
        mean, var, rstd, tmp = (self.stat[:, q, :] for q in range(4))
        cx.dve.tensor_copy(out=mean, in_=self.ps[:, b1, :])
        cx.dve.tensor_tensor(out=tmp, in0=mean, in1=mean, op=ALU.mult)
        cx.dve.tensor_tensor(out=var, in0=self.ps[:, b2, :], in1=tmp, op=ALU.subtract)
        cx.act.activation(out=rstd, in_=var, func=AF.Sqrt, bias=self.eps_ln[:, 0:1], scale=1.0)
        cx.dve.reciprocal(out=rstd, in_=rstd)
        for kc in range(KC):
            cx.dve.tensor_tensor(out=self.s[:, kc, :], in0=self.s[:, kc, :], in1=mean, op=ALU.subtract)
            cx.dve.tensor_tensor(out=self.s[:, kc, :], in0=self.s[:, kc, :], in1=rstd, op=ALU.mult)
            cx.act.activation(out=self.x[:, kc, :], in_=self.s[:, kc, :], func=AF.Identity,
                              bias=self.pvv(f"ln_b{i}.{j}", kc), scale=self.pvv(f"ln_g{i}.{j}", kc))
            cx.act.activation(out=self.xb[:, kc, :], in_=self.s[:, kc, :], func=AF.Identity,
                              bias=self.pvv(f"ln_b{i}.{j}", kc), scale=self.pvv(f"ln_g{i}.{j}", kc))

    def ffn(self, i):
        cx = self.c
        nfc = FH // 128
        cx.areset()
        self.hm = cx.carve("hm", [128, nfc, T], BF16, split=1)
        self.gsil = [cx.carve(f"gsil{q}", [128, T], F32) for q in range(2)]
        for b in range(11):
            w = self.weight(f"L{i}.f_in{b}")
            for h in range(2):
                m = b * 2 + h
                bg, bu = self.bank(), self.bank()
                for kc in range(KC):
                    cx.pe.matmul(self.ps[:, bg, :], lhsT=V(w.ap[:, kc, h * 128:(h + 1) * 128], w.bufs), rhs=self.xb[:, kc, :],
                                 start=(kc == 0), stop=(kc == KC - 1))
                for kc in range(KC):
                    cx.pe.matmul(self.ps[:, bu, :], lhsT=V(w.ap[:, kc, 256 + h * 128:256 + (h + 1) * 128], w.bufs), rhs=self.xb[:, kc, :],
                                 start=(kc == 0), stop=(kc == KC - 1))
                g = self.gsil[m % 2]
                cx.act.activation(out=g.all(), in_=self.ps[:, bg, :], func=AF.Silu)
                cx.dve.tensor_tensor(out=self.hm[:, m, :], in0=self.ps[:, bu, :], in1=g.all(), op=ALU.mult)
        for mo in range(KC):
            w = self.weight(f"L{i}.f_out{mo}")
            bo = self.bank()
            for m in range(nfc):
                cx.pe.matmul(self.ps[:, bo, :], lhsT=V(w.ap[:, m, :], w.bufs), rhs=self.hm[:, m, :], start=(m == 0), stop=(m == nfc - 1))
            cx.dve.scalar_tensor_tensor(out=self.s[:, mo, :], in0=self.x[:, mo, :], scalar=float(ALPHA), in1=self.ps[:, bo, :],
                                        op0=ALU.mult, op1=ALU.add)

    def mixer(self, i, s, ti):
        kind = i % 3
        for sub in range(T // self.TM):
            t0 = sub * self.TM
            self.xbv = Tile("xbv", self.xb.ap[:, :, t0:t0 + self.TM], 1, self.xb.bsplit)
            self.xv = Tile("xv", self.x.ap[:, :, t0:t0 + self.TM], 1, self.x.bsplit)
            self.sv = Tile("sv", self.s.ap[:, :, t0:t0 + self.TM], 1, self.s.bsplit)
            if kind == 0:
                self.mlstm(i)
            elif kind == 1:
                self.mamba(i)
            else:
                self.rwkv(i)

    def rwkv(self, i):
        TM = self.TM
        xb_t, x_t, s_t = self.xbv, self.xv, self.sv
        c = self.c
        ps = self.ps
        NCH = TM // 128
        ST, STb, xlast = self.rw_state[i]
        bcol = self.bc_names[f"rw{i}"]
        c.areset()
        AR = c.carve("AR", [128, 8, NCH, 256], BF16, split=1)
        BtT = c.carve("BtT", [128, 8, TM], BF16, split=1)
        KtT = c.carve("KtT", [128, 8, TM], BF16, split=1)
        rk_tok = c.carve("rk_tok", [128, NCH, 16], F32)
        WL = c.carve("WL", [128, NCH, 8], F32)
        offM = c.ar_off
        cw = c.carve("cw", [128, 8, TM], F32, split=1)
        asg = c.carve("asg", [128, 8, TM], BF16, split=1)
        xx = c.carve("xx", [128, 8, TM], BF16, split=1)
        offP = offM + 5 * (8 * TM * 2)
        assert c.ar_off <= offP
        c.ar_off = offP
        xj = c.carve("xj", [128, 8, TM], BF16, split=1)
        lo = [c.carve(f"lo{q}", [128, TM], BF16) for q in range(2)]
        tf = [c.carve(f"tf{q}", [128, TM], F32) for q in range(5)]
        sqk = c.carve("sqk", [128, TM], BF16)
        pr = c.carve("pr", [128, TM], BF16)
        pname = lambda nm, k: self.pvv(f"rw{i}_{nm}", k)

        def mix(j):
            for kc in range(KC):
                c.dve.scalar_tensor_tensor(out=xj[:, kc, :], in0=xx[:, kc, :], scalar=pname("mix", j * 8 + kc), in1=x_t[:, kc, :], op0=ALU.mult, op1=ALU.add)

        v4 = lambda t_: V(t_.ap.rearrange("p (c t) -> p c t", c=NCH), t_.bufs)
        for kc in range(KC):
            c.dve.tensor_tensor(out=xx[:, kc, 1:TM], in0=x_t[:, kc, 0:TM - 1], in1=x_t[:, kc, 1:TM], op=ALU.subtract)
            c.dve.tensor_tensor(out=xx[:, kc, 0:1], in0=xlast[:, kc, :], in1=x_t[:, kc, 0:1], op=ALU.subtract)
        c.pool.tensor_copy(out=xlast.all(), in_=V(x_t.ap[:, :, TM - 1:TM], x_t.bufs))
        mix(1)
        w = self.weight(f"L{i}.rw_w1")
        b = self.bank()
        for kc in range(KC):
            c.pe.matmul(ps[0:64, b, 0:TM], lhsT=V(w.ap[:, kc, :], w.bufs), rhs=xj[:, kc, :], start=(kc == 0), stop=(kc == KC - 1))
        c.act.activation(out=lo[0][0:64, :], in_=ps[0:64, b, 0:TM], func=AF.Tanh)
        w = self.weight(f"L{i}.rw_w2")
        for m in range(8):
            b = self.bank()
            c.pe.matmul(ps[:, b, 0:TM], lhsT=V(w.ap[0:64, 0, m * 128:(m + 1) * 128], w.bufs), rhs=lo[0][0:64, :], start=True, stop=True)
            c.act.activation(out=tf[m % 2].all(), in_=ps[:, b, 0:TM], func=AF.Sigmoid, bias=pname("w0", m), scale=1.0)
            c.dve.tensor_tensor_scan(out=cw[:, m, :], data0=self.rmask[:, 0:TM], data1=tf[m % 2].all(), initial=0.0, op0=ALU.mult, op1=ALU.add)
            c.act.activation(out=V(WL.ap[:, :, m], WL.bufs), in_=V(cw.ap[:, m, 127::128], cw.bsplit[m]), func=AF.Exp, scale=-WSC)
        mix(0)
        for rb in range(2):
            w = self.weight(f"L{i}.rw_r{rb}")
            for m in range(4):
                ko = rb * 4 + m
                b = self.bank()
                for kc in range(KC):
                    c.pe.matmul(ps[:, b, 0:TM], lhsT=V(w.ap[:, kc, m * 128:(m + 1) * 128], w.bufs), rhs=xj[:, kc, :], start=(kc == 0), stop=(kc == KC - 1))
                ex = tf[2 + ko % 2]
                c.act.activation(out=ex.all(), in_=cw[:, ko, :], func=AF.Exp, scale=-WSC)
                c.dve.tensor_tensor(out=V(AR.ap[:, ko, :, 128:256], AR.bsplit[ko]), in0=V(ps.ap[:, b, 0:TM].rearrange("p (c t) -> p c t", c=NCH), ps.bsplit[b]), in1=v4(ex), op=ALU.mult)
        mix(4)
        w = self.weight(f"L{i}.rw_a1")
        b = self.bank()
        for kc in range(KC):
            c.pe.matmul(ps[0:64, b, 0:TM], lhsT=V(w.ap[:, kc, :], w.bufs), rhs=xj[:, kc, :], start=(kc == 0), stop=(kc == KC - 1))
        c.act.activation(out=lo[1][0:64, :], in_=ps[0:64, b, 0:TM], func=AF.Copy)
        w = self.weight(f"L{i}.rw_a2")
        for m in range(8):
            b = self.bank()
            c.pe.matmul(ps[:, b, 0:TM], lhsT=V(w.ap[0:64, 0, m * 128:(m + 1) * 128], w.bufs), rhs=lo[1][0:64, :], start=True, stop=True)
            c.act.activation(out=asg[:, m, :], in_=ps[:, b, 0:TM], func=AF.Sigmoid, bias=pname("a0", m), scale=1.0)
        mix(2)
        for kb in range(2):
            w = self.weight(f"L{i}.rw_k{kb}")
            for m in range(4):
                ko = kb * 4 + m
                b = self.bank()
                for kc in range(KC):
                    c.pe.matmul(ps[:, b, 0:TM], lhsT=V(w.ap[:, kc, m * 128:(m + 1) * 128], w.bufs), rhs=xj[:, kc, :], start=(kc == 0), stop=(kc == KC - 1))
                kkr, ex, em, t1, kkn = tf
                c.act.activation(out=kkr.all(), in_=ps[:, b, 0:TM], func=AF.Copy, scale=pname("k_k", ko))
                c.act.activation(out=sqk.all(), in_=kkr.all(), func=AF.Square)
                b2 = self.bank()
                c.pe.matmul(ps[:, b2, 0:TM], lhsT=self.blockones.all(), rhs=sqk.all(), start=True, stop=True)
                c.dve.tensor_scalar(out=kkn.all(), in0=ps[:, b2, 0:TM], scalar1=1e-24, scalar2=None, op0=ALU.max)
                c.act.activation(out=kkn.all(), in_=kkn.all(), func=AF.Sqrt)
                c.dve.reciprocal(out=kkn.all(), in_=kkn.all())
                c.dve.tensor_tensor(out=kkn.all(), in0=kkn.all(), in1=kkr.all(), op=ALU.mult)
                c.act.activation(out=ex.all(), in_=cw[:, ko, :], func=AF.Exp, scale=-WSC)
                c.act.activation(out=em.all(), in_=cw[:, ko, :], func=AF.Exp, scale=WSC)
                kk4, ex4 = v4(kkn), v4(ex)
                c.dve.scalar_tensor_tensor(out=V(AR.ap[:, ko, :, 1:128], AR.bsplit[ko]), in0=V(kk4.ap[:, :, 1:128], kkn.bufs), scalar=-1.0, in1=V(ex4.ap[:, :, 0:127], ex.bufs),
                                           op0=ALU.mult, op1=ALU.mult)
                c.pool.tensor_scalar(out=V(AR.ap[:, ko, :, 0:1], AR.bsplit[ko]), in0=V(kk4.ap[:, :, 0:1], kkn.bufs), scalar1=-1.0, scalar2=None, op0=ALU.mult)
                c.dve.tensor_tensor(out=kkn.all(), in0=kkn.all(), in1=asg[:, ko, :], op=ALU.mult)
                c.dve.tensor_tensor(out=BtT[:, ko, :], in0=kkn.all(), in1=em.all(), op=ALU.mult)
                c.dve.tensor_scalar(out=t1.all(), in0=asg[:, ko, :], scalar1=pname("k_a", ko), scalar2=self.rw_omka[i][:, ko:ko + 1], op0=ALU.mult, op1=ALU.add)
                c.dve.tensor_tensor(out=t1.all(), in0=ps[:, b, 0:TM], in1=t1.all(), op=ALU.mult)
                c.dve.tensor_tensor(out=KtT[:, ko, :], in0=t1.all(), in1=em.all(), op=ALU.mult)
                c.dve.scalar_tensor_tensor(out=v4(pr), in0=V(AR.ap[:, ko, :, 128:256], AR.bsplit[ko]), scalar=pname("r_k", ko), in1=v4(V(KtT.ap[:, ko, :], KtT.bsplit[ko])),
                                           op0=ALU.mult, op1=ALU.mult)
                brk = self.bank()
                for ch in range(NCH):
                    c.pe.matmul(ps[:, brk, ch * 2:ch * 2 + 2], lhsT=pr[:, ch * 128:(ch + 1) * 128], rhs=self.sel2.all(), start=True, stop=True)
                c.dve.tensor_copy(out=V(rk_tok.ap[:, :, ko * 2:ko * 2 + 2], rk_tok.bufs), in_=V(ps.ap[:, brk, 0:NCH * 2].rearrange("p (c h) -> p c h", c=NCH), ps.bsplit[brk]))
        c.ar_off = offM
        Vt = c.carve("Vt", [128, NCH, 1024], BF16, split=1)
        gT = c.carve("gT", [128, 8, TM], BF16, split=1)
        Btok = c.carve("Btok", [128, NCH, 1024], BF16, split=1)
        Ktok = c.carve("Ktok", [128, NCH, 1024], BF16, split=1)
        yg = c.carve("yg", [128, 8, TM], BF16, split=1)
        assert c.ar_off <= offP
        mix(3)
        for vb in range(2):
            w = self.weight(f"L{i}.rw_v{vb}")
            for ch in range(NCH):
                b = self.bank()
                for kc in range(KC):
                    c.pe.matmul(ps[:, b, :], lhsT=xj[:, kc, ch * 128:(ch + 1) * 128], rhs=V(w.ap[:, kc, :], w.bufs), start=(kc == 0), stop=(kc == KC - 1))
                if (ch + vb) % 2:
                    c.act.activation(out=Vt[:, ch, vb * 512:(vb + 1) * 512], in_=ps[:, b, :], func=AF.Copy)
                else:
                    c.dve.tensor_copy(out=Vt[:, ch, vb * 512:(vb + 1) * 512], in_=ps[:, b, :])
        mix(5)
        w = self.weight(f"L{i}.rw_g1")
        b = self.bank()
        for kc in range(KC):
            c.pe.matmul(ps[:, b, 0:TM], lhsT=V(w.ap[:, kc, :], w.bufs), rhs=xj[:, kc, :], start=(kc == 0), stop=(kc == KC - 1))
        c.act.activation(out=lo[0].all(), in_=ps[:, b, 0:TM], func=AF.Sigmoid)
        w = self.weight(f"L{i}.rw_g2")
        for m in range(8):
            b = self.bank()
            c.pe.matmul(ps[:, b, 0:TM], lhsT=V(w.ap[:, 0, m * 128:(m + 1) * 128], w.bufs), rhs=lo[0].all(), start=True, stop=True)
            if m % 2:
                c.act.activation(out=gT[:, m, :], in_=ps[:, b, 0:TM], func=AF.Copy)
            else:
                c.dve.tensor_copy(out=gT[:, m, :], in_=ps[:, b, 0:TM])
        for src, dst in ((BtT, Btok), (KtT, Ktok)):
            for ch in range(NCH):
                for half in range(2):
                    bT = self.bank()
                    pb = ps.ap[:, bT, :].bitcast(BF16)
                    for r in range(4):
                        kc = half * 4 + r
                        c.pe.transpose(V(pb[:, r * 128:(r + 1) * 128], ps.bsplit[bT]), src[:, kc, ch * 128:(ch + 1) * 128], self.ident_b.all())
                    e = c.act if half else c.dve
                    if half:
                        c.act.activation(out=dst[:, ch, half * 512:(half + 1) * 512], in_=V(pb[:, 0:512], ps.bsplit[bT]), func=AF.Copy)
                    else:
                        c.dve.tensor_copy(out=dst[:, ch, half * 512:(half + 1) * 512], in_=V(pb[:, 0:512], ps.bsplit[bT]))
        import os
        dbg = int(os.environ.get("RWDBG", "0"))
        c.ar_off = offP
        XT = c.carve("XT", [128, 8, 4, 128], BF16)
        Xp = [c.carve(f"Xp{q}", [128, 8, 128], BF16) for q in range(2)]
        XTp = [c.carve(f"XTp{q}", [128, 8, 128], BF16) for q in range(2)]
        TTp = [c.carve(f"TTp{q}", [128, 8, 128], BF16) for q in range(2)]
        RHSb = c.carve("RHSb", [128, 8, 64], BF16)
        Ub = c.carve("Ub", [128, 8, 64], BF16)
        Ysb = c.carve("Ysb", [128, 8, 64], F32)
        Ysq = c.carve("Ysq", [128, 8, 64], F32)
        yv = c.carve("yv", [128, 8, 64], BF16)
        st8 = {k: c.carve("st8_" + k, [128, 8], F32) for k in ("s1", "s2", "mean", "var", "rstd")}
        if dbg == 1:
            for kc in range(KC):
                c.dve.tensor_copy(out=yg[:, kc, :], in_=gT[:, kc, :])
        for ch in range(NCH if dbg != 1 else 0):
            cs = slice(ch * 128, (ch + 1) * 128)
            for half in range(2):
                h0 = half * 8
                hk = lambda hh: ((h0 + hh) // 2, ((h0 + hh) % 2) * 64 if not os.environ.get("RWBASE0") else 0)
                for hh in range(8):
                    kc, ba = hk(hh)
                    bX = self.bank()
                    arR = V(AR.ap[ba:ba + 64, kc, ch, :], AR.bsplit[kc])
                    c.pe.matmul(ps[:, bX, 0:256], lhsT=V(BtT.ap[ba:ba + 64, kc, cs], BtT.bsplit[kc]), rhs=arR, start=True, stop=True)
                    c.pe.matmul(ps[:, bX, 256:512], lhsT=V(KtT.ap[ba:ba + 64, kc, cs], KtT.bsplit[kc]), rhs=arR, start=True, stop=True)
                    c.dve.tensor_tensor(out=V(XT.ap[:, hh, :, :].rearrange("p a t -> p (a t)"), XT.bufs), in0=ps[:, bX, :], in1=self.maskX.all(), op=ALU.mult)
                for q in range(2):
                    bM = self.bank()
                    for r in range(4):
                        hh = r * 2 + q
                        kc, ba = hk(hh)
                        c.pe.matmul(ps[:, bM, r * 128:(r + 1) * 128], lhsT=V(AR.ap[ba:ba + 64, kc, ch, 0:128], AR.bsplit[kc]), rhs=V(BtT.ap[ba:ba + 64, kc, cs], BtT.bsplit[kc]), start=True, stop=True)
                    c.dve.tensor_tensor(out=V(Xp[0].ap[:, q::2, :], Xp[0].bufs), in0=V(ps.ap[:, bM, :].rearrange("p (h t) -> p h t", h=4), ps.bsplit[bM]),
                                        in1=V(self.lowS.ap.unsqueeze(1).to_broadcast([128, 4, 128]), self.lowS.bufs), op=ALU.mult)
                if dbg == 3:
                    kc0 = h0 // 2
                    c.dve.tensor_copy(out=yg[:, kc0:kc0 + 4, cs], in_=V(XT.ap[:, 0:4, 0, :], XT.bufs))
                    continue
                c.dve.tensor_tensor(out=TTp[0].all(), in0=V(XT.ap[:, :, 0, :], XT.bufs), in1=V(self.ident_b.ap.unsqueeze(1).to_broadcast([128, 8, 128]), self.ident_b.bufs), op=ALU.add)
                xcur, xtcur, ttcur = Xp[0], None, TTp[0]

                def xt_of(hh):
                    if xtcur is None:
                        return V(XT.ap[:, hh, 0, :], XT.bufs)
                    return V(xtcur.ap[:, hh, :], xtcur.bufs)
                for lvl in range(1, 7):
                    xn = Xp[lvl % 2]
                    xtn = XTp[lvl % 2]
                    ttn = TTp[lvl % 2]
                    bXs = [self.bank(), self.bank()]
                    for hh in range(8):
                        c.pe.matmul(ps[:, bXs[hh // 4], (hh % 4) * 128:(hh % 4 + 1) * 128], lhsT=xt_of(hh), rhs=V(xcur.ap[:, hh, :], xcur.bufs), start=True, stop=True)
                    for q in range(2):
                        src_ = V(ps.ap[:, bXs[q], :].rearrange("p (h t) -> p h t", h=4), ps.bsplit[bXs[q]])
                        if q:
                            c.act.activation(out=V(xn.ap[:, q * 4:(q + 1) * 4, :], xn.bufs), in_=src_, func=AF.Copy)
                        else:
                            c.dve.tensor_copy(out=V(xn.ap[:, q * 4:(q + 1) * 4, :], xn.bufs), in_=src_)
                    if lvl < 6:
                        bTs = [self.bank(), self.bank()]
                        for hh in range(8):
                            c.pe.matmul(ps[:, bTs[hh // 4], (hh % 4) * 128:(hh % 4 + 1) * 128], lhsT=V(xcur.ap[:, hh, :], xcur.bufs), rhs=xt_of(hh), start=True, stop=True)
                        for q in range(2):
                            src_ = V(ps.ap[:, bTs[q], :].rearrange("p (h t) -> p h t", h=4), ps.bsplit[bTs[q]])
                            if q:
                                c.dve.tensor_copy(out=V(xtn.ap[:, q * 4:(q + 1) * 4, :], xtn.bufs), in_=src_)
                            else:
                                c.act.activation(out=V(xtn.ap[:, q * 4:(q + 1) * 4, :], xtn.bufs), in_=src_, func=AF.Copy)
                    bAs = [self.bank(), self.bank()]
                    for hh in range(8):
                        c.pe.matmul(ps[:, bAs[hh // 4], (hh % 4) * 128:(hh % 4 + 1) * 128], lhsT=V(xn.ap[:, hh, :], xn.bufs), rhs=V(ttcur.ap[:, hh, :], ttcur.bufs), start=True, stop=True)
                    for q in range(2):
                        c.dve.tensor_tensor(out=V(ttn.ap[:, q * 4:(q + 1) * 4, :], ttn.bufs), in0=V(ps.ap[:, bAs[q], :].rearrange("p (h t) -> p h t", h=4), ps.bsplit[bAs[q]]),
                                            in1=V(ttcur.ap[:, q * 4:(q + 1) * 4, :], ttcur.bufs), op=ALU.add)
                    xcur, ttcur = xn, ttn
                    xtcur = xtn
                if dbg == 2:
                    kc0 = h0 // 2
                    c.dve.tensor_copy(out=yg[:, kc0:kc0 + 4, cs], in_=V(ttcur.ap[:, 0:4, :], ttcur.bufs))
                    continue
                bR = [self.bank(), self.bank()]
                for hh in range(8):
                    kc, ba = hk(hh)
                    h = h0 + hh
                    o = ps[:, bR[hh % 2], (hh // 2) * 64:(hh // 2 + 1) * 64]
                    c.pe.matmul(o, lhsT=V(AR.ap[ba:ba + 64, kc, ch, 0:128], AR.bsplit[kc]), rhs=V(STb.ap[ba:ba + 64, kc, :], STb.bufs), start=True, stop=False)
                    c.pe.matmul(o, lhsT=V(XT.ap[:, hh, 2, :], XT.bufs), rhs=Vt[:, ch, h * 64:(h + 1) * 64], start=False, stop=True)
                for q in range(2):
                    src_ = V(ps.ap[:, bR[q], 0:256].rearrange("p (r v) -> p r v", r=4), ps.bsplit[bR[q]])
                    if q:
                        c.act.activation(out=V(RHSb.ap[:, q::2, :], RHSb.bufs), in_=src_, func=AF.Copy)
                    else:
                        c.dve.tensor_copy(out=V(RHSb.ap[:, q::2, :], RHSb.bufs), in_=src_)
                bU = self.bank()
                for hh in range(8):
                    c.pe.matmul(ps[:, bU, hh * 64:(hh + 1) * 64], lhsT=V(ttcur.ap[:, hh, :], ttcur.bufs), rhs=V(RHSb.ap[:, hh, :], RHSb.bufs), start=True, stop=True)
                c.dve.tensor_copy(out=V(Ub.ap.rearrange("p h v -> p (h v)"), Ub.bufs), in_=ps[:, bU, :])
                bYy = [self.bank(), self.bank()]
                for hh in range(8):
                    kc, ba = hk(hh)
                    h = h0 + hh
                    o = ps[:, bYy[hh % 2], (hh // 2) * 64:(hh // 2 + 1) * 64]
                    c.pe.matmul(o, lhsT=V(AR.ap[ba:ba + 64, kc, ch, 128:256], AR.bsplit[kc]), rhs=V(STb.ap[ba:ba + 64, kc, :], STb.bufs), start=True, stop=False)
                    c.pe.matmul(o, lhsT=V(XT.ap[:, hh, 1, :], XT.bufs), rhs=V(Ub.ap[:, hh, :], Ub.bufs), start=False, stop=False)
                    c.pe.matmul(o, lhsT=V(XT.ap[:, hh, 3, :], XT.bufs), rhs=Vt[:, ch, h * 64:(h + 1) * 64], start=False, stop=True)
                bSs = self.bank()
                for hh in range(8):
                    kc, ba = hk(hh)
                    h = h0 + hh
                    o = ps[:, bSs, hh * 64:(hh + 1) * 64]
                    c.pe.matmul(o, lhsT=Btok[:, ch, kc * 128:(kc + 1) * 128], rhs=V(Ub.ap[:, hh, :], Ub.bufs), start=True, stop=False)
                    c.pe.matmul(o, lhsT=Ktok[:, ch, kc * 128:(kc + 1) * 128], rhs=Vt[:, ch, h * 64:(h + 1) * 64], start=False, stop=True)
                kc0 = h0 // 2
                stv = V(ST.ap[:, kc0:kc0 + 4, :], ST.bufs)
                c.dve.tensor_tensor(out=stv, in0=stv, in1=V(WL.ap[:, ch, kc0:kc0 + 4].unsqueeze(2).to_broadcast([128, 4, 64]), WL.bufs), op=ALU.mult)
                for hh in range(8):
                    kc, ba = hk(hh)
                    sv = V(ST.ap[ba:ba + 64, kc, :], ST.bufs)
                    c.dve.scalar_tensor_tensor(out=sv, in0=V(ps.ap[ba:ba + 64, bSs, hh * 64:(hh + 1) * 64], ps.bsplit[bSs]), scalar=V(WL.ap[ba:ba + 64, ch, kc:kc + 1], WL.bufs), in1=sv,
                                               op0=ALU.mult, op1=ALU.add)
                c.act.activation(out=V(STb.ap[:, kc0:kc0 + 4, :], STb.bufs), in_=stv, func=AF.Copy)
                for q in range(2):
                    c.act.activation(out=V(Ysb.ap[:, q::2, :], Ysb.bufs), in_=V(ps.ap[:, bYy[q], 0:256].rearrange("p (r v) -> p r v", r=4), ps.bsplit[bYy[q]]), func=AF.Copy)
                c.act.activation(out=Ysq.all(), in_=Ysb.all(), func=AF.Square)
                c.dve.tensor_reduce(out=st8["s1"].all(), in_=Ysb.all(), axis=AX.X, op=ALU.add)
                c.dve.tensor_reduce(out=st8["s2"].all(), in_=Ysq.all(), axis=AX.X, op=ALU.add)
                c.dve.tensor_scalar(out=st8["mean"].all(), in0=st8["s1"].all(), scalar1=float(1.0 / 64.0), scalar2=None, op0=ALU.mult)
                c.dve.tensor_tensor(out=st8["var"].all(), in0=st8["mean"].all(), in1=st8["mean"].all(), op=ALU.mult)
                c.dve.scalar_tensor_tensor(out=st8["var"].all(), in0=st8["s2"].all(), scalar=float(1.0 / 64.0), in1=st8["var"].all(), op0=ALU.mult, op1=ALU.subtract)
                c.act.activation(out=st8["rstd"].all(), in_=st8["var"].all(), func=AF.Sqrt, bias=self.eps_lnx[:, 0:1], scale=1.0)
                c.dve.reciprocal(out=st8["rstd"].all(), in_=st8["rstd"].all())
                bc8 = lambda t_: V(t_.ap.unsqueeze(2).to_broadcast([128, 8, 64]), t_.bufs)
                c.dve.tensor_tensor(out=Ysb.all(), in0=Ysb.all(), in1=bc8(st8["mean"]), op=ALU.subtract)
                c.dve.tensor_tensor(out=Ysb.all(), in0=Ysb.all(), in1=bc8(st8["rstd"]), op=ALU.mult)
                f0 = h0 * 64
                lw_ = V(self.bc.ap[:, bcol + f0:bcol + f0 + 512].rearrange("p (h v) -> p h v", h=8), self.bc.bufs)
                lb_ = V(self.bc.ap[:, bcol + 1024 + f0:bcol + 1024 + f0 + 512].rearrange("p (h v) -> p h v", h=8), self.bc.bufs)
                c.dve.tensor_tensor(out=Ysb.all(), in0=Ysb.all(), in1=lw_, op=ALU.mult)
                c.dve.tensor_tensor(out=Ysb.all(), in0=Ysb.all(), in1=lb_, op=ALU.add)
                v3 = V(Vt.ap[:, ch, f0:f0 + 512].rearrange("p (h v) -> p h v", h=8), Vt.bsplit[ch])
                c.dve.tensor_tensor(out=Ysq.all(), in0=v3, in1=V(rk_tok.ap[:, ch, h0:h0 + 8].unsqueeze(2).to_broadcast([128, 8, 64]), rk_tok.bufs), op=ALU.mult)
                c.dve.tensor_tensor(out=yv.all(), in0=Ysb.all(), in1=Ysq.all(), op=ALU.add)
                bT = self.bank()
                pb = ps.ap[:, bT, :].bitcast(BF16)
                yvf = yv.ap.rearrange("p h v -> p (h v)")
                for r in range(4):
                    c.pe.transpose(V(pb[:, r * 128:(r + 1) * 128], ps.bsplit[bT]), V(yvf[:, r * 128:(r + 1) * 128], yv.bufs), self.ident_b.all())
                c.dve.tensor_tensor(out=yg[:, kc0:kc0 + 4, cs], in0=V(pb[:, 0:512].rearrange("p (k t) -> p k t", k=4), ps.bsplit[bT]), in1=gT[:, kc0:kc0 + 4, cs], op=ALU.mult)
        for ob in range(2):
            w = self.weight(f"L{i}.rw_o{ob}")
            for m in range(4):
                mo = ob * 4 + m
                b = self.bank()
                for kc in range(KC):
                    c.pe.matmul(ps[:, b, 0:TM], lhsT=V(w.ap[:, kc, m * 128:(m + 1) * 128], w.bufs), rhs=yg[:, kc, :], start=(kc == 0), stop=(kc == KC - 1))
                c.dve.scalar_tensor_tensor(out=s_t[:, mo, :], in0=x_t[:, mo, :], scalar=float(ALPHA), in1=ps[:, b, 0:TM], op0=ALU.mult, op1=ALU.add)

    def mamba(self, i):
        TM = self.TM
        xb_t, x_t, s_t = self.xbv, self.xv, self.sv
        c = self.c
        ps = self.ps
        NCH = TM // 128
        TW = TM + 4
        st, stb, hist, A_bc = self.ssm_state[i]
        bcol = self.bc_names[f"ssm{i}"]
        c.areset()
        uT = c.carve("uT", [128, 24, TW], BF16, split=1)
        offA = c.ar_off
        zs = c.carve("zs", [128, NCH, 2048], BF16, split=1)
        xs = c.carve("xs", [128, NCH, 2048], BF16, split=1)
        Btok = c.carve("Btok", [128, NCH, 512], BF16, split=1)
        BT = c.carve("BT", [128, 4, TM], BF16, split=1)
        CT = c.carve("CT", [128, 4, TM], BF16, split=1)
        yT = c.carve("yT", [128, 16, TM], BF16, split=1)
        sm = {k: c.carve("sm_" + k, [128, NCH, 32], F32) for k in ("dt", "dA", "cum", "ncum", "ecum", "cl", "ecl", "wx")}
        self.m_cbm = c.carve("m_cbm", [128, 512], BF16)
        cbrow = c.carve("cbrow", [128, 1024], BF16)
        self.m_xdt = c.carve("m_xdt", [128, 8, 64], BF16)
        self.m_xw = c.carve("m_xw", [128, 8, 64], BF16)
        self.m_yc = c.carve("m_yc", [128, 8, 64], F32)
        self.m_t1 = c.carve("m_t1", [128, 8, 64], BF16)
        self.m_yb = c.carve("m_yb", [128, 8, 64], BF16)
        self.m_ssq = c.carve("m_ssq", [128, 2], F32)
        self.m_rs = c.carve("m_rs", [128, 2], F32)
        w = self.weight(f"L{i}.ssm_dt")
        b = self.bank()
        for ch in range(NCH):
            for kc in range(KC):
                c.pe.matmul(ps[:, b, ch * 32:(ch + 1) * 32], lhsT=xb_t[:, kc, ch * 128:(ch + 1) * 128], rhs=V(w.ap[:, kc, :], w.bufs), start=(kc == 0), stop=(kc == KC - 1))
        dtb = V(self.bc.ap[:, bcol + 64:bcol + 96].unsqueeze(1).to_broadcast([128, NCH, 32]), self.bc.bufs)
        c.dve.tensor_tensor(out=sm["dt"].all(), in0=V(ps.ap[:, b, 0:NCH * 32].rearrange("p (c h) -> p c h", c=NCH), ps.bsplit[b]), in1=dtb, op=ALU.add)
        c.act.activation(out=sm["dt"].all(), in_=sm["dt"].all(), func=AF.Exp)
        c.act.activation(out=sm["dt"].all(), in_=sm["dt"].all(), func=AF.Ln, bias=self.one_c[:, 0:1], scale=1.0)
        c.dve.tensor_tensor(out=sm["dA"].all(), in0=sm["dt"].all(), in1=V(A_bc.ap.unsqueeze(1).to_broadcast([128, NCH, 32]), A_bc.bufs), op=ALU.mult)
        b = self.bank()
        flat = lambda t: V(t.ap.rearrange("p c h -> p (c h)"), t.bufs)
        nsm = NCH * 32
        c.pe.matmul(ps[:, b, 0:nsm], lhsT=self.tri.all(), rhs=flat(sm["dA"]), start=True, stop=True)
        c.pe.matmul(ps[:, b, 128:128 + nsm], lhsT=self.ones_f.all(), rhs=flat(sm["dA"]), start=True, stop=True)
        c.dve.tensor_copy(out=flat(sm["cum"]), in_=ps[:, b, 0:nsm])
        c.dve.tensor_copy(out=flat(sm["cl"]), in_=ps[:, b, 128:128 + nsm])
        c.dve.tensor_scalar(out=sm["ncum"].all(), in0=sm["cum"].all(), scalar1=-1.0, scalar2=None, op0=ALU.mult)
        c.act.activation(out=sm["ecum"].all(), in_=sm["cum"].all(), func=AF.Exp)
        c.act.activation(out=sm["ecl"].all(), in_=sm["cl"].all(), func=AF.Exp)
        c.dve.tensor_tensor(out=sm["wx"].all(), in0=sm["cl"].all(), in1=sm["cum"].all(), op=ALU.subtract)
        c.act.activation(out=sm["wx"].all(), in_=sm["wx"].all(), func=AF.Exp)
        c.dve.tensor_tensor(out=sm["wx"].all(), in0=sm["wx"].all(), in1=sm["dt"].all(), op=ALU.mult)
        for zb in range(4):
            w = self.weight(f"L{i}.ssm_z{zb}")
            for ch in range(NCH):
                b = self.bank()
                for kc in range(KC):
                    c.pe.matmul(ps[:, b, :], lhsT=xb_t[:, kc, ch * 128:(ch + 1) * 128], rhs=V(w.ap[:, kc, :], w.bufs), start=(kc == 0), stop=(kc == KC - 1))
                c.act.activation(out=zs[:, ch, zb * 512:(zb + 1) * 512], in_=ps[:, b, :], func=AF.Silu)
        c.pool.tensor_copy(out=V(uT.ap[:, :, 0:4], uT.bufs), in_=hist.all())
        for xb_ in range(6):
            w = self.weight(f"L{i}.ssm_x{xb_}")
            for m in range(4):
                cc = xb_ * 4 + m
                b = self.bank()
                for kc in range(KC):
                    c.pe.matmul(ps[:, b, 0:TM], lhsT=V(w.ap[:, kc, m * 128:(m + 1) * 128], w.bufs), rhs=xb_t[:, kc, :], start=(kc == 0), stop=(kc == KC - 1))
                if cc % 2:
                    c.act.activation(out=uT[:, cc, 4:4 + TM], in_=ps[:, b, 0:TM], func=AF.Copy)
                else:
                    c.dve.tensor_copy(out=uT[:, cc, 4:4 + TM], in_=ps[:, b, 0:TM])
        c.pool.tensor_copy(out=hist.all(), in_=V(uT.ap[:, :, TM:TM + 4], uT.bufs))
        wcb0 = self.weight(f"L{i}.ssm_cb")
        c.act.activation(out=cbrow[0:65, :], in_=V(wcb0.ap[0:65, 0, :], wcb0.bufs), func=AF.Copy)
        for cc in range(24):
            w = self.weight(f"L{i}.ssm_cw{cc}")
            if cc < 20:
                q = cc % 4
                if q == 0:
                    cvb = [self.bank() for _ in range(NCH)]
                for ch in range(NCH):
                    o = ps[:, cvb[ch], q * 128:(q + 1) * 128]
                    for tap in range(4):
                        c.pe.matmul(o, lhsT=uT[:, cc, 1 + tap + ch * 128:1 + tap + (ch + 1) * 128], rhs=V(w.ap[:, 0, tap * 128:(tap + 1) * 128], w.bufs), start=(tap == 0), stop=False)
                    c.pe.matmul(o, lhsT=self.ones1_b[(cc // 8) * 32:(cc // 8) * 32 + 1, :], rhs=cbrow[(cc // 8) * 32:(cc // 8) * 32 + 1, (cc % 8) * 128:(cc % 8 + 1) * 128], start=False, stop=True)
                if q == 3:
                    for ch in range(NCH):
                        if cc < 16:
                            c.act.activation(out=xs[:, ch, (cc - 3) * 128:(cc + 1) * 128], in_=ps[:, cvb[ch], :], func=AF.Silu)
                        else:
                            c.act.activation(out=Btok[:, ch, :], in_=ps[:, cvb[ch], :], func=AF.Silu)
            if cc >= 16:
                b = self.bank()
                for tap in range(4):
                    c.pe.matmul(ps[:, b, 0:TM], lhsT=V(w.ap[:, 0, tap * 128:(tap + 1) * 128], w.bufs), rhs=uT[:, cc, 1 + tap:1 + tap + TM], start=(tap == 0), stop=(tap == 3))
                dst = BT[:, cc - 16, :] if cc < 20 else CT[:, cc - 20, :]
                c.act.activation(out=dst, in_=ps[:, b, 0:TM], func=AF.Silu, bias=self.pvv(f"ssm_cb{i}", cc), scale=1.0)
        hi = c.ar_off
        c.ar_off = 0
        dg = c.carve("dg", [128, 8, 128], F32)
        seg = c.carve("seg", [128, 8, 128], F32)
        MT = c.carve("MT", [128, 8, 128], BF16)
        assert c.ar_off <= offA
        c.ar_off = hi
        for ch in range(NCH):
            cs = slice(ch * 128, (ch + 1) * 128)
            bCB = self.bank()
            for g in range(4):
                c.pe.matmul(ps[:, bCB, g * 128:(g + 1) * 128], lhsT=BT[:, g, cs], rhs=CT[:, g, cs], start=True, stop=True)
            c.dve.tensor_tensor(out=self.m_cbm.all(), in0=ps[:, bCB, :], in1=self.mask4.all(), op=ALU.mult)
            for blk in range(4):
                h0 = blk * 8
                g = blk
                xs3 = V(xs.ap[:, ch, h0 * 64:(h0 + 8) * 64].rearrange("p (h q) -> p h q", q=64), xs.bsplit[ch])
                zs3 = V(zs.ap[:, ch, h0 * 64:(h0 + 8) * 64].rearrange("p (h q) -> p h q", q=64), zs.bsplit[ch])

                def hb(t, n):
                    return V(t.ap[:, ch, h0:h0 + 8].unsqueeze(2).to_broadcast([128, 8, n]), t.bufs)
                xdt, xw, yc, t1, yb = self.m_xdt, self.m_xw, self.m_yc, self.m_t1, self.m_yb
                c.dve.tensor_tensor(out=xdt.all(), in0=xs3, in1=hb(sm["dt"], 64), op=ALU.mult)
                c.pool.tensor_tensor(out=xw.all(), in0=xs3, in1=hb(sm["wx"], 64), op=ALU.mult)
                bI = self.bank()
                c.pe.matmul(ps[:, bI, :], lhsT=CT[:, g, cs], rhs=V(stb.ap[:, h0:h0 + 8, :].rearrange("p h q -> p (h q)"), stb.bufs), start=True, stop=True)
                c.dve.tensor_tensor(out=yc.all(), in0=V(ps.ap[:, bI, :].rearrange("p (h q) -> p h q", q=64), ps.bsplit[bI]), in1=hb(sm["ecum"], 64), op=ALU.mult)
                c.dve.tensor_tensor(out=dg.all(), in0=V(self.ident_f.ap.unsqueeze(1).to_broadcast([128, 8, 128]), self.ident_f.bufs), in1=hb(sm["cum"], 128), op=ALU.mult)
                for q in range(2):
                    bG = self.bank()
                    c.pe.matmul(ps[:, bG, :], lhsT=self.ones_f.all(), rhs=V(dg.ap[:, q * 4:(q + 1) * 4, :].rearrange("p h t -> p (h t)"), dg.bufs), start=True, stop=True)
                    c.dve.tensor_tensor(out=V(seg.ap[:, q * 4:(q + 1) * 4, :], seg.bufs), in0=V(ps.ap[:, bG, :].rearrange("p (h t) -> p h t", t=128), ps.bsplit[bG]),
                                        in1=V(sm["ncum"].ap[:, ch, h0 + q * 4:h0 + (q + 1) * 4].unsqueeze(2).to_broadcast([128, 4, 128]), sm["ncum"].bufs), op=ALU.add)
                c.dve.tensor_scalar(out=seg.all(), in0=seg.all(), scalar1=0.0, scalar2=None, op0=ALU.min)
                c.act.activation(out=seg.all(), in_=seg.all(), func=AF.Exp)
                cb4 = V(self.m_cbm.ap[:, g * 128:(g + 1) * 128].unsqueeze(1).to_broadcast([128, 8, 128]), self.m_cbm.bufs)
                c.dve.tensor_tensor(out=MT.all(), in0=seg.all(), in1=cb4, op=ALU.mult)
                bY = self.bank()
                for hh in range(8):
                    c.pe.matmul(ps[:, bY, hh * 64:(hh + 1) * 64], lhsT=V(MT.ap[:, hh, :], MT.bufs), rhs=V(xdt.ap[:, hh, :], xdt.bufs), start=True, stop=True)
                c.dve.tensor_tensor(out=yc.all(), in0=V(ps.ap[:, bY, :].rearrange("p (h q) -> p h q", q=64), ps.bsplit[bY]), in1=yc.all(), op=ALU.add)
                dbc = V(self.bc.ap[:, bcol + 32 + h0:bcol + 32 + h0 + 8].unsqueeze(2).to_broadcast([128, 8, 64]), self.bc.bufs)
                c.dve.tensor_tensor(out=t1.all(), in0=xs3, in1=dbc, op=ALU.mult)
                c.dve.tensor_tensor(out=yc.all(), in0=yc.all(), in1=t1.all(), op=ALU.add)
                c.dve.tensor_tensor(out=yc.all(), in0=yc.all(), in1=zs3, op=ALU.mult)
                c.act.activation(out=t1.all(), in_=yc.all(), func=AF.Square, accum_out=self.m_ssq[:, 0:1])
                c.act.activation(out=self.m_rs[:, 0:1], in_=self.m_ssq[:, 0:1], func=AF.Sqrt, bias=self.eps_rms[:, 0:1], scale=float(1.0 / 512.0))
                c.dve.reciprocal(out=self.m_rs[:, 0:1], in_=self.m_rs[:, 0:1])
                c.dve.tensor_scalar(out=yb.all(), in0=yc.all(), scalar1=self.m_rs[:, 0:1], scalar2=None, op0=ALU.mult)
                ybf = yb.ap.rearrange("p h q -> p (h q)")
                bT = self.bank()
                pb = ps.ap[:, bT, :].bitcast(BF16)
                for r in range(4):
                    c.pe.transpose(V(pb[:, r * 128:(r + 1) * 128], ps.bsplit[bT]), V(ybf[:, r * 128:(r + 1) * 128], yb.bufs), self.ident_b.all())
                for r in range(4):
                    fc = blk * 4 + r
                    if r % 2:
                        c.act.activation(out=yT[:, fc, cs], in_=V(pb[:, r * 128:(r + 1) * 128], ps.bsplit[bT]), func=AF.Copy, scale=self.pvv(f"ssm_nw{i}", fc))
                    else:
                        c.dve.tensor_scalar(out=yT[:, fc, cs], in0=V(pb[:, r * 128:(r + 1) * 128], ps.bsplit[bT]), scalar1=self.pvv(f"ssm_nw{i}", fc), scalar2=None, op0=ALU.mult)
                bS = self.bank()
                c.pe.matmul(ps[:, bS, :], lhsT=Btok[:, ch, g * 128:(g + 1) * 128], rhs=V(xw.ap.rearrange("p h q -> p (h q)"), xw.bufs), start=True, stop=True)
                sv = V(st.ap[:, h0:h0 + 8, :], st.bufs)
                c.dve.tensor_tensor(out=sv, in0=sv, in1=hb(sm["ecl"], 64), op=ALU.mult)
                c.dve.tensor_tensor(out=sv, in0=V(ps.ap[:, bS, :].rearrange("p (h q) -> p h q", q=64), ps.bsplit[bS]), in1=sv, op=ALU.add)
                c.act.activation(out=V(stb.ap[:, h0:h0 + 8, :], stb.bufs), in_=sv, func=AF.Copy)
        for ob in range(4):
            w = self.weight(f"L{i}.ssm_out{ob}")
            for m in range(2):
                mo = ob * 2 + m
                b = self.bank()
                for kc in range(16):
                    c.pe.matmul(ps[:, b, 0:TM], lhsT=V(w.ap[:, kc, m * 128:(m + 1) * 128], w.bufs), rhs=yT[:, kc, :], start=(kc == 0), stop=(kc == 15))
                c.dve.scalar_tensor_tensor(out=s_t[:, mo, :], in0=x_t[:, mo, :], scalar=float(ALPHA), in1=ps[:, b, 0:TM], op0=ALU.mult, op1=ALU.add)

    def mlstm(self, i):
        TM = self.TM
        xb_t, x_t, s_t = self.xbv, self.xv, self.sv
        cx = self.c
        c = cx
        c.areset()
        self.qT = c.carve("qT", [128, 4, TM], BF16, split=1)
        self.kT = c.carve("kT", [128, 4, TM], BF16, split=1)
        self.ktok = c.carve("ktok", [128, TM // 128, 512], BF16, split=1)
        self.vtok = c.carve("vtok", [128, TM // 128, 1024], BF16, split=1)
        self.sgo = c.carve("sgo", [128, 8, TM], BF16, split=1)
        self.hn = c.carve("hn", [128, 8, TM], BF16, split=1)
        self.mg = {k: c.carve("mg_" + k, [128, n], F32) for k, n in
                   (("graw", 8 * (TM // 128)), ("gi", 4 * (TM // 128)), ("gf", 4 * (TM // 128)), ("bcum", 4 * (TM // 128)), ("blast", 4 * (TM // 128)), ("a_s", 4 * (TM // 128)), ("ws", 4 * (TM // 128)), ("dec", 4 * (TM // 128)))}
        self.diag = c.carve("diag", [128, 512], F32)
        self.scb = c.carve("scb", [128, 512], F32)
        self.dm = c.carve("dm", [128, 512], F32)
        self.pT = c.carve("pT", [128, 512], BF16)
        self.qs = c.carve("qs", [128, 512], BF16)
        self.rden = c.carve("rden", [128, 512], F32)
        self.hd = c.carve("hd", [128, 2, 512], F32, split=1)
        self.sqh = c.carve("sqh", [128, 2, 512], BF16, split=1)
        self.rstd_m = c.carve("rstd_m", [128, 512], F32)
        self.kw = c.carve("kw", [128, 512], BF16)
        st = self.ml_state[i]
        C, Cb, nbc, nbcb = st
        ps = self.ps
        NCH = TM // 128
        w = self.weight(f"L{i}.ml_g")
        bg = self.bank()
        for ch in range(NCH):
            for kc in range(KC):
                cx.pe.matmul(ps[:, bg, ch * 8:(ch + 1) * 8], lhsT=xb_t[:, kc, ch * 128:(ch + 1) * 128], rhs=V(w.ap[:, kc, :], w.bufs), start=(kc == 0), stop=(kc == KC - 1))
        g = self.mg
        bcol = self.bc_names[f"ml_bg{i}"]
        for ch in range(NCH):
            cx.dve.tensor_tensor(out=g["graw"][:, ch * 8:(ch + 1) * 8], in0=ps[:, bg, ch * 8:(ch + 1) * 8], in1=self.bc[:, bcol:bcol + 8], op=ALU.add)
        cx.act.activation(out=g["graw"].all(), in_=g["graw"].all(), func=AF.Tanh, scale=float(1.0 / 15.0))
        gr = g["graw"].ap.rearrange("p (c e) -> p c e", e=8)
        gi3 = g["gi"].ap.rearrange("p (c e) -> p c e", e=4)
        gf3 = g["gf"].ap.rearrange("p (c e) -> p c e", e=4)
        cx.dve.tensor_scalar(out=g["gi"].v(gi3), in0=g["graw"].v(gr[:, :, 0:4]), scalar1=15.0, scalar2=None, op0=ALU.mult)
        cx.act.activation(out=g["gf"].v(gf3), in_=g["graw"].v(gr[:, :, 4:8]), func=AF.Exp, scale=-15.0)
        cx.act.activation(out=g["gf"].all(), in_=g["gf"].all(), func=AF.Ln, bias=self.one_c[:, 0:1], scale=1.0)
        cx.dve.tensor_scalar(out=g["gf"].all(), in0=g["gf"].all(), scalar1=-1.0, scalar2=None, op0=ALU.mult)
        b1 = self.bank()
        ng = 4 * NCH
        cx.pe.matmul(ps[:, b1, 0:ng], lhsT=self.tri.all(), rhs=g["gf"].all(), start=True, stop=True)
        cx.pe.matmul(ps[:, b1, 32:32 + ng], lhsT=self.ones_f.all(), rhs=g["gf"].all(), start=True, stop=True)
        cx.dve.tensor_copy(out=g["bcum"].all(), in_=ps[:, b1, 0:ng])
        cx.dve.tensor_copy(out=g["blast"].all(), in_=ps[:, b1, 32:32 + ng])
        cx.dve.tensor_tensor(out=g["a_s"].all(), in0=g["gi"].all(), in1=g["bcum"].all(), op=ALU.subtract)
        cx.dve.tensor_tensor(out=g["ws"].all(), in0=g["a_s"].all(), in1=g["blast"].all(), op=ALU.add)
        cx.act.activation(out=g["ws"].all(), in_=g["ws"].all(), func=AF.Exp)
        cx.act.activation(out=g["dec"].all(), in_=g["blast"].all(), func=AF.Exp)
        w = self.weight(f"L{i}.ml_in0")
        for h in range(4):
            b = self.bank()
            for kc in range(KC):
                cx.pe.matmul(ps[:, b, 0:TM], lhsT=V(w.ap[:, kc, h * 128:(h + 1) * 128], w.bufs), rhs=xb_t[:, kc, :], start=(kc == 0), stop=(kc == KC - 1))
            cx.act.activation(out=self.qT[:, h, :], in_=ps[:, b, 0:TM], func=AF.Copy, scale=float(128 ** -0.5))
        w = self.weight(f"L{i}.ml_in1")
        for h in range(4):
            b = self.bank()
            for kc in range(KC):
                cx.pe.matmul(ps[:, b, 0:TM], lhsT=V(w.ap[:, kc, h * 128:(h + 1) * 128], w.bufs), rhs=xb_t[:, kc, :], start=(kc == 0), stop=(kc == KC - 1))
            cx.dve.tensor_copy(out=self.kT[:, h, :], in_=ps[:, b, 0:TM])
        for ch in range(NCH):
            b = self.bank()
            for kc in range(KC):
                cx.pe.matmul(ps[:, b, :], lhsT=xb_t[:, kc, ch * 128:(ch + 1) * 128], rhs=V(w.ap[:, kc, :], w.bufs), start=(kc == 0), stop=(kc == KC - 1))
            cx.act.activation(out=self.ktok[:, ch, :], in_=ps[:, b, :], func=AF.Copy)
        for vb in range(2):
            w = self.weight(f"L{i}.ml_in{2 + vb}")
            for ch in range(NCH):
                b = self.bank()
                for kc in range(KC):
                    cx.pe.matmul(ps[:, b, :], lhsT=xb_t[:, kc, ch * 128:(ch + 1) * 128], rhs=V(w.ap[:, kc, :], w.bufs), start=(kc == 0), stop=(kc == KC - 1))
                if (ch + vb) % 2:
                    cx.act.activation(out=self.vtok[:, ch, vb * 512:(vb + 1) * 512], in_=ps[:, b, :], func=AF.Copy)
                else:
                    cx.dve.tensor_copy(out=self.vtok[:, ch, vb * 512:(vb + 1) * 512], in_=ps[:, b, :])
        for ob in range(2):
            w = self.weight(f"L{i}.ml_in{4 + ob}")
            for m in range(4):
                fc = ob * 4 + m
                b = self.bank()
                for kc in range(KC):
                    cx.pe.matmul(ps[:, b, 0:TM], lhsT=V(w.ap[:, kc, m * 128:(m + 1) * 128], w.bufs), rhs=xb_t[:, kc, :], start=(kc == 0), stop=(kc == KC - 1))
                cx.act.activation(out=self.sgo[:, fc, :], in_=ps[:, b, 0:TM], func=AF.Sigmoid)
                cx.dve.tensor_scalar(out=self.sgo[:, fc, :], in0=self.sgo[:, fc, :], scalar1=self.pvv(f"ml_nw{i}", fc), scalar2=None, op0=ALU.mult)
        for ch in range(NCH):
            cs = slice(ch * 128, (ch + 1) * 128)
            bS, bB = self.bank(), self.bank()
            for h in range(4):
                cx.pe.matmul(ps[:, bS, h * 128:(h + 1) * 128], lhsT=self.kT[:, h, cs], rhs=self.qT[:, h, cs], start=True, stop=True)
            cx.dve.tensor_tensor(out=V(self.diag.ap.rearrange("p (h t) -> p h t", h=4), self.diag.bufs), in0=V(self.ident_f.ap.unsqueeze(1).to_broadcast([128, 4, 128]), self.ident_f.bufs),
                                 in1=V(g["bcum"].ap[:, ch * 4:(ch + 1) * 4].unsqueeze(2).to_broadcast([128, 4, 128]), g["bcum"].bufs), op=ALU.mult)
            cx.pe.matmul(ps[:, bB, :], lhsT=self.ones_f.all(), rhs=self.diag.all(), start=True, stop=True)
            cx.act.activation(out=self.scb.all(), in_=ps[:, bB, :], func=AF.Exp)
            for h in range(4):
                cx.dve.tensor_scalar(out=self.dm[:, h * 128:(h + 1) * 128], in0=ps[:, bB, h * 128:(h + 1) * 128], scalar1=g["a_s"][:, ch * 4 + h:ch * 4 + h + 1], scalar2=15.5,
                                     op0=ALU.add, op1=ALU.min)
            cx.act.activation(out=self.dm.all(), in_=self.dm.all(), func=AF.Exp)
            cx.dve.tensor_tensor(out=self.dm.all(), in0=self.dm.all(), in1=self.mask4.all(), op=ALU.mult)
            cx.dve.tensor_tensor(out=self.pT.all(), in0=ps[:, bS, :], in1=self.dm.all(), op=ALU.mult)
            qv = V(self.qT.ap[:, :, cs], self.qT.bufs)
            cx.dve.tensor_tensor(out=self.qs.v(self.qs.ap.rearrange("p (h t) -> p h t", h=4)), in0=qv, in1=self.scb.v(self.scb.ap.rearrange("p (h t) -> p h t", h=4)), op=ALU.mult)
            bD = self.bank()
            cx.pe.matmul(ps[:, bD, :], lhsT=self.ones1_b.all(), rhs=self.pT.all(), start=True, stop=False)
            for h in range(4):
                cx.pe.matmul(ps[:, bD, h * 128:(h + 1) * 128], lhsT=nbcb[:, h, :], rhs=self.qs[:, h * 128:(h + 1) * 128], start=False, stop=(h == 3))
            cx.dve.tensor_scalar(out=self.rden.all(), in0=ps[:, bD, :], scalar1=-1.0, scalar2=1.0, op0=ALU.mult, op1=ALU.max)
            cx.dve.tensor_tensor(out=self.rden.all(), in0=ps[:, bD, :], in1=self.rden.all(), op=ALU.max)
            cx.dve.reciprocal(out=self.rden.all(), in_=self.rden.all())
            bH = [self.bank(), self.bank()]
            for vc in range(2):
                for h in range(4):
                    cx.pe.matmul(ps[:, bH[vc], h * 128:(h + 1) * 128], lhsT=self.vtok[:, ch, h * 256 + vc * 128:h * 256 + (vc + 1) * 128], rhs=self.pT[:, h * 128:(h + 1) * 128],
                                 start=True, stop=False)
                    cx.pe.matmul(ps[:, bH[vc], h * 128:(h + 1) * 128], lhsT=Cb[:, h, vc * 128:(vc + 1) * 128], rhs=self.qs[:, h * 128:(h + 1) * 128], start=False, stop=True)
            for vc in range(2):
                cx.dve.tensor_tensor(out=self.hd[:, vc, :], in0=ps[:, bH[vc], :], in1=self.rden.all(), op=ALU.mult)
                cx.act.activation(out=self.sqh[:, vc, :], in_=self.hd[:, vc, :], func=AF.Square)
            bQ = self.bank()
            for vc in range(2):
                cx.pe.matmul(ps[:, bQ, :], lhsT=self.ones256_b.all(), rhs=self.sqh[:, vc, :], start=(vc == 0), stop=(vc == 1))
            cx.act.activation(out=self.rstd_m.all(), in_=ps[:, bQ, :], func=AF.Sqrt, bias=self.eps_rms[:, 0:1], scale=1.0)
            cx.dve.reciprocal(out=self.rstd_m.all(), in_=self.rstd_m.all())
            for vc in range(2):
                e = cx.dve
                e.tensor_tensor(out=self.hd[:, vc, :], in0=self.hd[:, vc, :], in1=self.rstd_m.all(), op=ALU.mult)
                hv = V(self.hd.ap[:, vc, :].rearrange("p (h t) -> p h t", h=4), self.hd.bsplit[vc])
                e.tensor_tensor(out=self.hn[:, vc::2, cs], in0=hv, in1=self.sgo[:, vc::2, cs], op=ALU.mult)
            cx.dve.tensor_tensor(out=V(self.kw.ap.rearrange("p (h t) -> p h t", h=4), self.kw.bufs), in0=V(self.ktok.ap[:, ch, :].rearrange("p (h t) -> p h t", h=4), self.ktok.bsplit[ch]),
                                 in1=V(g["ws"].ap[:, ch * 4:(ch + 1) * 4].unsqueeze(2).to_broadcast([128, 4, 128]), g["ws"].bufs), op=ALU.mult)
            bC = [self.bank(), self.bank()]
            bN = self.bank()
            for h in range(4):
                cx.pe.matmul(ps[:, bC[h // 2], (h % 2) * 256:(h % 2 + 1) * 256], lhsT=self.kw[:, h * 128:(h + 1) * 128], rhs=self.vtok[:, ch, h * 256:(h + 1) * 256], start=True, stop=True)
            for h in range(4):
                cx.pe.matmul(ps[:, bN, h * 128:(h + 1) * 128], lhsT=self.kw[:, h * 128:(h + 1) * 128], rhs=self.ones1_b.all(), start=True, stop=True)
            for h in range(4):
                dsc = g["dec"][:, ch * 4 + h:ch * 4 + h + 1]
                cx.dve.scalar_tensor_tensor(out=C[:, h, :], in0=C[:, h, :], scalar=dsc, in1=ps[:, bC[h // 2], (h % 2) * 256:(h % 2 + 1) * 256], op0=ALU.mult, op1=ALU.add)
                cx.dve.scalar_tensor_tensor(out=nbc[:, h, :], in0=nbc[:, h, :], scalar=dsc, in1=ps[:, bN, h * 128:(h + 1) * 128], op0=ALU.mult, op1=ALU.add)
            cx.act.activation(out=Cb.all(), in_=C.all(), func=AF.Copy)
            cx.act.activation(out=nbcb.all(), in_=nbc.all(), func=AF.Copy)
        for ob in range(2):
            w = self.weight(f"L{i}.ml_out{ob}")
            for m in range(4):
                mo = ob * 4 + m
                b = self.bank()
                for kc in range(KC):
                    cx.pe.matmul(ps[:, b, 0:TM], lhsT=V(w.ap[:, kc, m * 128:(m + 1) * 128], w.bufs), rhs=self.hn[:, kc, :], start=(kc == 0), stop=(kc == KC - 1))
                cx.dve.scalar_tensor_tensor(out=s_t[:, mo, :], in0=x_t[:, mo, :], scalar=float(ALPHA), in1=ps[:, b, 0:TM], op0=ALU.mult, op1=ALU.add)

    def pack_pv(self, inputs):
        pv = np.zeros((128, self.npv), np.float32)

        def put(name, vec):
            col = self.pv_names[name]
            n = vec.shape[0] // 128
            pv[:, col:col + n] = vec.reshape(n, 128).T
        for i in self.layers:
            for j in range(2):
                put(f"ln_g{i}.{j}", inputs["ln_g"][i, j])
                put(f"ln_b{i}.{j}", inputs["ln_b"][i, j])
            if i % 3 == 0 and self.mixers:
                put(f"ml_nw{i}", inputs["ml_norm_w"][i // 3])
            if i % 3 == 2 and self.mixers:
                j = i // 3
                put(f"rw{i}_mix", inputs["rw_mix"][j].reshape(-1))
                put(f"rw{i}_w0", inputs["rw_w0"][j])
                put(f"rw{i}_a0", inputs["rw_a0"][j])
                put(f"rw{i}_k_k", inputs["rw_k_k"][j])
                put(f"rw{i}_k_a", inputs["rw_k_a"][j])
                put(f"rw{i}_r_k", inputs["rw_r_k"][j].reshape(-1))
            if i % 3 == 1 and self.mixers:
                put(f"ssm_cb{i}", inputs["ssm_conv_b"][i // 3])
                put(f"ssm_nw{i}", inputs["ssm_norm_w"][i // 3])
        return pv

    def pack_bc(self, inputs):
        bc = np.zeros((128, self.nbc), np.float32)
        for i in self.layers:
            if i % 3 == 0 and self.mixers:
                col = self.bc_names[f"ml_bg{i}"]
                bc[:, col:col + 8] = inputs["ml_b_gate"][i // 3][None, :]
            if i % 3 == 2 and self.mixers:
                col = self.bc_names[f"rw{i}"]
                bc[:, col:col + 1024] = inputs["rw_lnx_w"][i // 3][None, :]
                bc[:, col + 1024:col + 2048] = inputs["rw_lnx_b"][i // 3][None, :]
            if i % 3 == 1 and self.mixers:
                col = self.bc_names[f"ssm{i}"]
                j = i // 3
                bc[:, col:col + 32] = inputs["ssm_a_log"][j][None, :]
                bc[:, col + 32:col + 64] = inputs["ssm_d"][j][None, :]
                bc[:, col + 64:col + 96] = inputs["ssm_dt_bias"][j][None, :]
        return bc


_orig_alloc = Prog.alloc


def _alloc2(self):
    _orig_alloc(self)
    c = self.c
    self.eps_ln = c.sbuf("eps_ln", [128, 1], F32)
    self.eps_rms = c.sbuf("eps_rms", [128, 1], F32)
    self.one_c = c.sbuf("one_c", [128, 1], F32)
    self.bc = c.sbuf("bc", [128, self.nbc], F32)
    self.tri = c.sbuf("tri", [128, 128], F32)
    self.ones_f = c.sbuf("ones_f", [128, 128], F32)
    self.ident_f = c.sbuf("ident_f", [128, 128], F32)
    self.mask4 = c.sbuf("mask4", [128, 512], F32)
    self.ones1_b = c.sbuf("ones1_b", [128, 128], BF16)
    self.ones256_b = c.sbuf("ones256_b", [128, 128], BF16)
    self.ident_b = c.sbuf("ident_b", [128, 128], BF16)
    self.maskX = c.sbuf("maskX", [128, 512], BF16)
    self.lowS = c.sbuf("lowS", [128, 128], BF16)
    self.blockones = c.sbuf("blockones", [128, 128], BF16)
    self.sel2 = c.sbuf("sel2", [128, 2], BF16)
    self.rmask = c.sbuf("rmask", [128, T], F32)
    self.eps_lnx = c.sbuf("eps_lnx", [128, 1], F32)
    self.rw_state = {}
    self.rw_omka = {}
    if self.mixers:
        for i in self.layers:
            if i % 3 == 2:
                self.rw_state[i] = (c.sbuf(f"rwS{i}", [128, 8, 64], F32), c.sbuf(f"rwSb{i}", [128, 8, 64], BF16), c.sbuf(f"rwX{i}", [128, 8, 1], F32))
                self.rw_omka[i] = c.sbuf(f"rwOmka{i}", [128, 8], F32)
    self.ssm_state = {}
    if self.mixers:
        for i in self.layers:
            if i % 3 == 1:
                self.ssm_state[i] = (c.sbuf(f"ssmS{i}", [128, 32, 64], F32), c.sbuf(f"ssmSb{i}", [128, 32, 64], BF16),
                                     c.sbuf(f"ssmH{i}", [128, 24, 4], BF16), c.sbuf(f"ssmA{i}", [128, 32], F32))
    if any(i % 3 == 0 for i in self.layers) and self.mixers:
        self.ml_state = {}
        for i in self.layers:
            if i % 3 == 0:
                self.ml_state[i] = (c.sbuf(f"mlC{i}", [128, 4, 256], F32, split=1), c.sbuf(f"mlCb{i}", [128, 4, 256], BF16),
                                    c.sbuf(f"mln{i}", [128, 4, 128], F32, split=1), c.sbuf(f"mlnb{i}", [128, 4, 128], BF16))


Prog.alloc = _alloc2
_orig_consts = Prog.consts


def _consts2(self):
    _orig_consts(self)
    c = self.c
    c.dve.memset(self.eps_ln.all(), LN_EPS)
    c.dve.memset(self.eps_rms.all(), RMS_EPS)
    c.dve.memset(self.one_c.all(), 1.0)
    c.dve.memset(self.ones_f.all(), 1.0)
    c.dve.memset(self.ones1_b.all(), 1.0)
    c.dve.memset(self.ones256_b.all(), 1.0 / 256.0)
    c.pool.memset(self.tri.all(), 1.0)
    c.pool.affine_select(out=self.tri.all(), in_=self.tri.all(), pattern=[[1, 128]], compare_op=ALU.is_ge, fill=0.0, base=0, channel_multiplier=-1)
    c.pool.memset(self.ident_f.all(), 1.0)
    c.pool.affine_select(out=self.ident_f.all(), in_=self.ident_f.all(), pattern=[[1, 128]], compare_op=ALU.is_equal, fill=0.0, base=0, channel_multiplier=-1)
    for h in range(4):
        c.pool.tensor_copy(out=self.mask4[:, h * 128:(h + 1) * 128], in_=self.tri.all())
    c.pool.tensor_copy(out=self.ident_b.all(), in_=self.ident_f.all())
    c.pool.memset(self.maskX.all(), 1.0)
    for q in range(4):
        c.pool.affine_select(out=self.maskX[:, q * 128:(q + 1) * 128], in_=self.maskX[:, q * 128:(q + 1) * 128], pattern=[[1, 128]],
                             compare_op=(ALU.is_ge if q % 2 else ALU.is_gt), fill=0.0, base=0, channel_multiplier=-1)
    c.pool.memset(self.lowS.all(), 1.0)
    c.pool.affine_select(out=self.lowS.all(), in_=self.lowS.all(), pattern=[[-1, 128]], compare_op=ALU.is_gt, fill=0.0, base=0, channel_multiplier=1)
    c.pool.memset(self.blockones.all(), 0.0)
    c.pool.memset(self.blockones[0:64, 0:64], 1.0)
    c.pool.memset(self.blockones[64:128, 64:128], 1.0)
    c.pool.memset(self.sel2.all(), 0.0)
    c.pool.memset(self.sel2[0:64, 0:1], 1.0)
    c.pool.memset(self.sel2[64:128, 1:2], 1.0)
    c.pool.memset(self.rmask.all(), 1.0)
    for q in range(T // 128):
        c.pool.memset(self.rmask[:, q * 128:q * 128 + 1], 0.0)
    c.pool.memset(self.eps_lnx.all(), 64e-5)
    for i, t_ in self.rw_omka.items():
        col = self.pv_names[f"rw{i}_k_a"]
        c.dve.tensor_scalar(out=t_.all(), in0=self.pv[:, col:col + 8], scalar1=-1.0, scalar2=1.0, op0=ALU.mult, op1=ALU.add)
    for i, st in self.ssm_state.items():
        bcol = self.bc_names[f"ssm{i}"]
        c.act.activation(out=st[3].all(), in_=self.bc[:, bcol:bcol + 32], func=AF.Exp)
        c.dve.tensor_scalar(out=st[3].all(), in0=st[3].all(), scalar1=-1.0, scalar2=None, op0=ALU.mult)


def _reset2(self):
    c = self.c
    if self.mixers:
        for i, st in getattr(self, "ml_state", {}).items():
            for t in st:
                c.pool.memset(t.all(), 0.0)
        for i, st in self.ssm_state.items():
            for t in st[:3]:
                c.pool.memset(t.all(), 0.0)
        for i, st in self.rw_state.items():
            for t in st:
                c.pool.memset(t.all(), 0.0)


Prog.reset_state = _reset2


Prog.consts = _consts2


def run(inputs, nseq_per_core, seqlen, layers, ncores, mixers=True):
    inputs = {k: np.asarray(v) for k, v in inputs.items()}
    prog = Prog(nseq_per_core, seqlen, layers, mixers)
    nc = prog.build()
    wf = prog.wp.pack(inputs)
    pv = prog.pack_pv(inputs)
    bcv = prog.pack_bc(inputs)
    x = inputs["x"]
    in_maps = []
    for cidx in range(ncores):
        xs = x[cidx * nseq_per_core:(cidx + 1) * nseq_per_core]
        in_maps.append({"xT": np.ascontiguousarray(xs.transpose(0, 2, 1)), "wf": wf, "pv": pv, "bc": bcv})
    res = run_bass_kernel_spmd(nc, in_maps, core_ids=list(range(ncores)))
    outs = [np.asarray(r["yT"]).transpose(0, 2, 1) for r in res.results]
    return np.ascontiguousarray(np.concatenate(outs, axis=0)).astype(np.float32)


def kernel(**inputs):
    return run(inputs, 2, 4096, list(range(DEPTH)), 8)
```

```python
import contextlib
import numpy as np
import concourse.bass as bass
import concourse.mybir as mybir
from concourse.bass_utils import run_bass_kernel_spmd

F32 = mybir.dt.float32
BF16 = mybir.dt.bfloat16
AF = mybir.ActivationFunctionType
ALU = mybir.AluOpType
AX = mybir.AxisListType

D = 1024
DEPTH = 4
FH = 2816
ALPHA = (2 * DEPTH) ** 0.25
LN_EPS = 1e-5
RMS_EPS = 1e-6
T = 512
KC = D // 128
EPOCH = 30000
WSC = float(np.exp(-0.5))
NOSAME = False


class Dom:
    def __init__(self, name, sems, unit, epoch):
        self.name, self.sems, self.unit, self.epoch = name, sems, unit, epoch
        self.count = 0

    def target(self, n):
        idx = (n - 1) // self.epoch
        return self.sems[idx], ((n - 1) % self.epoch + 1) * self.unit


class Eng(Dom):
    def __init__(self, name, be, sems, is_pe=False):
        super().__init__(name, sems, 1, EPOCH)
        self.be = be
        self.seen = {}
        self.is_pe = is_pe


class Buf:
    __slots__ = ("w", "r", "name")

    def __init__(self, name):
        self.w = None
        self.r = {}
        self.name = name


class V:
    __slots__ = ("ap", "bufs")

    def __init__(self, ap, bufs):
        self.ap = ap
        self.bufs = bufs


def _flat(lists):
    d = {}
    for l in lists:
        for b in l:
            d[id(b)] = b
    return list(d.values())


class Tile:
    def __init__(self, name, ap, split=None, bsplit=None):
        self.name = name
        self.ap = ap
        self.split = split
        n = ap.shape[split] if split is not None else 1
        self.bsplit = bsplit if bsplit is not None else [[Buf(f"{name}.{i}")] for i in range(n)]
        self.bufs = _flat(self.bsplit)

    def __getitem__(self, key):
        if not isinstance(key, tuple):
            key = (key,)
        bufs = self.bufs
        if self.split is not None and len(key) > self.split:
            k = key[self.split]
            if isinstance(k, int):
                bufs = self.bsplit[k]
            elif isinstance(k, slice):
                bufs = _flat(self.bsplit[k])
        return V(self.ap[key], bufs)

    def v(self, ap, bufs=None):
        return V(ap, self.bufs if bufs is None else bufs)

    def all(self):
        return V(self.ap, self.bufs)


class EP:
    def __init__(self, ctx, eng):
        self.ctx, self.eng = ctx, eng

    def __getattr__(self, name):
        meth = getattr(self.eng.be, name)
        ctx, eng = self.ctx, self.eng

        def call(*args, **kw):
            reads, writes = [], []

            def conv(k, v):
                if isinstance(v, V):
                    (writes if k in ("out", "accum_out") else reads).extend(v.bufs)
                    return v.ap
                return v
            args2 = [conv("out" if i == 0 else "in", a) for i, a in enumerate(args)]
            kw2 = {k: conv(k, v) for k, v in kw.items()}
            return ctx.issue(eng, lambda: meth(*args2, **kw2), reads, writes, dma=(name == "dma_start"))
        return call


class Ctx:
    def __init__(self, nc, es):
        self.nc, self.es = nc, es
        nsem_eng = {"pe": 5, "dve": 4, "act": 4, "pool": 3, "sp": 2}
        bes = {"pe": nc.tensor, "dve": nc.vector, "act": nc.scalar, "pool": nc.gpsimd, "sp": nc.sync}
        self.engs = {}
        for k, n in nsem_eng.items():
            sems = [es.enter_context(nc.semaphore(f"s_{k}{i}")) for i in range(n)]
            self.engs[k] = Eng(k, bes[k], sems, is_pe=(k == "pe"))
        self.pe, self.dve, self.act, self.pool, self.sp = (EP(self, self.engs[k]) for k in ("pe", "dve", "act", "pool", "sp"))
        self.dma_doms = [Dom(f"dma{i}", [es.enter_context(nc.semaphore(f"s_dma{i}"))], 16, 4000) for i in range(40)]
        self.dma_rr = 0
        self.ninst = 0
        self.nwait = 0
        self._rr = 0

    def sbuf(self, name, shape, dtype, split=None):
        t = self.es.enter_context(self.nc.sbuf_tensor("sb_" + name, list(shape), dtype))
        return Tile(name, t[:] if hasattr(t, "__getitem__") else t.ap(), split)

    def make_arena(self, nbytes, gran=1024):
        self.ar_tile = self.sbuf("arena", [128, nbytes // 4], F32)
        self.ar_gran = gran
        self.ar_bufs = [Buf(f"ar{i}") for i in range((nbytes + gran - 1) // gran)]
        self.ar_size = nbytes
        self.ar_off = 0
        self.ar_peak = 0

    def areset(self):
        self.ar_off = 0

    def carve(self, name, shape, dtype, split=None):
        esz = 4 if dtype == F32 else 2
        free = 1
        for d in shape[1:]:
            free *= d
        nbytes = free * esz
        off = (self.ar_off + 63) // 64 * 64
        assert off + nbytes <= self.ar_size, (name, off, nbytes, self.ar_size)
        self.ar_off = off + nbytes
        self.ar_peak = max(self.ar_peak, self.ar_off)
        ap = self.ar_tile.ap[:, off // 4:(off + nbytes) // 4]
        if dtype != F32:
            ap = ap.bitcast(dtype)
        if len(shape) == 3:
            ap = ap.rearrange("p (a b) -> p a b", a=shape[1])
        elif len(shape) == 4:
            ap = ap.rearrange("p (a b c) -> p a b c", a=shape[1], b=shape[2])
        g = self.ar_gran

        def regs(o0, o1):
            return self.ar_bufs[o0 // g:(o1 + g - 1) // g]
        if split is None:
            bs = [regs(off, off + nbytes)]
        else:
            assert split == 1
            per = nbytes // shape[1]
            bs = [regs(off + i * per, off + (i + 1) * per) for i in range(shape[1])]
        return Tile(name, ap, split, bs)

    def issue(self, eng, fn, reads, writes, dma=False):
        deps = {}

        def need(d, n):
            if deps.get(d, 0) < n:
                deps[d] = n
        for b in reads:
            if b.w:
                need(*b.w)
        for b in writes:
            if b.w:
                need(*b.w)
            for d, n in b.r.items():
                need(d, n)
        if dma:
            dom = self.dma_doms[self.dma_rr]
            self.dma_rr = (self.dma_rr + 1) % len(self.dma_doms)
            if dom.count:
                need(dom, dom.count)
        else:
            dom = eng
        for d, n in deps.items():
            if d is eng and (eng.is_pe or (NOSAME and eng.name in ("dve", "act"))):
                continue
            if eng.seen.get(d, 0) >= n:
                continue
            sem, val = d.target(n)
            eng.be.wait_ge(sem, val)
            eng.seen[d] = n
            self.nwait += 1
        inst = fn()
        dom.count += 1
        n = dom.count
        sem, _ = dom.target(n)
        inst.then_inc(sem, dom.unit)
        self.ninst += 1
        for b in reads:
            b.r[dom] = n
        for b in writes:
            b.w = (dom, n)
            b.r = {}
        return inst

    def any2(self):
        self._rr ^= 1
        return self.dve if self._rr else self.pool

    def finish(self, bufs):
        sp = self.engs["sp"]
        for b in bufs:
            if b.w:
                d, n = b.w
                if sp.seen.get(d, 0) < n:
                    sem, val = d.target(n)
                    sp.be.wait_ge(sem, val)
                    sp.seen[d] = n


class WPlan:
    def __init__(self):
        self.blocks = {}
        self.src = []
        self.tot = 0

    def add(self, name, key, idx, kdim, cols):
        kc = max(1, kdim // 128)
        nb = sum(c1 - c0 for c0, c1 in cols)
        self.blocks[name] = (self.tot, kc, nb)
        self.src.append((name, key, idx, kdim, cols))
        self.tot += kc * nb
        if self.tot % 2:
            self.tot += 1

    def add_custom(self, name, kc, nb, fn):
        self.blocks[name] = (self.tot, kc, nb)
        self.src.append((name, None, fn, None, None))
        self.tot += kc * nb
        if self.tot % 2:
            self.tot += 1

    def pack(self, inputs):
        out = np.zeros((128, self.tot), np.float32)
        for name, key, idx, kdim, cols in self.src:
            off, kc, nb = self.blocks[name]
            if key is None:
                out[:, off:off + kc * nb] = idx(inputs)
                continue
            w = inputs[key][idx]
            wc = np.concatenate([w[:, c0:c1] for c0, c1 in cols], axis=1)
            if kdim < 128:
                out[:kdim, off:off + nb] = wc
            else:
                out[:, off:off + kc * nb] = wc.reshape(kc, 128, nb).transpose(1, 0, 2).reshape(128, kc * nb)
        return out


def layer_kind(i):
    return i % 3


def make_plan(layers, mixers=True):
    wp = WPlan()
    for i in layers:
        kind, j = i % 3, i // 3
        if kind == 0 and mixers:
            for b in range(6):
                wp.add(f"L{i}.ml_in{b}", "ml_w_in", j, D, [(b * 512, (b + 1) * 512)])
            wp.add(f"L{i}.ml_g", "ml_w_in", j, D, [(3072, 3080)])
            for b in range(2):
                wp.add(f"L{i}.ml_out{b}", "ml_w_out", j, D, [(b * 512, (b + 1) * 512)])
        if kind == 1 and mixers:
            for b in range(4):
                wp.add(f"L{i}.ssm_z{b}", "ssm_w_in", j, D, [(b * 512, (b + 1) * 512)])
            for b in range(6):
                wp.add(f"L{i}.ssm_x{b}", "ssm_w_in", j, D, [(2048 + b * 512, 2048 + (b + 1) * 512)])
            wp.add(f"L{i}.ssm_dt", "ssm_w_in", j, D, [(5120, 5152)])

            def cbrow(inputs, j=j):
                a = np.zeros((128, 1024), np.float32)
                for r in range(3):
                    a[32 * r] = inputs["ssm_conv_b"][j][r * 1024:(r + 1) * 1024]
                return a
            wp.add_custom(f"L{i}.ssm_cb", 1, 1024, cbrow)
            for cc in range(24):
                def cw(inputs, j=j, cc=cc):
                    a = np.zeros((128, 4, 128), np.float32)
                    w = inputs["ssm_conv_w"][j]
                    for tap in range(4):
                        a[np.arange(128), tap, np.arange(128)] = w[tap, cc * 128:(cc + 1) * 128]
                    return a.reshape(128, 512)
                wp.add_custom(f"L{i}.ssm_cw{cc}", 1, 512, cw)
            for b in range(4):
                wp.add(f"L{i}.ssm_out{b}", "ssm_w_out", j, 2048, [(b * 256, (b + 1) * 256)])
        if kind == 2 and mixers:
            wp.add(f"L{i}.rw_w1", "rw_w1", j, D, [(0, 64)])
            wp.add(f"L{i}.rw_w2", "rw_w2", j, 64, [(0, 1024)])
            for b in range(2):
                wp.add(f"L{i}.rw_r{b}", "rw_w_rkv", (j, 0), D, [(b * 512, (b + 1) * 512)])
            wp.add(f"L{i}.rw_a1", "rw_a1", j, D, [(0, 64)])
            wp.add(f"L{i}.rw_a2", "rw_a2", j, 64, [(0, 1024)])
            for b in range(2):
                wp.add(f"L{i}.rw_k{b}", "rw_w_rkv", (j, 1), D, [(b * 512, (b + 1) * 512)])
            for b in range(2):
                wp.add(f"L{i}.rw_v{b}", "rw_w_rkv", (j, 2), D, [(b * 512, (b + 1) * 512)])
            wp.add(f"L{i}.rw_g1", "rw_g1", j, D, [(0, 128)])
            wp.add(f"L{i}.rw_g2", "rw_g2", j, 128, [(0, 1024)])
            for b in range(2):
                wp.add(f"L{i}.rw_o{b}", "rw_w_out", j, D, [(b * 512, (b + 1) * 512)])
        for b in range(11):
            wp.add(f"L{i}.f_in{b}", "ffn_w_in", i, D, [(b * 256, (b + 1) * 256), (FH + b * 256, FH + (b + 1) * 256)])
        for b in range(8):
            wp.add(f"L{i}.f_out{b}", "ffn_w_out", i, FH, [(b * 128, (b + 1) * 128)])
    return wp


def layer_block_seq(i, mixers=True, nsub=2):
    seq = []
    for _ in range(nsub):
        seq += mixer_block_seq(i, mixers)
    seq += [f"L{i}.f_in{b}" for b in range(11)] + [f"L{i}.f_out{b}" for b in range(8)]
    return seq


def mixer_block_seq(i, mixers=True):
    kind = i % 3
    seq = []
    if kind == 0 and mixers:
        seq += [f"L{i}.ml_g"] + [f"L{i}.ml_in{b}" for b in range(6)] + [f"L{i}.ml_out{b}" for b in range(2)]
    if kind == 1 and mixers:
        seq += [f"L{i}.ssm_dt"] + [f"L{i}.ssm_z{b}" for b in range(4)] + [f"L{i}.ssm_x{b}" for b in range(6)] + [f"L{i}.ssm_cb"]
        seq += [f"L{i}.ssm_cw{cc}" for cc in range(24)] + [f"L{i}.ssm_out{b}" for b in range(4)]
    if kind == 2 and mixers:
        seq += [f"L{i}.rw_w1", f"L{i}.rw_w2", f"L{i}.rw_r0", f"L{i}.rw_r1", f"L{i}.rw_a1", f"L{i}.rw_a2", f"L{i}.rw_k0", f"L{i}.rw_k1",
                f"L{i}.rw_v0", f"L{i}.rw_v1", f"L{i}.rw_g1", f"L{i}.rw_g2", f"L{i}.rw_o0", f"L{i}.rw_o1"]
    return seq


SLOT = 4096
NSLOT = 4
ARENA = 85504


class Prog:
    def __init__(self, nseq, seqlen, layers, mixers=True):
        self.nseq, self.seqlen, self.layers, self.mixers = nseq, seqlen, layers, mixers
        self.ntile = seqlen // T
        self.TM = 256
        self.wp = make_plan(layers, mixers)
        self.pv_names = {}
        self.npv = 0

    def pv_add(self, name, n):
        self.pv_names[name] = self.npv
        self.npv += n

    def build(self):
        nc = bass.Bass("TRN2", target_bir_lowering=False)
        self.nc = nc
        nseq, seqlen = self.nseq, self.seqlen
        for i in self.layers:
            for j in range(2):
                self.pv_add(f"ln_g{i}.{j}", KC)
                self.pv_add(f"ln_b{i}.{j}", KC)
        self.bc_names = {}
        self.nbc = 0
        for i in self.layers:
            if i % 3 == 0 and self.mixers:
                self.pv_add(f"ml_nw{i}", KC)
                self.bc_names[f"ml_bg{i}"] = self.nbc
                self.nbc += 8
            if i % 3 == 1 and self.mixers:
                self.pv_add(f"ssm_cb{i}", 24)
                self.pv_add(f"ssm_nw{i}", 16)
                self.bc_names[f"ssm{i}"] = self.nbc
                self.nbc += 96
            if i % 3 == 2 and self.mixers:
                for nm, n in (("mix", 48), ("w0", 8), ("a0", 8), ("k_k", 8), ("k_a", 8), ("r_k", 8)):
                    self.pv_add(f"rw{i}_{nm}", n)
                self.bc_names[f"rw{i}"] = self.nbc
                self.nbc += 2048
        self.nbc = max(self.nbc, 8)
        bc_d = nc.dram_tensor("bc", [128, self.nbc], F32, kind="ExternalInput").ap()
        xT_d = nc.dram_tensor("xT", [nseq, D, seqlen], F32, kind="ExternalInput").ap()
        wf_d = nc.dram_tensor("wf", [128, self.wp.tot], F32, kind="ExternalInput").ap()
        pv_d = nc.dram_tensor("pv", [128, self.npv], F32, kind="ExternalInput").ap()
        yT_d = nc.dram_tensor("yT", [nseq, D, seqlen], F32, kind="ExternalOutput").ap()
        wb_d = nc.dram_tensor("wb", [128, self.wp.tot], BF16, kind="Internal").ap()
        with contextlib.ExitStack() as es:
            c = Ctx(nc, es)
            self.c = c
            self.xT_t = Tile("xT", xT_d)
            self.yT_t = Tile("yT", yT_d)
            self.wf_t = Tile("wf", wf_d)
            self.pv_dt = Tile("pvd", pv_d)
            self.wb_t = Tile("wb", wb_d)
            self.bc_dt = Tile("bcd", bc_d)
            self.alloc()
            self.prepass()
            self.consts()
            self.useq = []
            for s in range(nseq):
                for ti in range(self.ntile):
                    for i in self.layers:
                        self.useq += layer_block_seq(i, self.mixers)
            self.upos = 0
            self.uissued = 0
            for s in range(nseq):
                self.reset_state()
                for ti in range(self.ntile):
                    self.tile(s, ti)
            assert self.upos == len(self.useq)
            c.finish(self.yT_t.bufs)
            print(f"[build] instructions={c.ninst} waits={c.nwait} arena_peak={c.ar_peak}")
        return nc

    def alloc(self):
        c = self.c
        ps = self.c.es.enter_context(self.nc.psum_tensor("psum_all", [128, 8, 512], F32))
        self.ps = Tile("ps", ps[:] if hasattr(ps, "__getitem__") else ps.ap(), split=1)
        self.bank_rr = 0
        self.x = c.sbuf("x", [128, KC, T], F32, split=1)
        self.xb = c.sbuf("xb", [128, KC, T], BF16, split=1)
        self.s = c.sbuf("s", [128, KC, T], F32, split=1)
        self.wslot = [c.sbuf(f"wslot{i}", [128, SLOT], BF16) for i in range(NSLOT)]
        self.pv = c.sbuf("pv", [128, self.npv], F32)
        self.ones_b = c.sbuf("ones_b", [128, 128], BF16)
        c.make_arena(ARENA)

    def bank(self):
        b = self.bank_rr
        self.bank_rr = (self.bank_rr + 1) % 8
        return b

    def prepass(self):
        c = self.c
        tot = self.wp.tot
        i = 0
        off = 0
        engs = [c.dve, c.act]
        c.areset()
        stg_f = [c.carve(f"stgf{q}", [128, SLOT], F32) for q in range(2)]
        stg_b = [c.carve(f"stgb{q}", [128, SLOT], BF16) for q in range(2)]
        while off < tot:
            sz = min(SLOT, tot - off)
            f, b = stg_f[i % 2], stg_b[i % 2]
            c.sp.dma_start(out=f[:, 0:sz], in_=self.wf_t[:, off:off + sz])
            e = engs[i % 2]
            if e is c.act:
                e.activation(out=b[:, 0:sz], in_=f[:, 0:sz], func=AF.Copy)
            else:
                e.tensor_copy(out=b[:, 0:sz], in_=f[:, 0:sz])
            c.sp.dma_start(out=self.wb_t[:, off:off + sz], in_=b[:, 0:sz])
            off += sz
            i += 1
        c.sp.dma_start(out=self.pv.all(), in_=self.pv_dt.all())
        c.sp.dma_start(out=self.bc.all(), in_=self.bc_dt.all())

    def consts(self):
        c = self.c
        c.dve.memset(self.ones_b.all(), 1.0 / D)

    def reset_state(self):
        pass

    def _issue_load(self, u):
        name = self.useq[u]
        off, kc, nb = self.wp.blocks[name]
        slot = self.wslot[u % NSLOT]
        self.c.sp.dma_start(out=slot[:, 0:kc * nb], in_=self.wb_t[:, off:off + kc * nb])

    def weight(self, name):
        assert self.useq[self.upos] == name, (self.useq[self.upos], name)
        while self.uissued < min(len(self.useq), self.upos + NSLOT):
            self._issue_load(self.uissued)
            self.uissued += 1
        off, kc, nb = self.wp.blocks[name]
        slot = self.wslot[self.upos % NSLOT]
        self.upos += 1
        ap = slot.ap[:, 0:kc * nb].rearrange("p (k n) -> p k n", k=kc)
        return V(ap, slot.bufs)

    def tile(self, s, ti):
        c = self.c
        t0 = ti * T
        src = self.xT_t.ap[s].rearrange("(k p) t -> p k t", p=128)[:, :, t0:t0 + T]
        c.sp.dma_start(out=self.x.all(), in_=self.xT_t.v(src))
        for kc in range(KC):
            if kc % 2:
                c.act.activation(out=self.xb[:, kc, :], in_=self.x[:, kc, :], func=AF.Copy)
            else:
                c.dve.tensor_copy(out=self.xb[:, kc, :], in_=self.x[:, kc, :])
        for i in self.layers:
            if self.mixers:
                self.mixer(i, s, ti)
                self.layernorm(i, 0)
            self.ffn(i)
            self.layernorm(i, 1)
        dst = self.yT_t.ap[s].rearrange("(k p) t -> p k t", p=128)[:, :, t0:t0 + T]
        c.sp.dma_start(out=self.yT_t.v(dst), in_=self.x.all())

    def pvv(self, name, k):
        col = self.pv_names[name] + k
        return self.pv[:, col:col + 1]

    def layernorm(self, i, j):
        cx = self.c
        cx.areset()
        self.sqb = cx.carve("sqb", [128, KC, T], BF16, split=1)
        self.sb = cx.carve("sb", [128, KC, T], BF16, split=1)
        self.stat = cx.carve("stat", [128, 4, T], F32, split=1)
        for kc in range(KC):
            cx.act.activation(out=self.sqb[:, kc, :], in_=self.s[:, kc, :], func=AF.Square)
            cx.act.activation(out=self.sb[:, kc, :], in_=self.s[:, kc, :], func=AF.Copy)
        b1, b2 = self.bank(), self.bank()
        for kc in range(KC):
            cx.pe.matmul(self.ps[:, b1, :], lhsT=self.ones_b.all(), rhs=self.sb[:, kc, :], start=(kc == 0), stop=(kc == KC - 1))
        for kc in range(KC):
            cx.pe.matmul(self.ps[:, b2, :], lhsT=self.ones_b.all(), rhs=self.sqb[:, kc, :], start=(kc == 0), stop=(kc == KC - 1))
        mean, var, rstd, tmp = (self.stat[:, q, :] for q in range(4))
        cx.dve.tensor_copy(out=mean, in_=self.ps[:, b1, :])
        cx.dve.tensor_tensor(out=tmp, in0=mean, in1=mean, op=ALU.mult)
        cx.dve.tensor_tensor(out=var, in0=self.ps[:, b2, :], in1=tmp, op=ALU.subtract)
        cx.act.activation(out=rstd, in_=var, func=AF.Sqrt, bias=self.eps_ln[:, 0:1], scale=1.0)
        cx.dve.reciprocal(out=rstd, in_=rstd)
        for kc in range(KC):
            cx.dve.tensor_tensor(out=self.s[:, kc, :], in0=self.s[:, kc, :], in1=mean, op=ALU.subtract)
            cx.dve.tensor_tensor(out=self.s[:, kc, :], in0=self.s[:, kc, :], in1=rstd, op=ALU.mult)
            cx.act.activation(out=self.x[:, kc, :], in_=self.s[:, kc, :], func=AF.Identity,
                              bias=self.pvv(f"ln_b{i}.{j}", kc), scale=self.pvv(f"ln_g{i}.{j}", kc))
            cx.act.activation(out=self.xb[:, kc, :], in_=self.s[:, kc, :], func=AF.Identity,
                              bias=self.pvv(f"ln_b{i}.{j}", kc), scale=self.pvv(f"ln_g{i}.{j}", kc))

    def ffn(self, i):
        cx = self.c
        nfc = FH // 128
        cx.areset()
        self.hm = cx.carve("hm", [128, nfc, T], BF16, split=1)
        self.gsil = [cx.carve(f"gsil{q}", [128, T], F32) for q in range(2)]
        for b in range(11):
            w = self.weight(f"L{i}.f_in{b}")
            for h in range(2):
                m = b * 2 + h
                bg, bu = self.bank(), self.bank()
                for kc in range(KC):
                    cx.pe.matmul(self.ps[:, bg, :], lhsT=V(w.ap[:, kc, h * 128:(h + 1) * 128], w.bufs), rhs=self.xb[:, kc, :],
                                 start=(kc == 0), stop=(kc == KC - 1))
                for kc in range(KC):
                    cx.pe.matmul(self.ps[:, bu, :], lhsT=V(w.ap[:, kc, 256 + h * 128:256 + (h + 1) * 128], w.bufs), rhs=self.xb[:, kc, :],
                                 start=(kc == 0), stop=(kc == KC - 1))
                g = self.gsil[m % 2]
                cx.act.activation(out=g.all(), in_=self.ps[:, bg, :], func=AF.Silu)
                cx.dve.tensor_tensor(out=self.hm[:, m, :], in0=self.ps[:, bu, :], in1=g.all(), op=ALU.mult)
        for mo in range(KC):
            w = self.weight(f"L{i}.f_out{mo}")
            bo = self.bank()
            for m in range(nfc):
                cx.pe.matmul(self.ps[:, bo, :], lhsT=V(w.ap[:, m, :], w.bufs), rhs=self.hm[:, m, :], start=(m == 0), stop=(m == nfc - 1))
            cx.dve.scalar_tensor_tensor(out=self.s[:, mo, :], in0=self.x[:, mo, :], scalar=float(ALPHA), in1=self.ps[:, bo, :],
                                        op0=ALU.mult, op1=ALU.add)

    def mixer(self, i, s, ti):
        kind = i % 3
        for sub in range(T // self.TM):
            t0 = sub * self.TM
            self.xbv = Tile("xbv", self.xb.ap[:, :, t0:t0 + self.TM], 1, self.xb.bsplit)
            self.xv = Tile("xv", self.x.ap[:, :, t0:t0 + self.TM], 1, self.x.bsplit)
            self.sv = Tile("sv", self.s.ap[:, :, t0:t0 + self.TM], 1, self.s.bsplit)
            if kind == 0:
                self.mlstm(i)
            elif kind == 1:
                self.mamba(i)
            else:
                self.rwkv(i)

    def rwkv(self, i):
        TM = self.TM
        xb_t, x_t, s_t = self.xbv, self.xv, self.sv
        c = self.c
        ps = self.ps
        NCH = TM // 128
        ST, STb, xlast = self.rw_state[i]
        bcol = self.bc_names[f"rw{i}"]
        c.areset()
        AR = c.carve("AR", [128, 8, NCH, 256], BF16, split=1)
        BtT = c.carve("BtT", [128, 8, TM], BF16, split=1)
        KtT = c.carve("KtT", [128, 8, TM], BF16, split=1)
        rk_tok = c.carve("rk_tok", [128, NCH, 16], F32)
        WL = c.carve("WL", [128, NCH, 8], F32)
        offM = c.ar_off
        cw = c.carve("cw", [128, 8, TM], F32, split=1)
        asg = c.carve("asg", [128, 8, TM], BF16, split=1)
        xx = c.carve("xx", [128, 8, TM], BF16, split=1)
        offP = offM + 5 * (8 * TM * 2)
        assert c.ar_off <= offP
        c.ar_off = offP
        xj = c.carve("xj", [128, 8, TM], BF16, split=1)
        lo = [c.carve(f"lo{q}", [128, TM], BF16) for q in range(2)]
        tf = [c.carve(f"tf{q}", [128, TM], F32) for q in range(5)]
        sqk = c.carve("sqk", [128, TM], BF16)
        pr = c.carve("pr", [128, TM], BF16)
        pname = lambda nm, k: self.pvv(f"rw{i}_{nm}", k)

        def mix(j):
            for kc in range(KC):
                c.dve.scalar_tensor_tensor(out=xj[:, kc, :], in0=xx[:, kc, :], scalar=pname("mix", j * 8 + kc), in1=x_t[:, kc, :], op0=ALU.mult, op1=ALU.add)

        v4 = lambda t_: V(t_.ap.rearrange("p (c t) -> p c t", c=NCH), t_.bufs)
        for kc in range(KC):
            c.dve.tensor_tensor(out=xx[:, kc, 1:TM], in0=x_t[:, kc, 0:TM - 1], in1=x_t[:, kc, 1:TM], op=ALU.subtract)
            c.dve.tensor_tensor(out=xx[:, kc, 0:1], in0=xlast[:, kc, :], in1=x_t[:, kc, 0:1], op=ALU.subtract)
        c.pool.tensor_copy(out=xlast.all(), in_=V(x_t.ap[:, :, TM - 1:TM], x_t.bufs))
        mix(1)
        w = self.weight(f"L{i}.rw_w1")
        b = self.bank()
        for kc in range(KC):
            c.pe.matmul(ps[0:64, b, 0:TM], lhsT=V(w.ap[:, kc, :], w.bufs), rhs=xj[:, kc, :], start=(kc == 0), stop=(kc == KC - 1))
        c.act.activation(out=lo[0][0:64, :], in_=ps[0:64, b, 0:TM], func=AF.Tanh)
        w = self.weight(f"L{i}.rw_w2")
        for m in range(8):
            b = self.bank()
            c.pe.matmul(ps[:, b, 0:TM], lhsT=V(w.ap[0:64, 0, m * 128:(m + 1) * 128], w.bufs), rhs=lo[0][0:64, :], start=True, stop=True)
            c.act.activation(out=tf[m % 2].all(), in_=ps[:, b, 0:TM], func=AF.Sigmoid, bias=pname("w0", m), scale=1.0)
            c.dve.tensor_tensor_scan(out=cw[:, m, :], data0=self.rmask[:, 0:TM], data1=tf[m % 2].all(), initial=0.0, op0=ALU.mult, op1=ALU.add)
            c.act.activation(out=V(WL.ap[:, :, m], WL.bufs), in_=V(cw.ap[:, m, 127::128], cw.bsplit[m]), func=AF.Exp, scale=-WSC)
        mix(0)
        for rb in range(2):
            w = self.weight(f"L{i}.rw_r{rb}")
            for m in range(4):
                ko = rb * 4 + m
                b = self.bank()
                for kc in range(KC):
                    c.pe.matmul(ps[:, b, 0:TM], lhsT=V(w.ap[:, kc, m * 128:(m + 1) * 128], w.bufs), rhs=xj[:, kc, :], start=(kc == 0), stop=(kc == KC - 1))
                ex = tf[2 + ko % 2]
                c.act.activation(out=ex.all(), in_=cw[:, ko, :], func=AF.Exp, scale=-WSC)
                c.dve.tensor_tensor(out=V(AR.ap[:, ko, :, 128:256], AR.bsplit[ko]), in0=V(ps.ap[:, b, 0:TM].rearrange("p (c t) -> p c t", c=NCH), ps.bsplit[b]), in1=v4(ex), op=ALU.mult)
        mix(4)
        w = self.weight(f"L{i}.rw_a1")
        b = self.bank()
        for kc in range(KC):
            c.pe.matmul(ps[0:64, b, 0:TM], lhsT=V(w.ap[:, kc, :], w.bufs), rhs=xj[:, kc, :], start=(kc == 0), stop=(kc == KC - 1))
        c.act.activation(out=lo[1][0:64, :], in_=ps[0:64, b, 0:TM], func=AF.Copy)
        w = self.weight(f"L{i}.rw_a2")
        for m in range(8):
            b = self.bank()
            c.pe.matmul(ps[:, b, 0:TM], lhsT=V(w.ap[0:64, 0, m * 128:(m + 1) * 128], w.bufs), rhs=lo[1][0:64, :], start=True, stop=True)
            c.act.activation(out=asg[:, m, :], in_=ps[:, b, 0:TM], func=AF.Sigmoid, bias=pname("a0", m), scale=1.0)
        mix(2)
        for kb in range(2):
            w = self.weight(f"L{i}.rw_k{kb}")
            for m in range(4):
                ko = kb * 4 + m
                b = self.bank()
                for kc in range(KC):
                    c.pe.matmul(ps[:, b, 0:TM], lhsT=V(w.ap[:, kc, m * 128:(m + 1) * 128], w.bufs), rhs=xj[:, kc, :], start=(kc == 0), stop=(kc == KC - 1))
                kkr, ex, em, t1, kkn = tf
                c.act.activation(out=kkr.all(), in_=ps[:, b, 0:TM], func=AF.Copy, scale=pname("k_k", ko))
                c.act.activation(out=sqk.all(), in_=kkr.all(), func=AF.Square)
                b2 = self.bank()
                c.pe.matmul(ps[:, b2, 0:TM], lhsT=self.blockones.all(), rhs=sqk.all(), start=True, stop=True)
                c.dve.tensor_scalar(out=kkn.all(), in0=ps[:, b2, 0:TM], scalar1=1e-24, scalar2=None, op0=ALU.max)
                c.act.activation(out=kkn.all(), in_=kkn.all(), func=AF.Sqrt)
                c.dve.reciprocal(out=kkn.all(), in_=kkn.all())
                c.dve.tensor_tensor(out=kkn.all(), in0=kkn.all(), in1=kkr.all(), op=ALU.mult)
                c.act.activation(out=ex.all(), in_=cw[:, ko, :], func=AF.Exp, scale=-WSC)
                c.act.activation(out=em.all(), in_=cw[:, ko, :], func=AF.Exp, scale=WSC)
                kk4, ex4 = v4(kkn), v4(ex)
                c.dve.scalar_tensor_tensor(out=V(AR.ap[:, ko, :, 1:128], AR.bsplit[ko]), in0=V(kk4.ap[:, :, 1:128], kkn.bufs), scalar=-1.0, in1=V(ex4.ap[:, :, 0:127], ex.bufs),
                                           op0=ALU.mult, op1=ALU.mult)
                c.pool.tensor_scalar(out=V(AR.ap[:, ko, :, 0:1], AR.bsplit[ko]), in0=V(kk4.ap[:, :, 0:1], kkn.bufs), scalar1=-1.0, scalar2=None, op0=ALU.mult)
                c.dve.tensor_tensor(out=kkn.all(), in0=kkn.all(), in1=asg[:, ko, :], op=ALU.mult)
                c.dve.tensor_tensor(out=BtT[:, ko, :], in0=kkn.all(), in1=em.all(), op=ALU.mult)
                c.dve.tensor_scalar(out=t1.all(), in0=asg[:, ko, :], scalar1=pname("k_a", ko), scalar2=self.rw_omka[i][:, ko:ko + 1], op0=ALU.mult, op1=ALU.add)
                c.dve.tensor_tensor(out=t1.all(), in0=ps[:, b, 0:TM], in1=t1.all(), op=ALU.mult)
                c.dve.tensor_tensor(out=KtT[:, ko, :], in0=t1.all(), in1=em.all(), op=ALU.mult)
                c.dve.scalar_tensor_tensor(out=v4(pr), in0=V(AR.ap[:, ko, :, 128:256], AR.bsplit[ko]), scalar=pname("r_k", ko), in1=v4(V(KtT.ap[:, ko, :], KtT.bsplit[ko])),
                                           op0=ALU.mult, op1=ALU.mult)
                brk = self.bank()
                for ch in range(NCH):
                    c.pe.matmul(ps[:, brk, ch * 2:ch * 2 + 2], lhsT=pr[:, ch * 128:(ch + 1) * 128], rhs=self.sel2.all(), start=True, stop=True)
                c.dve.tensor_copy(out=V(rk_tok.ap[:, :, ko * 2:ko * 2 + 2], rk_tok.bufs), in_=V(ps.ap[:, brk, 0:NCH * 2].rearrange("p (c h) -> p c h", c=NCH), ps.bsplit[brk]))
        c.ar_off = offM
        Vt = c.carve("Vt", [128, NCH, 1024], BF16, split=1)
        gT = c.carve("gT", [128, 8, TM], BF16, split=1)
        Btok = c.carve("Btok", [128, NCH, 1024], BF16, split=1)
        Ktok = c.carve("Ktok", [128, NCH, 1024], BF16, split=1)
        yg = c.carve("yg", [128, 8, TM], BF16, split=1)
        assert c.ar_off <= offP
        mix(3)
        for vb in range(2):
            w = self.weight(f"L{i}.rw_v{vb}")
            for ch in range(NCH):
                b = self.bank()
                for kc in range(KC):
                    c.pe.matmul(ps[:, b, :], lhsT=xj[:, kc, ch * 128:(ch + 1) * 128], rhs=V(w.ap[:, kc, :], w.bufs), start=(kc == 0), stop=(kc == KC - 1))
                if (ch + vb) % 2:
                    c.act.activation(out=Vt[:, ch, vb * 512:(vb + 1) * 512], in_=ps[:, b, :], func=AF.Copy)
                else:
                    c.dve.tensor_copy(out=Vt[:, ch, vb * 512:(vb + 1) * 512], in_=ps[:, b, :])
        mix(5)
        w = self.weight(f"L{i}.rw_g1")
        b = self.bank()
        for kc in range(KC):
            c.pe.matmul(ps[:, b, 0:TM], lhsT=V(w.ap[:, kc, :], w.bufs), rhs=xj[:, kc, :], start=(kc == 0), stop=(kc == KC - 1))
        c.act.activation(out=lo[0].all(), in_=ps[:, b, 0:TM], func=AF.Sigmoid)
        w = self.weight(f"L{i}.rw_g2")
        for m in range(8):
            b = self.bank()
            c.pe.matmul(ps[:, b, 0:TM], lhsT=V(w.ap[:, 0, m * 128:(m + 1) * 128], w.bufs), rhs=lo[0].all(), start=True, stop=True)
            if m % 2:
                c.act.activation(out=gT[:, m, :], in_=ps[:, b, 0:TM], func=AF.Copy)
            else:
                c.dve.tensor_copy(out=gT[:, m, :], in_=ps[:, b, 0:TM])
        for src, dst in ((BtT, Btok), (KtT, Ktok)):
            for ch in range(NCH):
                for half in range(2):
                    bT = self.bank()
                    pb = ps.ap[:, bT, :].bitcast(BF16)
                    for r in range(4):
                        kc = half * 4 + r
                        c.pe.transpose(V(pb[:, r * 128:(r + 1) * 128], ps.bsplit[bT]), src[:, kc, ch * 128:(ch + 1) * 128], self.ident_b.all())
                    e = c.act if half else c.dve
                    if half:
                        c.act.activation(out=dst[:, ch, half * 512:(half + 1) * 512], in_=V(pb[:, 0:512], ps.bsplit[bT]), func=AF.Copy)
                    else:
                        c.dve.tensor_copy(out=dst[:, ch, half * 512:(half + 1) * 512], in_=V(pb[:, 0:512], ps.bsplit[bT]))
        c.ar_off = offP
        SETS = []
        for q_ in range(2):
            SETS.append(dict(XT=c.carve(f"XT{q_}", [128, 8, 4, 128], BF16),
                             Xp=[c.carve(f"Xp{q_}{q}", [128, 8, 128], BF16) for q in range(2)],
                             XTp=[c.carve(f"XTp{q_}{q}", [128, 8, 128], BF16) for q in range(2)],
                             TTp=[c.carve(f"TTp{q_}{q}", [128, 8, 128], BF16) for q in range(2)]))
        RHSb = c.carve("RHSb", [128, 8, 64], BF16)
        Ub = c.carve("Ub", [128, 8, 64], BF16)
        Ysb = c.carve("Ysb", [128, 8, 64], F32)
        Ysq = c.carve("Ysq", [128, 8, 64], F32)
        yv = c.carve("yv", [128, 8, 64], BF16)
        st8 = {k: c.carve("st8_" + k, [128, 8], F32) for k in ("s1", "s2", "mean", "var", "rstd")}

        def hkf(h0):
            return lambda hh: ((h0 + hh) // 2, ((h0 + hh) % 2) * 64)

        def chain(ch, half, S):
            cs = slice(ch * 128, (ch + 1) * 128)
            h0 = half * 8
            hk = hkf(h0)
            XT, Xp, XTp, TTp = S["XT"], S["Xp"], S["XTp"], S["TTp"]
            for hh in range(8):
                kc, ba = hk(hh)
                bX = self.bank()
                arR = V(AR.ap[ba:ba + 64, kc, ch, :], AR.bsplit[kc])
                c.pe.matmul(ps[:, bX, 0:256], lhsT=V(BtT.ap[ba:ba + 64, kc, cs], BtT.bsplit[kc]), rhs=arR, start=True, stop=True)
                c.pe.matmul(ps[:, bX, 256:512], lhsT=V(KtT.ap[ba:ba + 64, kc, cs], KtT.bsplit[kc]), rhs=arR, start=True, stop=True)
                c.dve.tensor_tensor(out=V(XT.ap[:, hh, :, :].rearrange("p a t -> p (a t)"), XT.bufs), in0=ps[:, bX, :], in1=self.maskX.all(), op=ALU.mult)
                if hh % 2:
                    yield
            for q in range(2):
                bM = self.bank()
                for r in range(4):
                    hh = r * 2 + q
                    kc, ba = hk(hh)
                    c.pe.matmul(ps[:, bM, r * 128:(r + 1) * 128], lhsT=V(AR.ap[ba:ba + 64, kc, ch, 0:128], AR.bsplit[kc]), rhs=V(BtT.ap[ba:ba + 64, kc, cs], BtT.bsplit[kc]), start=True, stop=True)
                c.dve.tensor_tensor(out=V(Xp[0].ap[:, q::2, :], Xp[0].bufs), in0=V(ps.ap[:, bM, :].rearrange("p (h t) -> p h t", h=4), ps.bsplit[bM]),
                                    in1=V(self.lowS.ap.unsqueeze(1).to_broadcast([128, 4, 128]), self.lowS.bufs), op=ALU.mult)
            c.dve.tensor_tensor(out=TTp[0].all(), in0=V(XT.ap[:, :, 0, :], XT.bufs), in1=V(self.ident_b.ap.unsqueeze(1).to_broadcast([128, 8, 128]), self.ident_b.bufs), op=ALU.add)
            yield
            xcur, xtcur, ttcur = Xp[0], None, TTp[0]
            for lvl in range(1, 7):
                xn, xtn, ttn = Xp[lvl % 2], XTp[lvl % 2], TTp[lvl % 2]

                def xt_of(hh, xtcur=xtcur):
                    if xtcur is None:
                        return V(XT.ap[:, hh, 0, :], XT.bufs)
                    return V(xtcur.ap[:, hh, :], xtcur.bufs)
                bXs = [self.bank(), self.bank()]
                for hh in range(8):
                    c.pe.matmul(ps[:, bXs[hh // 4], (hh % 4) * 128:(hh % 4 + 1) * 128], lhsT=xt_of(hh), rhs=V(xcur.ap[:, hh, :], xcur.bufs), start=True, stop=True)
                if lvl < 6:
                    bTs = [self.bank(), self.bank()]
                    for hh in range(8):
                        c.pe.matmul(ps[:, bTs[hh // 4], (hh % 4) * 128:(hh % 4 + 1) * 128], lhsT=V(xcur.ap[:, hh, :], xcur.bufs), rhs=xt_of(hh), start=True, stop=True)
                for q in range(2):
                    src_ = V(ps.ap[:, bXs[q], :].rearrange("p (h t) -> p h t", h=4), ps.bsplit[bXs[q]])
                    if q:
                        c.act.activation(out=V(xn.ap[:, q * 4:(q + 1) * 4, :], xn.bufs), in_=src_, func=AF.Copy)
                    else:
                        c.dve.tensor_copy(out=V(xn.ap[:, q * 4:(q + 1) * 4, :], xn.bufs), in_=src_)
                if lvl < 6:
                    for q in range(2):
                        src_ = V(ps.ap[:, bTs[q], :].rearrange("p (h t) -> p h t", h=4), ps.bsplit[bTs[q]])
                        if q:
                            c.dve.tensor_copy(out=V(xtn.ap[:, q * 4:(q + 1) * 4, :], xtn.bufs), in_=src_)
                        else:
                            c.act.activation(out=V(xtn.ap[:, q * 4:(q + 1) * 4, :], xtn.bufs), in_=src_, func=AF.Copy)
                yield
                bAs = [self.bank(), self.bank()]
                for hh in range(8):
                    c.pe.matmul(ps[:, bAs[hh // 4], (hh % 4) * 128:(hh % 4 + 1) * 128], lhsT=V(xn.ap[:, hh, :], xn.bufs), rhs=V(ttcur.ap[:, hh, :], ttcur.bufs), start=True, stop=True)
                for q in range(2):
                    c.dve.tensor_tensor(out=V(ttn.ap[:, q * 4:(q + 1) * 4, :], ttn.bufs), in0=V(ps.ap[:, bAs[q], :].rearrange("p (h t) -> p h t", h=4), ps.bsplit[bAs[q]]),
                                        in1=V(ttcur.ap[:, q * 4:(q + 1) * 4, :], ttcur.bufs), op=ALU.add)
                xcur, ttcur, xtcur = xn, ttn, xtn
                yield
            S["ttf"] = ttcur

        def seq(ch, half, S):
            cs = slice(ch * 128, (ch + 1) * 128)
            h0 = half * 8
            hk = hkf(h0)
            XT, ttcur = S["XT"], S["ttf"]
            bR = [self.bank(), self.bank()]
            for hh in range(8):
                kc, ba = hk(hh)
                h = h0 + hh
                o = ps[:, bR[hh % 2], (hh // 2) * 64:(hh // 2 + 1) * 64]
                c.pe.matmul(o, lhsT=V(AR.ap[ba:ba + 64, kc, ch, 0:128], AR.bsplit[kc]), rhs=V(STb.ap[ba:ba + 64, kc, :], STb.bufs), start=True, stop=False)
                c.pe.matmul(o, lhsT=V(XT.ap[:, hh, 2, :], XT.bufs), rhs=Vt[:, ch, h * 64:(h + 1) * 64], start=False, stop=True)
            for q in range(2):
                src_ = V(ps.ap[:, bR[q], 0:256].rearrange("p (r v) -> p r v", r=4), ps.bsplit[bR[q]])
                if q:
                    c.act.activation(out=V(RHSb.ap[:, q::2, :], RHSb.bufs), in_=src_, func=AF.Copy)
                else:
                    c.dve.tensor_copy(out=V(RHSb.ap[:, q::2, :], RHSb.bufs), in_=src_)
            yield
            bU = self.bank()
            for hh in range(8):
                c.pe.matmul(ps[:, bU, hh * 64:(hh + 1) * 64], lhsT=V(ttcur.ap[:, hh, :], ttcur.bufs), rhs=V(RHSb.ap[:, hh, :], RHSb.bufs), start=True, stop=True)
            c.dve.tensor_copy(out=V(Ub.ap.rearrange("p h v -> p (h v)"), Ub.bufs), in_=ps[:, bU, :])
            yield
            bYy = [self.bank(), self.bank()]
            for hh in range(8):
                kc, ba = hk(hh)
                h = h0 + hh
                o = ps[:, bYy[hh % 2], (hh // 2) * 64:(hh // 2 + 1) * 64]
                c.pe.matmul(o, lhsT=V(AR.ap[ba:ba + 64, kc, ch, 128:256], AR.bsplit[kc]), rhs=V(STb.ap[ba:ba + 64, kc, :], STb.bufs), start=True, stop=False)
                c.pe.matmul(o, lhsT=V(XT.ap[:, hh, 1, :], XT.bufs), rhs=V(Ub.ap[:, hh, :], Ub.bufs), start=False, stop=False)
                c.pe.matmul(o, lhsT=V(XT.ap[:, hh, 3, :], XT.bufs), rhs=Vt[:, ch, h * 64:(h + 1) * 64], start=False, stop=True)
            for q in range(2):
                c.act.activation(out=V(Ysb.ap[:, q::2, :], Ysb.bufs), in_=V(ps.ap[:, bYy[q], 0:256].rearrange("p (r v) -> p r v", r=4), ps.bsplit[bYy[q]]), func=AF.Copy)
            yield
            bSs = self.bank()
            for hh in range(8):
                kc, ba = hk(hh)
                h = h0 + hh
                o = ps[:, bSs, hh * 64:(hh + 1) * 64]
                c.pe.matmul(o, lhsT=Btok[:, ch, kc * 128:(kc + 1) * 128], rhs=V(Ub.ap[:, hh, :], Ub.bufs), start=True, stop=False)
                c.pe.matmul(o, lhsT=Ktok[:, ch, kc * 128:(kc + 1) * 128], rhs=Vt[:, ch, h * 64:(h + 1) * 64], start=False, stop=True)
            kc0 = h0 // 2
            stv = V(ST.ap[:, kc0:kc0 + 4, :], ST.bufs)
            c.dve.tensor_tensor(out=stv, in0=stv, in1=V(WL.ap[:, ch, kc0:kc0 + 4].unsqueeze(2).to_broadcast([128, 4, 64]), WL.bufs), op=ALU.mult)
            for hh in range(8):
                kc, ba = hk(hh)
                sv = V(ST.ap[ba:ba + 64, kc, :], ST.bufs)
                c.dve.scalar_tensor_tensor(out=sv, in0=V(ps.ap[ba:ba + 64, bSs, hh * 64:(hh + 1) * 64], ps.bsplit[bSs]), scalar=V(WL.ap[ba:ba + 64, ch, kc:kc + 1], WL.bufs), in1=sv,
                                           op0=ALU.mult, op1=ALU.add)
            c.act.activation(out=V(STb.ap[:, kc0:kc0 + 4, :], STb.bufs), in_=stv, func=AF.Copy)
            yield
            c.act.activation(out=Ysq.all(), in_=Ysb.all(), func=AF.Square)
            c.dve.tensor_reduce(out=st8["s1"].all(), in_=Ysb.all(), axis=AX.X, op=ALU.add)
            c.dve.tensor_reduce(out=st8["s2"].all(), in_=Ysq.all(), axis=AX.X, op=ALU.add)
            c.dve.tensor_scalar(out=st8["mean"].all(), in0=st8["s1"].all(), scalar1=float(1.0 / 64.0), scalar2=None, op0=ALU.mult)
            c.dve.tensor_tensor(out=st8["var"].all(), in0=st8["mean"].all(), in1=st8["mean"].all(), op=ALU.mult)
            c.dve.scalar_tensor_tensor(out=st8["var"].all(), in0=st8["s2"].all(), scalar=float(1.0 / 64.0), in1=st8["var"].all(), op0=ALU.mult, op1=ALU.subtract)
            c.act.activation(out=st8["rstd"].all(), in_=st8["var"].all(), func=AF.Sqrt, bias=self.eps_lnx[:, 0:1], scale=1.0)
            c.dve.reciprocal(out=st8["rstd"].all(), in_=st8["rstd"].all())
            yield
            bc8 = lambda t_: V(t_.ap.unsqueeze(2).to_broadcast([128, 8, 64]), t_.bufs)
            c.dve.tensor_tensor(out=Ysb.all(), in0=Ysb.all(), in1=bc8(st8["mean"]), op=ALU.subtract)
            c.dve.tensor_tensor(out=Ysb.all(), in0=Ysb.all(), in1=bc8(st8["rstd"]), op=ALU.mult)
            f0 = h0 * 64
            lw_ = V(self.bc.ap[:, bcol + f0:bcol + f0 + 512].rearrange("p (h v) -> p h v", h=8), self.bc.bufs)
            lb_ = V(self.bc.ap[:, bcol + 1024 + f0:bcol + 1024 + f0 + 512].rearrange("p (h v) -> p h v", h=8), self.bc.bufs)
            c.dve.tensor_tensor(out=Ysb.all(), in0=Ysb.all(), in1=lw_, op=ALU.mult)
            c.dve.tensor_tensor(out=Ysb.all(), in0=Ysb.all(), in1=lb_, op=ALU.add)
            v3 = V(Vt.ap[:, ch, f0:f0 + 512].rearrange("p (h v) -> p h v", h=8), Vt.bsplit[ch])
            c.pool.tensor_tensor(out=Ysq.all(), in0=v3, in1=V(rk_tok.ap[:, ch, h0:h0 + 8].unsqueeze(2).to_broadcast([128, 8, 64]), rk_tok.bufs), op=ALU.mult)
            c.dve.tensor_tensor(out=yv.all(), in0=Ysb.all(), in1=Ysq.all(), op=ALU.add)
            yield
            bT = self.bank()
            pb = ps.ap[:, bT, :].bitcast(BF16)
            yvf = yv.ap.rearrange("p h v -> p (h v)")
            for r in range(4):
                c.pe.transpose(V(pb[:, r * 128:(r + 1) * 128], ps.bsplit[bT]), V(yvf[:, r * 128:(r + 1) * 128], yv.bufs), self.ident_b.all())
            c.dve.tensor_tensor(out=yg[:, kc0:kc0 + 4, cs], in0=V(pb[:, 0:512].rearrange("p (k t) -> p k t", k=4), ps.bsplit[bT]), in1=gT[:, kc0:kc0 + 4, cs], op=ALU.mult)

        def drain(*gens):
            gens = [g for g in gens if g is not None]
            while gens:
                for g in list(gens):
                    try:
                        next(g)
                    except StopIteration:
                        gens.remove(g)

        units = [(ch, half) for ch in range(NCH) for half in range(2)]
        prev = None
        for ui, (ch, half) in enumerate(units):
            S = SETS[ui % 2]
            drain(seq(*prev) if prev is not None else None, chain(ch, half, S))
            prev = (ch, half, S)
        drain(seq(*prev))
        for ob in range(2):
            w = self.weight(f"L{i}.rw_o{ob}")
            for m in range(4):
                mo = ob * 4 + m
                b = self.bank()
                for kc in range(KC):
                    c.pe.matmul(ps[:, b, 0:TM], lhsT=V(w.ap[:, kc, m * 128:(m + 1) * 128], w.bufs), rhs=yg[:, kc, :], start=(kc == 0), stop=(kc == KC - 1))
                c.dve.scalar_tensor_tensor(out=s_t[:, mo, :], in0=x_t[:, mo, :], scalar=float(ALPHA), in1=ps[:, b, 0:TM], op0=ALU.mult, op1=ALU.add)

    def mamba(self, i):
        TM = self.TM
        xb_t, x_t, s_t = self.xbv, self.xv, self.sv
        c = self.c
        ps = self.ps
        NCH = TM // 128
        TW = TM + 4
        st, stb, hist, A_bc = self.ssm_state[i]
        bcol = self.bc_names[f"ssm{i}"]
        c.areset()
        uT = c.carve("uT", [128, 24, TW], BF16, split=1)
        offA = c.ar_off
        zs = c.carve("zs", [128, NCH, 2048], BF16, split=1)
        xs = c.carve("xs", [128, NCH, 2048], BF16, split=1)
        Btok = c.carve("Btok", [128, NCH, 512], BF16, split=1)
        BT = c.carve("BT", [128, 4, TM], BF16, split=1)
        CT = c.carve("CT", [128, 4, TM], BF16, split=1)
        yT = c.carve("yT", [128, 16, TM], BF16, split=1)
        sm = {k: c.carve("sm_" + k, [128, NCH, 32], F32) for k in ("dt", "dA", "cum", "ncum", "ecum", "cl", "ecl", "wx")}
        self.m_cbm = c.carve("m_cbm", [128, 512], BF16)
        cbrow = c.carve("cbrow", [128, 1024], BF16)
        self.m_xdt = c.carve("m_xdt", [128, 8, 64], BF16)
        self.m_xw = c.carve("m_xw", [128, 8, 64], BF16)
        self.m_yc = c.carve("m_yc", [128, 8, 64], F32)
        self.m_t1 = c.carve("m_t1", [128, 8, 64], BF16)
        self.m_yb = c.carve("m_yb", [128, 8, 64], BF16)
        self.m_ssq = c.carve("m_ssq", [128, 2], F32)
        self.m_rs = c.carve("m_rs", [128, 2], F32)
        w = self.weight(f"L{i}.ssm_dt")
        b = self.bank()
        for ch in range(NCH):
            for kc in range(KC):
                c.pe.matmul(ps[:, b, ch * 32:(ch + 1) * 32], lhsT=xb_t[:, kc, ch * 128:(ch + 1) * 128], rhs=V(w.ap[:, kc, :], w.bufs), start=(kc == 0), stop=(kc == KC - 1))
        dtb = V(self.bc.ap[:, bcol + 64:bcol + 96].unsqueeze(1).to_broadcast([128, NCH, 32]), self.bc.bufs)
        c.dve.tensor_tensor(out=sm["dt"].all(), in0=V(ps.ap[:, b, 0:NCH * 32].rearrange("p (c h) -> p c h", c=NCH), ps.bsplit[b]), in1=dtb, op=ALU.add)
        c.act.activation(out=sm["dt"].all(), in_=sm["dt"].all(), func=AF.Exp)
        c.act.activation(out=sm["dt"].all(), in_=sm["dt"].all(), func=AF.Ln, bias=self.one_c[:, 0:1], scale=1.0)
        c.dve.tensor_tensor(out=sm["dA"].all(), in0=sm["dt"].all(), in1=V(A_bc.ap.unsqueeze(1).to_broadcast([128, NCH, 32]), A_bc.bufs), op=ALU.mult)
        b = self.bank()
        flat = lambda t: V(t.ap.rearrange("p c h -> p (c h)"), t.bufs)
        nsm = NCH * 32
        c.pe.matmul(ps[:, b, 0:nsm], lhsT=self.tri.all(), rhs=flat(sm["dA"]), start=True, stop=True)
        c.pe.matmul(ps[:, b, 128:128 + nsm], lhsT=self.ones_f.all(), rhs=flat(sm["dA"]), start=True, stop=True)
        c.dve.tensor_copy(out=flat(sm["cum"]), in_=ps[:, b, 0:nsm])
        c.dve.tensor_copy(out=flat(sm["cl"]), in_=ps[:, b, 128:128 + nsm])
        c.dve.tensor_scalar(out=sm["ncum"].all(), in0=sm["cum"].all(), scalar1=-1.0, scalar2=None, op0=ALU.mult)
        c.act.activation(out=sm["ecum"].all(), in_=sm["cum"].all(), func=AF.Exp)
        c.act.activation(out=sm["ecl"].all(), in_=sm["cl"].all(), func=AF.Exp)
        c.dve.tensor_tensor(out=sm["wx"].all(), in0=sm["cl"].all(), in1=sm["cum"].all(), op=ALU.subtract)
        c.act.activation(out=sm["wx"].all(), in_=sm["wx"].all(), func=AF.Exp)
        c.dve.tensor_tensor(out=sm["wx"].all(), in0=sm["wx"].all(), in1=sm["dt"].all(), op=ALU.mult)
        for zb in range(4):
            w = self.weight(f"L{i}.ssm_z{zb}")
            for ch in range(NCH):
                b = self.bank()
                for kc in range(KC):
                    c.pe.matmul(ps[:, b, :], lhsT=xb_t[:, kc, ch * 128:(ch + 1) * 128], rhs=V(w.ap[:, kc, :], w.bufs), start=(kc == 0), stop=(kc == KC - 1))
                c.act.activation(out=zs[:, ch, zb * 512:(zb + 1) * 512], in_=ps[:, b, :], func=AF.Silu)
        c.pool.tensor_copy(out=V(uT.ap[:, :, 0:4], uT.bufs), in_=hist.all())
        for xb_ in range(6):
            w = self.weight(f"L{i}.ssm_x{xb_}")
            for m in range(4):
                cc = xb_ * 4 + m
                b = self.bank()
                for kc in range(KC):
                    c.pe.matmul(ps[:, b, 0:TM], lhsT=V(w.ap[:, kc, m * 128:(m + 1) * 128], w.bufs), rhs=xb_t[:, kc, :], start=(kc == 0), stop=(kc == KC - 1))
                if cc % 2:
                    c.act.activation(out=uT[:, cc, 4:4 + TM], in_=ps[:, b, 0:TM], func=AF.Copy)
                else:
                    c.dve.tensor_copy(out=uT[:, cc, 4:4 + TM], in_=ps[:, b, 0:TM])
        c.pool.tensor_copy(out=hist.all(), in_=V(uT.ap[:, :, TM:TM + 4], uT.bufs))
        wcb0 = self.weight(f"L{i}.ssm_cb")
        c.act.activation(out=cbrow[0:65, :], in_=V(wcb0.ap[0:65, 0, :], wcb0.bufs), func=AF.Copy)
        for cc in range(24):
            w = self.weight(f"L{i}.ssm_cw{cc}")
            if cc < 20:
                q = cc % 4
                if q == 0:
                    cvb = [self.bank() for _ in range(NCH)]
                for ch in range(NCH):
                    o = ps[:, cvb[ch], q * 128:(q + 1) * 128]
                    for tap in range(4):
                        c.pe.matmul(o, lhsT=uT[:, cc, 1 + tap + ch * 128:1 + tap + (ch + 1) * 128], rhs=V(w.ap[:, 0, tap * 128:(tap + 1) * 128], w.bufs), start=(tap == 0), stop=False)
                    c.pe.matmul(o, lhsT=self.ones1_b[(cc // 8) * 32:(cc // 8) * 32 + 1, :], rhs=cbrow[(cc // 8) * 32:(cc // 8) * 32 + 1, (cc % 8) * 128:(cc % 8 + 1) * 128], start=False, stop=True)
                if q == 3:
                    for ch in range(NCH):
                        if cc < 16:
                            c.act.activation(out=xs[:, ch, (cc - 3) * 128:(cc + 1) * 128], in_=ps[:, cvb[ch], :], func=AF.Silu)
                        else:
                            c.act.activation(out=Btok[:, ch, :], in_=ps[:, cvb[ch], :], func=AF.Silu)
            if cc >= 16:
                b = self.bank()
                for tap in range(4):
                    c.pe.matmul(ps[:, b, 0:TM], lhsT=V(w.ap[:, 0, tap * 128:(tap + 1) * 128], w.bufs), rhs=uT[:, cc, 1 + tap:1 + tap + TM], start=(tap == 0), stop=(tap == 3))
                dst = BT[:, cc - 16, :] if cc < 20 else CT[:, cc - 20, :]
                c.act.activation(out=dst, in_=ps[:, b, 0:TM], func=AF.Silu, bias=self.pvv(f"ssm_cb{i}", cc), scale=1.0)
        hi = c.ar_off
        c.ar_off = 0
        dg = c.carve("dg", [128, 8, 128], F32)
        seg = c.carve("seg", [128, 8, 128], F32)
        MT = c.carve("MT", [128, 8, 128], BF16)
        assert c.ar_off <= offA
        c.ar_off = hi
        for ch in range(NCH):
            cs = slice(ch * 128, (ch + 1) * 128)
            bCB = self.bank()
            for g in range(4):
                c.pe.matmul(ps[:, bCB, g * 128:(g + 1) * 128], lhsT=BT[:, g, cs], rhs=CT[:, g, cs], start=True, stop=True)
            c.dve.tensor_tensor(out=self.m_cbm.all(), in0=ps[:, bCB, :], in1=self.mask4.all(), op=ALU.mult)
            for blk in range(4):
                h0 = blk * 8
                g = blk
                xs3 = V(xs.ap[:, ch, h0 * 64:(h0 + 8) * 64].rearrange("p (h q) -> p h q", q=64), xs.bsplit[ch])
                zs3 = V(zs.ap[:, ch, h0 * 64:(h0 + 8) * 64].rearrange("p (h q) -> p h q", q=64), zs.bsplit[ch])

                def hb(t, n):
                    return V(t.ap[:, ch, h0:h0 + 8].unsqueeze(2).to_broadcast([128, 8, n]), t.bufs)
                xdt, xw, yc, t1, yb = self.m_xdt, self.m_xw, self.m_yc, self.m_t1, self.m_yb
                c.dve.tensor_tensor(out=xdt.all(), in0=xs3, in1=hb(sm["dt"], 64), op=ALU.mult)
                c.pool.tensor_tensor(out=xw.all(), in0=xs3, in1=hb(sm["wx"], 64), op=ALU.mult)
                bI = self.bank()
                c.pe.matmul(ps[:, bI, :], lhsT=CT[:, g, cs], rhs=V(stb.ap[:, h0:h0 + 8, :].rearrange("p h q -> p (h q)"), stb.bufs), start=True, stop=True)
                c.dve.tensor_tensor(out=yc.all(), in0=V(ps.ap[:, bI, :].rearrange("p (h q) -> p h q", q=64), ps.bsplit[bI]), in1=hb(sm["ecum"], 64), op=ALU.mult)
                c.dve.tensor_tensor(out=dg.all(), in0=V(self.ident_f.ap.unsqueeze(1).to_broadcast([128, 8, 128]), self.ident_f.bufs), in1=hb(sm["cum"], 128), op=ALU.mult)
                for q in range(2):
                    bG = self.bank()
                    c.pe.matmul(ps[:, bG, :], lhsT=self.ones_f.all(), rhs=V(dg.ap[:, q * 4:(q + 1) * 4, :].rearrange("p h t -> p (h t)"), dg.bufs), start=True, stop=True)
                    c.dve.tensor_tensor(out=V(seg.ap[:, q * 4:(q + 1) * 4, :], seg.bufs), in0=V(ps.ap[:, bG, :].rearrange("p (h t) -> p h t", t=128), ps.bsplit[bG]),
                                        in1=V(sm["ncum"].ap[:, ch, h0 + q * 4:h0 + (q + 1) * 4].unsqueeze(2).to_broadcast([128, 4, 128]), sm["ncum"].bufs), op=ALU.add)
                c.dve.tensor_scalar(out=seg.all(), in0=seg.all(), scalar1=0.0, scalar2=None, op0=ALU.min)
                c.act.activation(out=seg.all(), in_=seg.all(), func=AF.Exp)
                cb4 = V(self.m_cbm.ap[:, g * 128:(g + 1) * 128].unsqueeze(1).to_broadcast([128, 8, 128]), self.m_cbm.bufs)
                c.dve.tensor_tensor(out=MT.all(), in0=seg.all(), in1=cb4, op=ALU.mult)
                bY = self.bank()
                for hh in range(8):
                    c.pe.matmul(ps[:, bY, hh * 64:(hh + 1) * 64], lhsT=V(MT.ap[:, hh, :], MT.bufs), rhs=V(xdt.ap[:, hh, :], xdt.bufs), start=True, stop=True)
                c.dve.tensor_tensor(out=yc.all(), in0=V(ps.ap[:, bY, :].rearrange("p (h q) -> p h q", q=64), ps.bsplit[bY]), in1=yc.all(), op=ALU.add)
                dbc = V(self.bc.ap[:, bcol + 32 + h0:bcol + 32 + h0 + 8].unsqueeze(2).to_broadcast([128, 8, 64]), self.bc.bufs)
                c.dve.tensor_tensor(out=t1.all(), in0=xs3, in1=dbc, op=ALU.mult)
                c.dve.tensor_tensor(out=yc.all(), in0=yc.all(), in1=t1.all(), op=ALU.add)
                c.dve.tensor_tensor(out=yc.all(), in0=yc.all(), in1=zs3, op=ALU.mult)
                c.act.activation(out=t1.all(), in_=yc.all(), func=AF.Square, accum_out=self.m_ssq[:, 0:1])
                c.act.activation(out=self.m_rs[:, 0:1], in_=self.m_ssq[:, 0:1], func=AF.Sqrt, bias=self.eps_rms[:, 0:1], scale=float(1.0 / 512.0))
                c.dve.reciprocal(out=self.m_rs[:, 0:1], in_=self.m_rs[:, 0:1])
                c.dve.tensor_scalar(out=yb.all(), in0=yc.all(), scalar1=self.m_rs[:, 0:1], scalar2=None, op0=ALU.mult)
                ybf = yb.ap.rearrange("p h q -> p (h q)")
                bT = self.bank()
                pb = ps.ap[:, bT, :].bitcast(BF16)
                for r in range(4):
                    c.pe.transpose(V(pb[:, r * 128:(r + 1) * 128], ps.bsplit[bT]), V(ybf[:, r * 128:(r + 1) * 128], yb.bufs), self.ident_b.all())
                for r in range(4):
                    fc = blk * 4 + r
                    if r % 2:
                        c.act.activation(out=yT[:, fc, cs], in_=V(pb[:, r * 128:(r + 1) * 128], ps.bsplit[bT]), func=AF.Copy, scale=self.pvv(f"ssm_nw{i}", fc))
                    else:
                        c.dve.tensor_scalar(out=yT[:, fc, cs], in0=V(pb[:, r * 128:(r + 1) * 128], ps.bsplit[bT]), scalar1=self.pvv(f"ssm_nw{i}", fc), scalar2=None, op0=ALU.mult)
                bS = self.bank()
                c.pe.matmul(ps[:, bS, :], lhsT=Btok[:, ch, g * 128:(g + 1) * 128], rhs=V(xw.ap.rearrange("p h q -> p (h q)"), xw.bufs), start=True, stop=True)
                sv = V(st.ap[:, h0:h0 + 8, :], st.bufs)
                c.dve.tensor_tensor(out=sv, in0=sv, in1=hb(sm["ecl"], 64), op=ALU.mult)
                c.dve.tensor_tensor(out=sv, in0=V(ps.ap[:, bS, :].rearrange("p (h q) -> p h q", q=64), ps.bsplit[bS]), in1=sv, op=ALU.add)
                c.act.activation(out=V(stb.ap[:, h0:h0 + 8, :], stb.bufs), in_=sv, func=AF.Copy)
        for ob in range(4):
            w = self.weight(f"L{i}.ssm_out{ob}")
            for m in range(2):
                mo = ob * 2 + m
                b = self.bank()
                for kc in range(16):
                    c.pe.matmul(ps[:, b, 0:TM], lhsT=V(w.ap[:, kc, m * 128:(m + 1) * 128], w.bufs), rhs=yT[:, kc, :], start=(kc == 0), stop=(kc == 15))
                c.dve.scalar_tensor_tensor(out=s_t[:, mo, :], in0=x_t[:, mo, :], scalar=float(ALPHA), in1=ps[:, b, 0:TM], op0=ALU.mult, op1=ALU.add)

    def mlstm(self, i):
        TM = self.TM
        xb_t, x_t, s_t = self.xbv, self.xv, self.sv
        cx = self.c
        c = cx
        c.areset()
        self.qT = c.carve("qT", [128, 4, TM], BF16, split=1)
        self.kT = c.carve("kT", [128, 4, TM], BF16, split=1)
        self.ktok = c.carve("ktok", [128, TM // 128, 512], BF16, split=1)
        self.vtok = c.carve("vtok", [128, TM // 128, 1024], BF16, split=1)
        self.sgo = c.carve("sgo", [128, 8, TM], BF16, split=1)
        self.hn = c.carve("hn", [128, 8, TM], BF16, split=1)
        self.mg = {k: c.carve("mg_" + k, [128, n], F32) for k, n in
                   (("graw", 8 * (TM // 128)), ("gi", 4 * (TM // 128)), ("gf", 4 * (TM // 128)), ("bcum", 4 * (TM // 128)), ("blast", 4 * (TM // 128)), ("a_s", 4 * (TM // 128)), ("ws", 4 * (TM // 128)), ("dec", 4 * (TM // 128)))}
        self.diag = c.carve("diag", [128, 512], F32)
        self.scb = c.carve("scb", [128, 512], F32)
        self.dm = c.carve("dm", [128, 512], F32)
        self.pT = c.carve("pT", [128, 512], BF16)
        self.qs = c.carve("qs", [128, 512], BF16)
        self.rden = c.carve("rden", [128, 512], F32)
        self.hd = c.carve("hd", [128, 2, 512], F32, split=1)
        self.sqh = c.carve("sqh", [128, 2, 512], BF16, split=1)
        self.rstd_m = c.carve("rstd_m", [128, 512], F32)
        self.kw = c.carve("kw", [128, 512], BF16)
        st = self.ml_state[i]
        C, Cb, nbc, nbcb = st
        ps = self.ps
        NCH = TM // 128
        w = self.weight(f"L{i}.ml_g")
        bg = self.bank()
        for ch in range(NCH):
            for kc in range(KC):
                cx.pe.matmul(ps[:, bg, ch * 8:(ch + 1) * 8], lhsT=xb_t[:, kc, ch * 128:(ch + 1) * 128], rhs=V(w.ap[:, kc, :], w.bufs), start=(kc == 0), stop=(kc == KC - 1))
        g = self.mg
        bcol = self.bc_names[f"ml_bg{i}"]
        for ch in range(NCH):
            cx.dve.tensor_tensor(out=g["graw"][:, ch * 8:(ch + 1) * 8], in0=ps[:, bg, ch * 8:(ch + 1) * 8], in1=self.bc[:, bcol:bcol + 8], op=ALU.add)
        cx.act.activation(out=g["graw"].all(), in_=g["graw"].all(), func=AF.Tanh, scale=float(1.0 / 15.0))
        gr = g["graw"].ap.rearrange("p (c e) -> p c e", e=8)
        gi3 = g["gi"].ap.rearrange("p (c e) -> p c e", e=4)
        gf3 = g["gf"].ap.rearrange("p (c e) -> p c e", e=4)
        cx.dve.tensor_scalar(out=g["gi"].v(gi3), in0=g["graw"].v(gr[:, :, 0:4]), scalar1=15.0, scalar2=None, op0=ALU.mult)
        cx.act.activation(out=g["gf"].v(gf3), in_=g["graw"].v(gr[:, :, 4:8]), func=AF.Exp, scale=-15.0)
        cx.act.activation(out=g["gf"].all(), in_=g["gf"].all(), func=AF.Ln, bias=self.one_c[:, 0:1], scale=1.0)
        cx.dve.tensor_scalar(out=g["gf"].all(), in0=g["gf"].all(), scalar1=-1.0, scalar2=None, op0=ALU.mult)
        b1 = self.bank()
        ng = 4 * NCH
        cx.pe.matmul(ps[:, b1, 0:ng], lhsT=self.tri.all(), rhs=g["gf"].all(), start=True, stop=True)
        cx.pe.matmul(ps[:, b1, 32:32 + ng], lhsT=self.ones_f.all(), rhs=g["gf"].all(), start=True, stop=True)
        cx.dve.tensor_copy(out=g["bcum"].all(), in_=ps[:, b1, 0:ng])
        cx.dve.tensor_copy(out=g["blast"].all(), in_=ps[:, b1, 32:32 + ng])
        cx.dve.tensor_tensor(out=g["a_s"].all(), in0=g["gi"].all(), in1=g["bcum"].all(), op=ALU.subtract)
        cx.dve.tensor_tensor(out=g["ws"].all(), in0=g["a_s"].all(), in1=g["blast"].all(), op=ALU.add)
        cx.act.activation(out=g["ws"].all(), in_=g["ws"].all(), func=AF.Exp)
        cx.act.activation(out=g["dec"].all(), in_=g["blast"].all(), func=AF.Exp)
        w = self.weight(f"L{i}.ml_in0")
        for h in range(4):
            b = self.bank()
            for kc in range(KC):
                cx.pe.matmul(ps[:, b, 0:TM], lhsT=V(w.ap[:, kc, h * 128:(h + 1) * 128], w.bufs), rhs=xb_t[:, kc, :], start=(kc == 0), stop=(kc == KC - 1))
            cx.act.activation(out=self.qT[:, h, :], in_=ps[:, b, 0:TM], func=AF.Copy, scale=float(128 ** -0.5))
        w = self.weight(f"L{i}.ml_in1")
        for h in range(4):
            b = self.bank()
            for kc in range(KC):
                cx.pe.matmul(ps[:, b, 0:TM], lhsT=V(w.ap[:, kc, h * 128:(h + 1) * 128], w.bufs), rhs=xb_t[:, kc, :], start=(kc == 0), stop=(kc == KC - 1))
            cx.dve.tensor_copy(out=self.kT[:, h, :], in_=ps[:, b, 0:TM])
        for ch in range(NCH):
            b = self.bank()
            for kc in range(KC):
                cx.pe.matmul(ps[:, b, :], lhsT=xb_t[:, kc, ch * 128:(ch + 1) * 128], rhs=V(w.ap[:, kc, :], w.bufs), start=(kc == 0), stop=(kc == KC - 1))
            cx.act.activation(out=self.ktok[:, ch, :], in_=ps[:, b, :], func=AF.Copy)
        for vb in range(2):
            w = self.weight(f"L{i}.ml_in{2 + vb}")
            for ch in range(NCH):
                b = self.bank()
                for kc in range(KC):
                    cx.pe.matmul(ps[:, b, :], lhsT=xb_t[:, kc, ch * 128:(ch + 1) * 128], rhs=V(w.ap[:, kc, :], w.bufs), start=(kc == 0), stop=(kc == KC - 1))
                if (ch + vb) % 2:
                    cx.act.activation(out=self.vtok[:, ch, vb * 512:(vb + 1) * 512], in_=ps[:, b, :], func=AF.Copy)
                else:
                    cx.dve.tensor_copy(out=self.vtok[:, ch, vb * 512:(vb + 1) * 512], in_=ps[:, b, :])
        for ob in range(2):
            w = self.weight(f"L{i}.ml_in{4 + ob}")
            for m in range(4):
                fc = ob * 4 + m
                b = self.bank()
                for kc in range(KC):
                    cx.pe.matmul(ps[:, b, 0:TM], lhsT=V(w.ap[:, kc, m * 128:(m + 1) * 128], w.bufs), rhs=xb_t[:, kc, :], start=(kc == 0), stop=(kc == KC - 1))
                cx.act.activation(out=self.sgo[:, fc, :], in_=ps[:, b, 0:TM], func=AF.Sigmoid)
                cx.dve.tensor_scalar(out=self.sgo[:, fc, :], in0=self.sgo[:, fc, :], scalar1=self.pvv(f"ml_nw{i}", fc), scalar2=None, op0=ALU.mult)
        for ch in range(NCH):
            cs = slice(ch * 128, (ch + 1) * 128)
            bS, bB = self.bank(), self.bank()
            for h in range(4):
                cx.pe.matmul(ps[:, bS, h * 128:(h + 1) * 128], lhsT=self.kT[:, h, cs], rhs=self.qT[:, h, cs], start=True, stop=True)
            cx.dve.tensor_tensor(out=V(self.diag.ap.rearrange("p (h t) -> p h t", h=4), self.diag.bufs), in0=V(self.ident_f.ap.unsqueeze(1).to_broadcast([128, 4, 128]), self.ident_f.bufs),
                                 in1=V(g["bcum"].ap[:, ch * 4:(ch + 1) * 4].unsqueeze(2).to_broadcast([128, 4, 128]), g["bcum"].bufs), op=ALU.mult)
            cx.pe.matmul(ps[:, bB, :], lhsT=self.ones_f.all(), rhs=self.diag.all(), start=True, stop=True)
            cx.act.activation(out=self.scb.all(), in_=ps[:, bB, :], func=AF.Exp)
            for h in range(4):
                cx.dve.tensor_scalar(out=self.dm[:, h * 128:(h + 1) * 128], in0=ps[:, bB, h * 128:(h + 1) * 128], scalar1=g["a_s"][:, ch * 4 + h:ch * 4 + h + 1], scalar2=15.5,
                                     op0=ALU.add, op1=ALU.min)
            cx.act.activation(out=self.dm.all(), in_=self.dm.all(), func=AF.Exp)
            cx.dve.tensor_tensor(out=self.dm.all(), in0=self.dm.all(), in1=self.mask4.all(), op=ALU.mult)
            cx.dve.tensor_tensor(out=self.pT.all(), in0=ps[:, bS, :], in1=self.dm.all(), op=ALU.mult)
            qv = V(self.qT.ap[:, :, cs], self.qT.bufs)
            cx.dve.tensor_tensor(out=self.qs.v(self.qs.ap.rearrange("p (h t) -> p h t", h=4)), in0=qv, in1=self.scb.v(self.scb.ap.rearrange("p (h t) -> p h t", h=4)), op=ALU.mult)
            bD = self.bank()
            cx.pe.matmul(ps[:, bD, :], lhsT=self.ones1_b.all(), rhs=self.pT.all(), start=True, stop=False)
            for h in range(4):
                cx.pe.matmul(ps[:, bD, h * 128:(h + 1) * 128], lhsT=nbcb[:, h, :], rhs=self.qs[:, h * 128:(h + 1) * 128], start=False, stop=(h == 3))
            cx.dve.tensor_scalar(out=self.rden.all(), in0=ps[:, bD, :], scalar1=-1.0, scalar2=1.0, op0=ALU.mult, op1=ALU.max)
            cx.dve.tensor_tensor(out=self.rden.all(), in0=ps[:, bD, :], in1=self.rden.all(), op=ALU.max)
            cx.dve.reciprocal(out=self.rden.all(), in_=self.rden.all())
            bH = [self.bank(), self.bank()]
            for vc in range(2):
                for h in range(4):
                    cx.pe.matmul(ps[:, bH[vc], h * 128:(h + 1) * 128], lhsT=self.vtok[:, ch, h * 256 + vc * 128:h * 256 + (vc + 1) * 128], rhs=self.pT[:, h * 128:(h + 1) * 128],
                                 start=True, stop=False)
                    cx.pe.matmul(ps[:, bH[vc], h * 128:(h + 1) * 128], lhsT=Cb[:, h, vc * 128:(vc + 1) * 128], rhs=self.qs[:, h * 128:(h + 1) * 128], start=False, stop=True)
            for vc in range(2):
                cx.dve.tensor_tensor(out=self.hd[:, vc, :], in0=ps[:, bH[vc], :], in1=self.rden.all(), op=ALU.mult)
                cx.act.activation(out=self.sqh[:, vc, :], in_=self.hd[:, vc, :], func=AF.Square)
            bQ = self.bank()
            for vc in range(2):
                cx.pe.matmul(ps[:, bQ, :], lhsT=self.ones256_b.all(), rhs=self.sqh[:, vc, :], start=(vc == 0), stop=(vc == 1))
            cx.act.activation(out=self.rstd_m.all(), in_=ps[:, bQ, :], func=AF.Sqrt, bias=self.eps_rms[:, 0:1], scale=1.0)
            cx.dve.reciprocal(out=self.rstd_m.all(), in_=self.rstd_m.all())
            for vc in range(2):
                e = cx.dve
                e.tensor_tensor(out=self.hd[:, vc, :], in0=self.hd[:, vc, :], in1=self.rstd_m.all(), op=ALU.mult)
                hv = V(self.hd.ap[:, vc, :].rearrange("p (h t) -> p h t", h=4), self.hd.bsplit[vc])
                e.tensor_tensor(out=self.hn[:, vc::2, cs], in0=hv, in1=self.sgo[:, vc::2, cs], op=ALU.mult)
            cx.dve.tensor_tensor(out=V(self.kw.ap.rearrange("p (h t) -> p h t", h=4), self.kw.bufs), in0=V(self.ktok.ap[:, ch, :].rearrange("p (h t) -> p h t", h=4), self.ktok.bsplit[ch]),
                                 in1=V(g["ws"].ap[:, ch * 4:(ch + 1) * 4].unsqueeze(2).to_broadcast([128, 4, 128]), g["ws"].bufs), op=ALU.mult)
            bC = [self.bank(), self.bank()]
            bN = self.bank()
            for h in range(4):
                cx.pe.matmul(ps[:, bC[h // 2], (h % 2) * 256:(h % 2 + 1) * 256], lhsT=self.kw[:, h * 128:(h + 1) * 128], rhs=self.vtok[:, ch, h * 256:(h + 1) * 256], start=True, stop=True)
            for h in range(4):
                cx.pe.matmul(ps[:, bN, h * 128:(h + 1) * 128], lhsT=self.kw[:, h * 128:(h + 1) * 128], rhs=self.ones1_b.all(), start=True, stop=True)
            for h in range(4):
                dsc = g["dec"][:, ch * 4 + h:ch * 4 + h + 1]
                cx.dve.scalar_tensor_tensor(out=C[:, h, :], in0=C[:, h, :], scalar=dsc, in1=ps[:, bC[h // 2], (h % 2) * 256:(h % 2 + 1) * 256], op0=ALU.mult, op1=ALU.add)
                cx.dve.scalar_tensor_tensor(out=nbc[:, h, :], in0=nbc[:, h, :], scalar=dsc, in1=ps[:, bN, h * 128:(h + 1) * 128], op0=ALU.mult, op1=ALU.add)
            cx.act.activation(out=Cb.all(), in_=C.all(), func=AF.Copy)
            cx.act.activation(out=nbcb.all(), in_=nbc.all(), func=AF.Copy)
        for ob in range(2):
            w = self.weight(f"L{i}.ml_out{ob}")
            for m in range(4):
                mo = ob * 4 + m
                b = self.bank()
                for kc in range(KC):
                    cx.pe.matmul(ps[:, b, 0:TM], lhsT=V(w.ap[:, kc, m * 128:(m + 1) * 128], w.bufs), rhs=self.hn[:, kc, :], start=(kc == 0), stop=(kc == KC - 1))
                cx.dve.scalar_tensor_tensor(out=s_t[:, mo, :], in0=x_t[:, mo, :], scalar=float(ALPHA), in1=ps[:, b, 0:TM], op0=ALU.mult, op1=ALU.add)

    def pack_pv(self, inputs):
        pv = np.zeros((128, self.npv), np.float32)

        def put(name, vec):
            col = self.pv_names[name]
            n = vec.shape[0] // 128
            pv[:, col:col + n] = vec.reshape(n, 128).T
        for i in self.layers:
            for j in range(2):
                put(f"ln_g{i}.{j}", inputs["ln_g"][i, j])
                put(f"ln_b{i}.{j}", inputs["ln_b"][i, j])
            if i % 3 == 0 and self.mixers:
                put(f"ml_nw{i}", inputs["ml_norm_w"][i // 3])
            if i % 3 == 2 and self.mixers:
                j = i // 3
                put(f"rw{i}_mix", inputs["rw_mix"][j].reshape(-1))
                put(f"rw{i}_w0", inputs["rw_w0"][j])
                put(f"rw{i}_a0", inputs["rw_a0"][j])
                put(f"rw{i}_k_k", inputs["rw_k_k"][j])
                put(f"rw{i}_k_a", inputs["rw_k_a"][j])
                put(f"rw{i}_r_k", inputs["rw_r_k"][j].reshape(-1))
            if i % 3 == 1 and self.mixers:
                put(f"ssm_cb{i}", inputs["ssm_conv_b"][i // 3])
                put(f"ssm_nw{i}", inputs["ssm_norm_w"][i // 3])
        return pv

    def pack_bc(self, inputs):
        bc = np.zeros((128, self.nbc), np.float32)
        for i in self.layers:
            if i % 3 == 0 and self.mixers:
                col = self.bc_names[f"ml_bg{i}"]
                bc[:, col:col + 8] = inputs["ml_b_gate"][i // 3][None, :]
            if i % 3 == 2 and self.mixers:
                col = self.bc_names[f"rw{i}"]
                bc[:, col:col + 1024] = inputs["rw_lnx_w"][i // 3][None, :]
                bc[:, col + 1024:col + 2048] = inputs["rw_lnx_b"][i // 3][None, :]
            if i % 3 == 1 and self.mixers:
                col = self.bc_names[f"ssm{i}"]
                j = i // 3
                bc[:, col:col + 32] = inputs["ssm_a_log"][j][None, :]
                bc[:, col + 32:col + 64] = inputs["ssm_d"][j][None, :]
                bc[:, col + 64:col + 96] = inputs["ssm_dt_bias"][j][None, :]
        return bc


_orig_alloc = Prog.alloc


def _alloc2(self):
    _orig_alloc(self)
    c = self.c
    self.eps_ln = c.sbuf("eps_ln", [128, 1], F32)
    self.eps_rms = c.sbuf("eps_rms", [128, 1], F32)
    self.one_c = c.sbuf("one_c", [128, 1], F32)
    self.bc = c.sbuf("bc", [128, self.nbc], F32)
    self.tri = c.sbuf("tri", [128, 128], F32)
    self.ones_f = c.sbuf("ones_f", [128, 128], F32)
    self.ident_f = c.sbuf("ident_f", [128, 128], F32)
    self.mask4 = c.sbuf("mask4", [128, 512], F32)
    self.ones1_b = c.sbuf("ones1_b", [128, 128], BF16)
    self.ones256_b = c.sbuf("ones256_b", [128, 128], BF16)
    self.ident_b = c.sbuf("ident_b", [128, 128], BF16)
    self.maskX = c.sbuf("maskX", [128, 512], BF16)
    self.lowS = c.sbuf("lowS", [128, 128], BF16)
    self.blockones = c.sbuf("blockones", [128, 128], BF16)
    self.sel2 = c.sbuf("sel2", [128, 2], BF16)
    self.rmask = c.sbuf("rmask", [128, T], F32)
    self.eps_lnx = c.sbuf("eps_lnx", [128, 1], F32)
    self.rw_state = {}
    self.rw_omka = {}
    if self.mixers:
        for i in self.layers:
            if i % 3 == 2:
                self.rw_state[i] = (c.sbuf(f"rwS{i}", [128, 8, 64], F32), c.sbuf(f"rwSb{i}", [128, 8, 64], BF16), c.sbuf(f"rwX{i}", [128, 8, 1], F32))
                self.rw_omka[i] = c.sbuf(f"rwOmka{i}", [128, 8], F32)
    self.ssm_state = {}
    if self.mixers:
        for i in self.layers:
            if i % 3 == 1:
                self.ssm_state[i] = (c.sbuf(f"ssmS{i}", [128, 32, 64], F32), c.sbuf(f"ssmSb{i}", [128, 32, 64], BF16),
                                     c.sbuf(f"ssmH{i}", [128, 24, 4], BF16), c.sbuf(f"ssmA{i}", [128, 32], F32))
    if any(i % 3 == 0 for i in self.layers) and self.mixers:
        self.ml_state = {}
        for i in self.layers:
            if i % 3 == 0:
                self.ml_state[i] = (c.sbuf(f"mlC{i}", [128, 4, 256], F32, split=1), c.sbuf(f"mlCb{i}", [128, 4, 256], BF16),
                                    c.sbuf(f"mln{i}", [128, 4, 128], F32, split=1), c.sbuf(f"mlnb{i}", [128, 4, 128], BF16))


Prog.alloc = _alloc2
_orig_consts = Prog.consts


def _consts2(self):
    _orig_consts(self)
    c = self.c
    c.dve.memset(self.eps_ln.all(), LN_EPS)
    c.dve.memset(self.eps_rms.all(), RMS_EPS)
    c.dve.memset(self.one_c.all(), 1.0)
    c.dve.memset(self.ones_f.all(), 1.0)
    c.dve.memset(self.ones1_b.all(), 1.0)
    c.dve.memset(self.ones256_b.all(), 1.0 / 256.0)
    c.pool.memset(self.tri.all(), 1.0)
    c.pool.affine_select(out=self.tri.all(), in_=self.tri.all(), pattern=[[1, 128]], compare_op=ALU.is_ge, fill=0.0, base=0, channel_multiplier=-1)
    c.pool.memset(self.ident_f.all(), 1.0)
    c.pool.affine_select(out=self.ident_f.all(), in_=self.ident_f.all(), pattern=[[1, 128]], compare_op=ALU.is_equal, fill=0.0, base=0, channel_multiplier=-1)
    for h in range(4):
        c.pool.tensor_copy(out=self.mask4[:, h * 128:(h + 1) * 128], in_=self.tri.all())
    c.pool.tensor_copy(out=self.ident_b.all(), in_=self.ident_f.all())
    c.pool.memset(self.maskX.all(), 1.0)
    for q in range(4):
        c.pool.affine_select(out=self.maskX[:, q * 128:(q + 1) * 128], in_=self.maskX[:, q * 128:(q + 1) * 128], pattern=[[1, 128]],
                             compare_op=(ALU.is_ge if q % 2 else ALU.is_gt), fill=0.0, base=0, channel_multiplier=-1)
    c.pool.memset(self.lowS.all(), 1.0)
    c.pool.affine_select(out=self.lowS.all(), in_=self.lowS.all(), pattern=[[-1, 128]], compare_op=ALU.is_gt, fill=0.0, base=0, channel_multiplier=1)
    c.pool.memset(self.blockones.all(), 0.0)
    c.pool.memset(self.blockones[0:64, 0:64], 1.0)
    c.pool.memset(self.blockones[64:128, 64:128], 1.0)
    c.pool.memset(self.sel2.all(), 0.0)
    c.pool.memset(self.sel2[0:64, 0:1], 1.0)
    c.pool.memset(self.sel2[64:128, 1:2], 1.0)
    c.pool.memset(self.rmask.all(), 1.0)
    for q in range(T // 128):
        c.pool.memset(self.rmask[:, q * 128:q * 128 + 1], 0.0)
    c.pool.memset(self.eps_lnx.all(), 64e-5)
    for i, t_ in self.rw_omka.items():
        col = self.pv_names[f"rw{i}_k_a"]
        c.dve.tensor_scalar(out=t_.all(), in0=self.pv[:, col:col + 8], scalar1=-1.0, scalar2=1.0, op0=ALU.mult, op1=ALU.add)
    for i, st in self.ssm_state.items():
        bcol = self.bc_names[f"ssm{i}"]
        c.act.activation(out=st[3].all(), in_=self.bc[:, bcol:bcol + 32], func=AF.Exp)
        c.dve.tensor_scalar(out=st[3].all(), in0=st[3].all(), scalar1=-1.0, scalar2=None, op0=ALU.mult)


def _reset2(self):
    c = self.c
    if self.mixers:
        for i, st in getattr(self, "ml_state", {}).items():
            for t in st:
                c.pool.memset(t.all(), 0.0)
        for i, st in self.ssm_state.items():
            for t in st[:3]:
                c.pool.memset(t.all(), 0.0)
        for i, st in self.rw_state.items():
            for t in st:
                c.pool.memset(t.all(), 0.0)


Prog.reset_state = _reset2


Prog.consts = _consts2


def run(inputs, nseq_per_core, seqlen, layers, ncores, mixers=True):
    inputs = {k: np.asarray(v) for k, v in inputs.items()}
    prog = Prog(nseq_per_core, seqlen, layers, mixers)
    nc = prog.build()
    wf = prog.wp.pack(inputs)
    pv = prog.pack_pv(inputs)
    bcv = prog.pack_bc(inputs)
    x = inputs["x"]
    in_maps = []
    for cidx in range(ncores):
        xs = x[cidx * nseq_per_core:(cidx + 1) * nseq_per_core]
        in_maps.append({"xT": np.ascontiguousarray(xs.transpose(0, 2, 1)), "wf": wf, "pv": pv, "bc": bcv})
    res = run_bass_kernel_spmd(nc, in_maps, core_ids=list(range(ncores)))
    outs = [np.asarray(r["yT"]).transpose(0, 2, 1) for r in res.results]
    return np.ascontiguousarray(np.concatenate(outs, axis=0)).astype(np.float32)


def kernel(**inputs):
    return run(inputs, 2, 4096, list(range(DEPTH)), 8)
```

```python
import contextlib
import numpy as np
import concourse.bass as bass
import concourse.mybir as mybir
from concourse.bass_utils import run_bass_kernel_spmd

F32 = mybir.dt.float32
BF16 = mybir.dt.bfloat16
AF = mybir.ActivationFunctionType
ALU = mybir.AluOpType
AX = mybir.AxisListType

D = 1024
DEPTH = 4
FH = 2816
ALPHA = (2 * DEPTH) ** 0.25
LN_EPS = 1e-5
RMS_EPS = 1e-6
T = 512
KC = D // 128
EPOCH = 30000
WSC = float(np.exp(-0.5))
NOSAME = False


class Dom:
    def __init__(self, name, sems, unit, epoch):
        self.name, self.sems, self.unit, self.epoch = name, sems, unit, epoch
        self.count = 0

    def target(self, n):
        idx = (n - 1) // self.epoch
        return self.sems[idx], ((n - 1) % self.epoch + 1) * self.unit


class Eng(Dom):
    def __init__(self, name, be, sems, is_pe=False):
        super().__init__(name, sems, 1, EPOCH)
        self.be = be
        self.seen = {}
        self.is_pe = is_pe


class Buf:
    __slots__ = ("w", "r", "name")

    def __init__(self, name):
        self.w = None
        self.r = {}
        self.name = name


class V:
    __slots__ = ("ap", "bufs")

    def __init__(self, ap, bufs):
        self.ap = ap
        self.bufs = bufs


def _flat(lists):
    d = {}
    for l in lists:
        for b in l:
            d[id(b)] = b
    return list(d.values())


class Tile:
    def __init__(self, name, ap, split=None, bsplit=None):
        self.name = name
        self.ap = ap
        self.split = split
        n = ap.shape[split] if split is not None else 1
        self.bsplit = bsplit if bsplit is not None else [[Buf(f"{name}.{i}")] for i in range(n)]
        self.bufs = _flat(self.bsplit)

    def __getitem__(self, key):
        if not isinstance(key, tuple):
            key = (key,)
        bufs = self.bufs
        if self.split is not None and len(key) > self.split:
            k = key[self.split]
            if isinstance(k, int):
                bufs = self.bsplit[k]
            elif isinstance(k, slice):
                bufs = _flat(self.bsplit[k])
        return V(self.ap[key], bufs)

    def v(self, ap, bufs=None):
        return V(ap, self.bufs if bufs is None else bufs)

    def all(self):
        return V(self.ap, self.bufs)


class EP:
    def __init__(self, ctx, eng):
        self.ctx, self.eng = ctx, eng

    def __getattr__(self, name):
        meth = getattr(self.eng.be, name)
        ctx, eng = self.ctx, self.eng

        def call(*args, **kw):
            reads, writes = [], []

            def conv(k, v):
                if isinstance(v, V):
                    (writes if k in ("out", "accum_out") else reads).extend(v.bufs)
                    return v.ap
                return v
            args2 = [conv("out" if i == 0 else "in", a) for i, a in enumerate(args)]
            kw2 = {k: conv(k, v) for k, v in kw.items()}
            return ctx.issue(eng, lambda: meth(*args2, **kw2), reads, writes, dma=(name == "dma_start"))
        return call


class Ctx:
    def __init__(self, nc, es):
        self.nc, self.es = nc, es
        nsem_eng = {"pe": 5, "dve": 4, "act": 4, "pool": 3, "sp": 2}
        bes = {"pe": nc.tensor, "dve": nc.vector, "act": nc.scalar, "pool": nc.gpsimd, "sp": nc.sync}
        self.engs = {}
        for k, n in nsem_eng.items():
            sems = [es.enter_context(nc.semaphore(f"s_{k}{i}")) for i in range(n)]
            self.engs[k] = Eng(k, bes[k], sems, is_pe=(k == "pe"))
        self.pe, self.dve, self.act, self.pool, self.sp = (EP(self, self.engs[k]) for k in ("pe", "dve", "act", "pool", "sp"))
        self.dma_doms = [Dom(f"dma{i}", [es.enter_context(nc.semaphore(f"s_dma{i}"))], 16, 4000) for i in range(40)]
        self.dma_rr = 0
        self.ninst = 0
        self.nwait = 0
        self._rr = 0

    def sbuf(self, name, shape, dtype, split=None):
        t = self.es.enter_context(self.nc.sbuf_tensor("sb_" + name, list(shape), dtype))
        return Tile(name, t[:] if hasattr(t, "__getitem__") else t.ap(), split)

    def make_arena(self, nbytes, gran=1024):
        self.ar_tile = self.sbuf("arena", [128, nbytes // 4], F32)
        self.ar_gran = gran
        self.ar_bufs = [Buf(f"ar{i}") for i in range((nbytes + gran - 1) // gran)]
        self.ar_size = nbytes
        self.ar_off = 0
        self.ar_peak = 0

    def areset(self):
        self.ar_off = 0

    def carve(self, name, shape, dtype, split=None):
        esz = 4 if dtype == F32 else 2
        free = 1
        for d in shape[1:]:
            free *= d
        nbytes = free * esz
        off = (self.ar_off + 63) // 64 * 64
        assert off + nbytes <= self.ar_size, (name, off, nbytes, self.ar_size)
        self.ar_off = off + nbytes
        self.ar_peak = max(self.ar_peak, self.ar_off)
        ap = self.ar_tile.ap[:, off // 4:(off + nbytes) // 4]
        if dtype != F32:
            ap = ap.bitcast(dtype)
        if len(shape) == 3:
            ap = ap.rearrange("p (a b) -> p a b", a=shape[1])
        elif len(shape) == 4:
            ap = ap.rearrange("p (a b c) -> p a b c", a=shape[1], b=shape[2])
        g = self.ar_gran

        def regs(o0, o1):
            return self.ar_bufs[o0 // g:(o1 + g - 1) // g]
        if split is None:
            bs = [regs(off, off + nbytes)]
        else:
            assert split == 1
            per = nbytes // shape[1]
            bs = [regs(off + i * per, off + (i + 1) * per) for i in range(shape[1])]
        return Tile(name, ap, split, bs)

    def issue(self, eng, fn, reads, writes, dma=False):
        deps = {}

        def need(d, n):
            if deps.get(d, 0) < n:
                deps[d] = n
        for b in reads:
            if b.w:
                need(*b.w)
        for b in writes:
            if b.w:
                need(*b.w)
            for d, n in b.r.items():
                need(d, n)
        if dma:
            dom = self.dma_doms[self.dma_rr]
            self.dma_rr = (self.dma_rr + 1) % len(self.dma_doms)
            if dom.count:
                need(dom, dom.count)
        else:
            dom = eng
        for d, n in deps.items():
            if d is eng and (eng.is_pe or (NOSAME and eng.name in ("dve", "act"))):
                continue
            if eng.seen.get(d, 0) >= n:
                continue
            sem, val = d.target(n)
            eng.be.wait_ge(sem, val)
            eng.seen[d] = n
            self.nwait += 1
        inst = fn()
        dom.count += 1
        n = dom.count
        sem, _ = dom.target(n)
        inst.then_inc(sem, dom.unit)
        self.ninst += 1
        for b in reads:
            b.r[dom] = n
        for b in writes:
            b.w = (dom, n)
            b.r = {}
        return inst

    def any2(self):
        self._rr ^= 1
        return self.dve if self._rr else self.pool

    def finish(self, bufs):
        sp = self.engs["sp"]
        for b in bufs:
            if b.w:
                d, n = b.w
                if sp.seen.get(d, 0) < n:
                    sem, val = d.target(n)
                    sp.be.wait_ge(sem, val)
                    sp.seen[d] = n


class WPlan:
    def __init__(self):
        self.blocks = {}
        self.src = []
        self.tot = 0

    def add(self, name, key, idx, kdim, cols):
        kc = max(1, kdim // 128)
        nb = sum(c1 - c0 for c0, c1 in cols)
        self.blocks[name] = (self.tot, kc, nb)
        self.src.append((name, key, idx, kdim, cols))
        self.tot += kc * nb
        if self.tot % 2:
            self.tot += 1

    def add_custom(self, name, kc, nb, fn):
        self.blocks[name] = (self.tot, kc, nb)
        self.src.append((name, None, fn, None, None))
        self.tot += kc * nb
        if self.tot % 2:
            self.tot += 1

    def pack(self, inputs):
        out = np.zeros((128, self.tot), np.float32)
        for name, key, idx, kdim, cols in self.src:
            off, kc, nb = self.blocks[name]
            if key is None:
                out[:, off:off + kc * nb] = idx(inputs)
                continue
            w = inputs[key][idx]
            wc = np.concatenate([w[:, c0:c1] for c0, c1 in cols], axis=1)
            if kdim < 128:
                out[:kdim, off:off + nb] = wc
            else:
                out[:, off:off + kc * nb] = wc.reshape(kc, 128, nb).transpose(1, 0, 2).reshape(128, kc * nb)
        return out


def layer_kind(i):
    return i % 3


def make_plan(layers, mixers=True):
    wp = WPlan()
    for i in layers:
        kind, j = i % 3, i // 3
        if kind == 0 and mixers:
            for b in range(6):
                wp.add(f"L{i}.ml_in{b}", "ml_w_in", j, D, [(b * 512, (b + 1) * 512)])
            wp.add(f"L{i}.ml_g", "ml_w_in", j, D, [(3072, 3080)])
            for b in range(2):
                wp.add(f"L{i}.ml_out{b}", "ml_w_out", j, D, [(b * 512, (b + 1) * 512)])
        if kind == 1 and mixers:
            for b in range(4):
                wp.add(f"L{i}.ssm_z{b}", "ssm_w_in", j, D, [(b * 512, (b + 1) * 512)])
            for b in range(6):
                wp.add(f"L{i}.ssm_x{b}", "ssm_w_in", j, D, [(2048 + b * 512, 2048 + (b + 1) * 512)])
            wp.add(f"L{i}.ssm_dt", "ssm_w_in", j, D, [(5120, 5152)])

            def cbrow(inputs, j=j):
                a = np.zeros((128, 1024), np.float32)
                for r in range(3):
                    a[32 * r] = inputs["ssm_conv_b"][j][r * 1024:(r + 1) * 1024]
                return a
            wp.add_custom(f"L{i}.ssm_cb", 1, 1024, cbrow)
            for cc in range(24):
                def cw(inputs, j=j, cc=cc):
                    a = np.zeros((128, 4, 128), np.float32)
                    w = inputs["ssm_conv_w"][j]
                    for tap in range(4):
                        a[np.arange(128), tap, np.arange(128)] = w[tap, cc * 128:(cc + 1) * 128]
                    return a.reshape(128, 512)
                wp.add_custom(f"L{i}.ssm_cw{cc}", 1, 512, cw)
            for b in range(4):
                wp.add(f"L{i}.ssm_out{b}", "ssm_w_out", j, 2048, [(b * 256, (b + 1) * 256)])
        if kind == 2 and mixers:
            wp.add(f"L{i}.rw_w1", "rw_w1", j, D, [(0, 64)])
            wp.add(f"L{i}.rw_w2", "rw_w2", j, 64, [(0, 1024)])
            for b in range(2):
                wp.add(f"L{i}.rw_r{b}", "rw_w_rkv", (j, 0), D, [(b * 512, (b + 1) * 512)])
            wp.add(f"L{i}.rw_a1", "rw_a1", j, D, [(0, 64)])
            wp.add(f"L{i}.rw_a2", "rw_a2", j, 64, [(0, 1024)])
            for b in range(2):
                wp.add(f"L{i}.rw_k{b}", "rw_w_rkv", (j, 1), D, [(b * 512, (b + 1) * 512)])
            for b in range(2):
                wp.add(f"L{i}.rw_v{b}", "rw_w_rkv", (j, 2), D, [(b * 512, (b + 1) * 512)])
            wp.add(f"L{i}.rw_g1", "rw_g1", j, D, [(0, 128)])
            wp.add(f"L{i}.rw_g2", "rw_g2", j, 128, [(0, 1024)])
            for b in range(2):
                wp.add(f"L{i}.rw_o{b}", "rw_w_out", j, D, [(b * 512, (b + 1) * 512)])
        for b in range(11):
            wp.add(f"L{i}.f_in{b}", "ffn_w_in", i, D, [(b * 256, (b + 1) * 256), (FH + b * 256, FH + (b + 1) * 256)])
        for b in range(8):
            wp.add(f"L{i}.f_out{b}", "ffn_w_out", i, FH, [(b * 128, (b + 1) * 128)])
    return wp


def layer_block_seq(i, mixers=True, nsub=2):
    seq = []
    for _ in range(nsub):
        seq += mixer_block_seq(i, mixers)
    seq += [f"L{i}.f_in{b}" for b in range(11)] + [f"L{i}.f_out{b}" for b in range(8)]
    return seq


def mixer_block_seq(i, mixers=True):
    kind = i % 3
    seq = []
    if kind == 0 and mixers:
        seq += [f"L{i}.ml_g"] + [f"L{i}.ml_in{b}" for b in range(6)] + [f"L{i}.ml_out{b}" for b in range(2)]
    if kind == 1 and mixers:
        seq += [f"L{i}.ssm_dt"] + [f"L{i}.ssm_z{b}" for b in range(4)] + [f"L{i}.ssm_x{b}" for b in range(6)] + [f"L{i}.ssm_cb"]
        seq += [f"L{i}.ssm_cw{cc}" for cc in range(24)] + [f"L{i}.ssm_out{b}" for b in range(4)]
    if kind == 2 and mixers:
        seq += [f"L{i}.rw_w1", f"L{i}.rw_w2", f"L{i}.rw_r0", f"L{i}.rw_r1", f"L{i}.rw_a1", f"L{i}.rw_a2", f"L{i}.rw_k0", f"L{i}.rw_k1",
                f"L{i}.rw_v0", f"L{i}.rw_v1", f"L{i}.rw_g1", f"L{i}.rw_g2", f"L{i}.rw_o0", f"L{i}.rw_o1"]
    return seq


SLOT = 4096
NSLOT = 4
ARENA = 85504


class Prog:
    def __init__(self, nseq, seqlen, layers, mixers=True):
        self.nseq, self.seqlen, self.layers, self.mixers = nseq, seqlen, layers, mixers
        self.ntile = seqlen // T
        self.TM = 256
        self.wp = make_plan(layers, mixers)
        self.pv_names = {}
        self.npv = 0

    def pv_add(self, name, n):
        self.pv_names[name] = self.npv
        self.npv += n

    def build(self):
        nc = bass.Bass("TRN2", target_bir_lowering=False)
        self.nc = nc
        nseq, seqlen = self.nseq, self.seqlen
        for i in self.layers:
            for j in range(2):
                self.pv_add(f"ln_g{i}.{j}", KC)
                self.pv_add(f"ln_b{i}.{j}", KC)
        self.bc_names = {}
        self.nbc = 0
        for i in self.layers:
            if i % 3 == 0 and self.mixers:
                self.pv_add(f"ml_nw{i}", KC)
                self.bc_names[f"ml_bg{i}"] = self.nbc
                self.nbc += 8
            if i % 3 == 1 and self.mixers:
                self.pv_add(f"ssm_cb{i}", 24)
                self.pv_add(f"ssm_nw{i}", 16)
                self.bc_names[f"ssm{i}"] = self.nbc
                self.nbc += 96
            if i % 3 == 2 and self.mixers:
                for nm, n in (("mix", 48), ("w0", 8), ("a0", 8), ("k_k", 8), ("k_a", 8), ("r_k", 8)):
                    self.pv_add(f"rw{i}_{nm}", n)
                self.bc_names[f"rw{i}"] = self.nbc
                self.nbc += 2048
        self.nbc = max(self.nbc, 8)
        bc_d = nc.dram_tensor("bc", [128, self.nbc], F32, kind="ExternalInput").ap()
        xT_d = nc.dram_tensor("xT", [nseq, D, seqlen], F32, kind="ExternalInput").ap()
        wf_d = nc.dram_tensor("wf", [128, self.wp.tot], F32, kind="ExternalInput").ap()
        pv_d = nc.dram_tensor("pv", [128, self.npv], F32, kind="ExternalInput").ap()
        yT_d = nc.dram_tensor("yT", [nseq, D, seqlen], F32, kind="ExternalOutput").ap()
        wb_d = nc.dram_tensor("wb", [128, self.wp.tot], BF16, kind="Internal").ap()
        with contextlib.ExitStack() as es:
            c = Ctx(nc, es)
            self.c = c
            self.xT_t = Tile("xT", xT_d)
            self.yT_t = Tile("yT", yT_d)
            self.wf_t = Tile("wf", wf_d)
            self.pv_dt = Tile("pvd", pv_d)
            self.wb_t = Tile("wb", wb_d)
            self.bc_dt = Tile("bcd", bc_d)
            self.alloc()
            self.prepass()
            self.consts()
            self.useq = []
            for s in range(nseq):
                for ti in range(self.ntile):
                    for i in self.layers:
                        self.useq += layer_block_seq(i, self.mixers)
            self.upos = 0
            self.uissued = 0
            for s in range(nseq):
                self.reset_state()
                for ti in range(self.ntile):
                    self.tile(s, ti)
            assert self.upos == len(self.useq)
            c.finish(self.yT_t.bufs)
            print(f"[build] instructions={c.ninst} waits={c.nwait} arena_peak={c.ar_peak}")
        return nc

    def alloc(self):
        c = self.c
        ps = self.c.es.enter_context(self.nc.psum_tensor("psum_all", [128, 8, 512], F32))
        self.ps = Tile("ps", ps[:] if hasattr(ps, "__getitem__") else ps.ap(), split=1)
        self.bank_rr = 0
        self.x = c.sbuf("x", [128, KC, T], F32, split=1)
        self.xb = c.sbuf("xb", [128, KC, T], BF16, split=1)
        self.s = c.sbuf("s", [128, KC, T], F32, split=1)
        self.wslot = [c.sbuf(f"wslot{i}", [128, SLOT], BF16) for i in range(NSLOT)]
        self.pv = c.sbuf("pv", [128, self.npv], F32)
        self.ones_b = c.sbuf("ones_b", [128, 128], BF16)
        c.make_arena(ARENA)

    def bank(self):
        b = self.bank_rr
        self.bank_rr = (self.bank_rr + 1) % 8
        return b

    def prepass(self):
        c = self.c
        tot = self.wp.tot
        i = 0
        off = 0
        engs = [c.dve, c.act]
        c.areset()
        stg_f = [c.carve(f"stgf{q}", [128, SLOT], F32) for q in range(2)]
        stg_b = [c.carve(f"stgb{q}", [128, SLOT], BF16) for q in range(2)]
        while off < tot:
            sz = min(SLOT, tot - off)
            f, b = stg_f[i % 2], stg_b[i % 2]
            c.sp.dma_start(out=f[:, 0:sz], in_=self.wf_t[:, off:off + sz])
            e = engs[i % 2]
            if e is c.act:
                e.activation(out=b[:, 0:sz], in_=f[:, 0:sz], func=AF.Copy)
            else:
                e.tensor_copy(out=b[:, 0:sz], in_=f[:, 0:sz])
            c.sp.dma_start(out=self.wb_t[:, off:off + sz], in_=b[:, 0:sz])
            off += sz
            i += 1
        c.sp.dma_start(out=self.pv.all(), in_=self.pv_dt.all())
        c.sp.dma_start(out=self.bc.all(), in_=self.bc_dt.all())

    def consts(self):
        c = self.c
        c.dve.memset(self.ones_b.all(), 1.0 / D)

    def reset_state(self):
        pass

    def _issue_load(self, u):
        name = self.useq[u]
        off, kc, nb = self.wp.blocks[name]
        slot = self.wslot[u % NSLOT]
        self.c.sp.dma_start(out=slot[:, 0:kc * nb], in_=self.wb_t[:, off:off + kc * nb])

    def weight(self, name):
        assert self.useq[self.upos] == name, (self.useq[self.upos], name)
        while self.uissued < min(len(self.useq), self.upos + NSLOT):
            self._issue_load(self.uissued)
            self.uissued += 1
        off, kc, nb = self.wp.blocks[name]
        slot = self.wslot[self.upos % NSLOT]
        self.upos += 1
        ap = slot.ap[:, 0:kc * nb].rearrange("p (k n) -> p k n", k=kc)
        return V(ap, slot.bufs)

    def tile(self, s, ti):
        c = self.c
        t0 = ti * T
        src = self.xT_t.ap[s].rearrange("(k p) t -> p k t", p=128)[:, :, t0:t0 + T]
        c.sp.dma_start(out=self.x.all(), in_=self.xT_t.v(src))
        for kc in range(KC):
            if kc % 2:
                c.act.activation(out=self.xb[:, kc, :], in_=self.x[:, kc, :], func=AF.Copy)
            else:
                c.dve.tensor_copy(out=self.xb[:, kc, :], in_=self.x[:, kc, :])
        for i in self.layers:
            if self.mixers:
                self.mixer(i, s, ti)
                self.layernorm(i, 0)
            self.ffn(i)
            self.layernorm(i, 1)
        dst = self.yT_t.ap[s].rearrange("(k p) t -> p k t", p=128)[:, :, t0:t0 + T]
        c.sp.dma_start(out=self.yT_t.v(dst), in_=self.x.all())

    def pvv(self, name, k):
        col = self.pv_names[name] + k
        return self.pv[:, col:col + 1]

    def layernorm(self, i, j):
        cx = self.c
        cx.areset()
        self.sqb = cx.carve("sqb", [128, KC, T], BF16, split=1)
        self.sb = cx.carve("sb", [128, KC, T], BF16, split=1)
        self.stat = cx.carve("stat", [128, 4, T], F32, split=1)
        for kc in range(KC):
            cx.act.activation(out=self.sqb[:, kc, :], in_=self.s[:, kc, :], func=AF.Square)
            cx.act.activation(out=self.sb[:, kc, :], in_=self.s[:, kc, :], func=AF.Copy)
        b1, b2 = self.bank(), self.bank()
        for kc in range(KC):
            cx.pe.matmul(self.ps[:, b1, :], lhsT=self.ones_b.all(), rhs=self.sb[:, kc, :], start=(kc == 0), stop=(kc == KC - 1))
        for kc in range(KC):
            cx.pe.matmul(self.ps[:, b2, :], lhsT=self.ones_b.all(), rhs=self.sqb[:, kc, :], start=(kc == 0), stop=(kc == KC - 1))
        mean, var, rstd, tmp = (self.stat[:, q, :] for q in range(4))
        cx.dve.tensor_copy(out=mean, in_=self.ps[:, b1, :])
        cx.dve.tensor_tensor(out=tmp, in0=mean, in1=mean, op=ALU.mult)
        cx.dve.tensor_tensor(out=var, in0=self.ps[:, b2, :], in1=tmp, op=ALU.subtract)
        cx.act.activation(out=rstd, in_=var, func=AF.Sqrt, bias=self.eps_ln[:, 0:1], scale=1.0)
        cx.dve.reciprocal(out=rstd, in_=rstd)
        for kc in range(KC):
            cx.dve.tensor_tensor(out=self.s[:, kc, :], in0=self.s[:, kc, :], in1=mean, op=ALU.subtract)
            cx.dve.tensor_tensor(out=self.s[:, kc, :], in0=self.s[:, kc, :], in1=rstd, op=ALU.mult)
            cx.act.activation(out=self.x[:, kc, :], in_=self.s[:, kc, :], func=AF.Identity,
                              bias=self.pvv(f"ln_b{i}.{j}", kc), scale=self.pvv(f"ln_g{i}.{j}", kc))
            cx.act.activation(out=self.xb[:, kc, :], in_=self.s[:, kc, :], func=AF.Identity,
                              bias=self.pvv(f"ln_b{i}.{j}", kc), scale=self.pvv(f"ln_g{i}.{j}", kc))

    def ffn(self, i):
        cx = self.c
        nfc = FH // 128
        cx.areset()
        self.hm = cx.carve("hm", [128, nfc, T], BF16, split=1)
        self.gsil = [cx.carve(f"gsil{q}", [128, T], F32) for q in range(2)]
        for b in range(11):
            w = self.weight(f"L{i}.f_in{b}")
            for h in range(2):
                m = b * 2 + h
                bg, bu = self.bank(), self.bank()
                for kc in range(KC):
                    cx.pe.matmul(self.ps[:, bg, :], lhsT=V(w.ap[:, kc, h * 128:(h + 1) * 128], w.bufs), rhs=self.xb[:, kc, :],
                                 start=(kc == 0), stop=(kc == KC - 1))
                for kc in range(KC):
                    cx.pe.matmul(self.ps[:, bu, :], lhsT=V(w.ap[:, kc, 256 + h * 128:256 + (h + 1) * 128], w.bufs), rhs=self.xb[:, kc, :],
                                 start=(kc == 0), stop=(kc == KC - 1))
                g = self.gsil[m % 2]
                cx.act.activation(out=g.all(), in_=self.ps[:, bg, :], func=AF.Silu)
                cx.dve.tensor_tensor(out=self.hm[:, m, :], in0=self.ps[:, bu, :], in1=g.all(), op=ALU.mult)
        for mo in range(KC):
            w = self.weight(f"L{i}.f_out{mo}")
            bo = self.bank()
            for m in range(nfc):
                cx.pe.matmul(self.ps[:, bo, :], lhsT=V(w.ap[:, m, :], w.bufs), rhs=self.hm[:, m, :], start=(m == 0), stop=(m == nfc - 1))
            cx.dve.scalar_tensor_tensor(out=self.s[:, mo, :], in0=self.x[:, mo, :], scalar=float(ALPHA), in1=self.ps[:, bo, :],
                                        op0=ALU.mult, op1=ALU.add)

    def mixer(self, i, s, ti):
        kind = i % 3
        for sub in range(T // self.TM):
            t0 = sub * self.TM
            self.xbv = Tile("xbv", self.xb.ap[:, :, t0:t0 + self.TM], 1, self.xb.bsplit)
            self.xv = Tile("xv", self.x.ap[:, :, t0:t0 + self.TM], 1, self.x.bsplit)
            self.sv = Tile("sv", self.s.ap[:, :, t0:t0 + self.TM], 1, self.s.bsplit)
            if kind == 0:
                self.mlstm(i)
            elif kind == 1:
                self.mamba(i)
            else:
                self.rwkv(i)

    def rwkv(self, i):
        TM = self.TM
        xb_t, x_t, s_t = self.xbv, self.xv, self.sv
        c = self.c
        ps = self.ps
        NCH = TM // 128
        ST, STb, xlast = self.rw_state[i]
        bcol = self.bc_names[f"rw{i}"]
        c.areset()
        AR = c.carve("AR", [128, 8, NCH, 256], BF16, split=1)
        BtT = c.carve("BtT", [128, 8, TM], BF16, split=1)
        KtT = c.carve("KtT", [128, 8, TM], BF16, split=1)
        rk_tok = c.carve("rk_tok", [128, NCH, 16], F32)
        WL = c.carve("WL", [128, NCH, 8], F32)
        offM = c.ar_off
        cw = c.carve("cw", [128, 8, TM], F32, split=1)
        asg = c.carve("asg", [128, 8, TM], BF16, split=1)
        xx = c.carve("xx", [128, 8, TM], BF16, split=1)
        offP = offM + 5 * (8 * TM * 2)
        assert c.ar_off <= offP
        c.ar_off = offP
        xj = c.carve("xj", [128, 8, TM], BF16, split=1)
        lo = [c.carve(f"lo{q}", [128, TM], BF16) for q in range(2)]
        tf = [c.carve(f"tf{q}", [128, TM], F32) for q in range(5)]
        sqk = c.carve("sqk", [128, TM], BF16)
        pr = c.carve("pr", [128, TM], BF16)
        pname = lambda nm, k: self.pvv(f"rw{i}_{nm}", k)

        def mix(j):
            for kc in range(KC):
                c.dve.scalar_tensor_tensor(out=xj[:, kc, :], in0=xx[:, kc, :], scalar=pname("mix", j * 8 + kc), in1=x_t[:, kc, :], op0=ALU.mult, op1=ALU.add)

        v4 = lambda t_: V(t_.ap.rearrange("p (c t) -> p c t", c=NCH), t_.bufs)
        for kc in range(KC):
            c.dve.tensor_tensor(out=xx[:, kc, 1:TM], in0=x_t[:, kc, 0:TM - 1], in1=x_t[:, kc, 1:TM], op=ALU.subtract)
            c.dve.tensor_tensor(out=xx[:, kc, 0:1], in0=xlast[:, kc, :], in1=x_t[:, kc, 0:1], op=ALU.subtract)
        c.pool.tensor_copy(out=xlast.all(), in_=V(x_t.ap[:, :, TM - 1:TM], x_t.bufs))
        mix(1)
        w = self.weight(f"L{i}.rw_w1")
        b = self.bank()
        for kc in range(KC):
            c.pe.matmul(ps[0:64, b, 0:TM], lhsT=V(w.ap[:, kc, :], w.bufs), rhs=xj[:, kc, :], start=(kc == 0), stop=(kc == KC - 1))
        c.act.activation(out=lo[0][0:64, :], in_=ps[0:64, b, 0:TM], func=AF.Tanh)
        w = self.weight(f"L{i}.rw_w2")
        for m in range(8):
            b = self.bank()
            c.pe.matmul(ps[:, b, 0:TM], lhsT=V(w.ap[0:64, 0, m * 128:(m + 1) * 128], w.bufs), rhs=lo[0][0:64, :], start=True, stop=True)
            c.act.activation(out=tf[m % 2].all(), in_=ps[:, b, 0:TM], func=AF.Sigmoid, bias=pname("w0", m), scale=1.0)
            c.dve.tensor_tensor_scan(out=cw[:, m, :], data0=self.rmask[:, 0:TM], data1=tf[m % 2].all(), initial=0.0, op0=ALU.mult, op1=ALU.add)
            c.act.activation(out=V(WL.ap[:, :, m], WL.bufs), in_=V(cw.ap[:, m, 127::128], cw.bsplit[m]), func=AF.Exp, scale=-WSC)
        mix(0)
        for rb in range(2):
            w = self.weight(f"L{i}.rw_r{rb}")
            for m in range(4):
                ko = rb * 4 + m
                b = self.bank()
                for kc in range(KC):
                    c.pe.matmul(ps[:, b, 0:TM], lhsT=V(w.ap[:, kc, m * 128:(m + 1) * 128], w.bufs), rhs=xj[:, kc, :], start=(kc == 0), stop=(kc == KC - 1))
                ex = tf[2 + ko % 2]
                c.act.activation(out=ex.all(), in_=cw[:, ko, :], func=AF.Exp, scale=-WSC)
                c.dve.tensor_tensor(out=V(AR.ap[:, ko, :, 128:256], AR.bsplit[ko]), in0=V(ps.ap[:, b, 0:TM].rearrange("p (c t) -> p c t", c=NCH), ps.bsplit[b]), in1=v4(ex), op=ALU.mult)
        mix(4)
        w = self.weight(f"L{i}.rw_a1")
        b = self.bank()
        for kc in range(KC):
            c.pe.matmul(ps[0:64, b, 0:TM], lhsT=V(w.ap[:, kc, :], w.bufs), rhs=xj[:, kc, :], start=(kc == 0), stop=(kc == KC - 1))
        c.act.activation(out=lo[1][0:64, :], in_=ps[0:64, b, 0:TM], func=AF.Copy)
        w = self.weight(f"L{i}.rw_a2")
        for m in range(8):
            b = self.bank()
            c.pe.matmul(ps[:, b, 0:TM], lhsT=V(w.ap[0:64, 0, m * 128:(m + 1) * 128], w.bufs), rhs=lo[1][0:64, :], start=True, stop=True)
            c.act.activation(out=asg[:, m, :], in_=ps[:, b, 0:TM], func=AF.Sigmoid, bias=pname("a0", m), scale=1.0)
        mix(2)
        for kb in range(2):
            w = self.weight(f"L{i}.rw_k{kb}")
            for m in range(4):
                ko = kb * 4 + m
                b = self.bank()
                for kc in range(KC):
                    c.pe.matmul(ps[:, b, 0:TM], lhsT=V(w.ap[:, kc, m * 128:(m + 1) * 128], w.bufs), rhs=xj[:, kc, :], start=(kc == 0), stop=(kc == KC - 1))
                kkr, ex, em, t1, kkn = tf
                c.act.activation(out=kkr.all(), in_=ps[:, b, 0:TM], func=AF.Copy, scale=pname("k_k", ko))
                c.act.activation(out=sqk.all(), in_=kkr.all(), func=AF.Square)
                b2 = self.bank()
                c.pe.matmul(ps[:, b2, 0:TM], lhsT=self.blockones.all(), rhs=sqk.all(), start=True, stop=True)
                c.dve.tensor_scalar(out=kkn.all(), in0=ps[:, b2, 0:TM], scalar1=1e-24, scalar2=None, op0=ALU.max)
                c.act.activation(out=kkn.all(), in_=kkn.all(), func=AF.Sqrt)
                c.dve.reciprocal(out=kkn.all(), in_=kkn.all())
                c.dve.tensor_tensor(out=kkn.all(), in0=kkn.all(), in1=kkr.all(), op=ALU.mult)
                c.act.activation(out=ex.all(), in_=cw[:, ko, :], func=AF.Exp, scale=-WSC)
                c.act.activation(out=em.all(), in_=cw[:, ko, :], func=AF.Exp, scale=WSC)
                kk4, ex4 = v4(kkn), v4(ex)
                c.dve.scalar_tensor_tensor(out=V(AR.ap[:, ko, :, 1:128], AR.bsplit[ko]), in0=V(kk4.ap[:, :, 1:128], kkn.bufs), scalar=-1.0, in1=V(ex4.ap[:, :, 0:127], ex.bufs),
                                           op0=ALU.mult, op1=ALU.mult)
                c.pool.tensor_scalar(out=V(AR.ap[:, ko, :, 0:1], AR.bsplit[ko]), in0=V(kk4.ap[:, :, 0:1], kkn.bufs), scalar1=-1.0, scalar2=None, op0=ALU.mult)
                c.dve.tensor_tensor(out=kkn.all(), in0=kkn.all(), in1=asg[:, ko, :], op=ALU.mult)
                c.dve.tensor_tensor(out=BtT[:, ko, :], in0=kkn.all(), in1=em.all(), op=ALU.mult)
                c.dve.tensor_scalar(out=t1.all(), in0=asg[:, ko, :], scalar1=pname("k_a", ko), scalar2=self.rw_omka[i][:, ko:ko + 1], op0=ALU.mult, op1=ALU.add)
                c.dve.tensor_tensor(out=t1.all(), in0=ps[:, b, 0:TM], in1=t1.all(), op=ALU.mult)
                c.dve.tensor_tensor(out=KtT[:, ko, :], in0=t1.all(), in1=em.all(), op=ALU.mult)
                c.dve.scalar_tensor_tensor(out=v4(pr), in0=V(AR.ap[:, ko, :, 128:256], AR.bsplit[ko]), scalar=pname("r_k", ko), in1=v4(V(KtT.ap[:, ko, :], KtT.bsplit[ko])),
                                           op0=ALU.mult, op1=ALU.mult)
                brk = self.bank()
                for ch in range(NCH):
                    c.pe.matmul(ps[:, brk, ch * 2:ch * 2 + 2], lhsT=pr[:, ch * 128:(ch + 1) * 128], rhs=self.sel2.all(), start=True, stop=True)
                c.dve.tensor_copy(out=V(rk_tok.ap[:, :, ko * 2:ko * 2 + 2], rk_tok.bufs), in_=V(ps.ap[:, brk, 0:NCH * 2].rearrange("p (c h) -> p c h", c=NCH), ps.bsplit[brk]))
        c.ar_off = offM
        Vt = c.carve("Vt", [128, NCH, 1024], BF16, split=1)
        gT = c.carve("gT", [128, 8, TM], BF16, split=1)
        Btok = c.carve("Btok", [128, NCH, 1024], BF16, split=1)
        Ktok = c.carve("Ktok", [128, NCH, 1024], BF16, split=1)
        yg = c.carve("yg", [128, 8, TM], BF16, split=1)
        assert c.ar_off <= offP
        mix(3)
        for vb in range(2):
            w = self.weight(f"L{i}.rw_v{vb}")
            for ch in range(NCH):
                b = self.bank()
                for kc in range(KC):
                    c.pe.matmul(ps[:, b, :], lhsT=xj[:, kc, ch * 128:(ch + 1) * 128], rhs=V(w.ap[:, kc, :], w.bufs), start=(kc == 0), stop=(kc == KC - 1))
                if (ch + vb) % 2:
                    c.act.activation(out=Vt[:, ch, vb * 512:(vb + 1) * 512], in_=ps[:, b, :], func=AF.Copy)
                else:
                    c.dve.tensor_copy(out=Vt[:, ch, vb * 512:(vb + 1) * 512], in_=ps[:, b, :])
        mix(5)
        w = self.weight(f"L{i}.rw_g1")
        b = self.bank()
        for kc in range(KC):
            c.pe.matmul(ps[:, b, 0:TM], lhsT=V(w.ap[:, kc, :], w.bufs), rhs=xj[:, kc, :], start=(kc == 0), stop=(kc == KC - 1))
        c.act.activation(out=lo[0].all(), in_=ps[:, b, 0:TM], func=AF.Sigmoid)
        w = self.weight(f"L{i}.rw_g2")
        for m in range(8):
            b = self.bank()
            c.pe.matmul(ps[:, b, 0:TM], lhsT=V(w.ap[:, 0, m * 128:(m + 1) * 128], w.bufs), rhs=lo[0].all(), start=True, stop=True)
            if m % 2:
                c.act.activation(out=gT[:, m, :], in_=ps[:, b, 0:TM], func=AF.Copy)
            else:
                c.dve.tensor_copy(out=gT[:, m, :], in_=ps[:, b, 0:TM])
        for src, dst in ((BtT, Btok), (KtT, Ktok)):
            for ch in range(NCH):
                for half in range(2):
                    bT = self.bank()
                    pb = ps.ap[:, bT, :].bitcast(BF16)
                    for r in range(4):
                        kc = half * 4 + r
                        c.pe.transpose(V(pb[:, r * 128:(r + 1) * 128], ps.bsplit[bT]), src[:, kc, ch * 128:(ch + 1) * 128], self.ident_b.all())
                    e = c.act if half else c.dve
                    if half:
                        c.act.activation(out=dst[:, ch, half * 512:(half + 1) * 512], in_=V(pb[:, 0:512], ps.bsplit[bT]), func=AF.Copy)
                    else:
                        c.dve.tensor_copy(out=dst[:, ch, half * 512:(half + 1) * 512], in_=V(pb[:, 0:512], ps.bsplit[bT]))
        c.ar_off = offP
        SETS = []
        for q_ in range(2):
            SETS.append(dict(XT=c.carve(f"XT{q_}", [128, 8, 4, 128], BF16),
                             Xp=[c.carve(f"Xp{q_}{q}", [128, 8, 128], BF16) for q in range(2)],
                             XTp=[c.carve(f"XTp{q_}{q}", [128, 8, 128], BF16) for q in range(2)],
                             TTp=[c.carve(f"TTp{q_}{q}", [128, 8, 128], BF16) for q in range(2)]))
        RHSb = c.carve("RHSb", [128, 8, 64], BF16)
        Ub = c.carve("Ub", [128, 8, 64], BF16)
        Ysb = c.carve("Ysb", [128, 8, 64], F32)
        Ysq = c.carve("Ysq", [128, 8, 64], F32)
        yv = c.carve("yv", [128, 8, 64], BF16)
        st8 = {k: c.carve("st8_" + k, [128, 8], F32) for k in ("s1", "s2", "mean", "var", "rstd")}

        def hkf(h0):
            return lambda hh: ((h0 + hh) // 2, ((h0 + hh) % 2) * 64)

        def chain(ch, half, S):
            cs = slice(ch * 128, (ch + 1) * 128)
            h0 = half * 8
            hk = hkf(h0)
            XT, Xp, XTp, TTp = S["XT"], S["Xp"], S["XTp"], S["TTp"]
            for hh in range(8):
                kc, ba = hk(hh)
                bX = self.bank()
                arR = V(AR.ap[ba:ba + 64, kc, ch, :], AR.bsplit[kc])
                c.pe.matmul(ps[:, bX, 0:256], lhsT=V(BtT.ap[ba:ba + 64, kc, cs], BtT.bsplit[kc]), rhs=arR, start=True, stop=True)
                c.pe.matmul(ps[:, bX, 256:512], lhsT=V(KtT.ap[ba:ba + 64, kc, cs], KtT.bsplit[kc]), rhs=arR, start=True, stop=True)
                c.dve.tensor_tensor(out=V(XT.ap[:, hh, :, :].rearrange("p a t -> p (a t)"), XT.bufs), in0=ps[:, bX, :], in1=self.maskX.all(), op=ALU.mult)
                if hh % 2:
                    yield
            for q in range(2):
                bM = self.bank()
                for r in range(4):
                    hh = r * 2 + q
                    kc, ba = hk(hh)
                    c.pe.matmul(ps[:, bM, r * 128:(r + 1) * 128], lhsT=V(AR.ap[ba:ba + 64, kc, ch, 0:128], AR.bsplit[kc]), rhs=V(BtT.ap[ba:ba + 64, kc, cs], BtT.bsplit[kc]), start=True, stop=True)
                c.dve.tensor_tensor(out=V(Xp[0].ap[:, q::2, :], Xp[0].bufs), in0=V(ps.ap[:, bM, :].rearrange("p (h t) -> p h t", h=4), ps.bsplit[bM]),
                                    in1=V(self.lowS.ap.unsqueeze(1).to_broadcast([128, 4, 128]), self.lowS.bufs), op=ALU.mult)
            c.dve.tensor_tensor(out=TTp[0].all(), in0=V(XT.ap[:, :, 0, :], XT.bufs), in1=V(self.ident_b.ap.unsqueeze(1).to_broadcast([128, 8, 128]), self.ident_b.bufs), op=ALU.add)
            yield
            xcur, xtcur, ttcur = Xp[0], None, TTp[0]
            for lvl in range(1, 7):
                xn, xtn, ttn = Xp[lvl % 2], XTp[lvl % 2], TTp[lvl % 2]

                def xt_of(hh, xtcur=xtcur):
                    if xtcur is None:
                        return V(XT.ap[:, hh, 0, :], XT.bufs)
                    return V(xtcur.ap[:, hh, :], xtcur.bufs)
                bXs = [self.bank(), self.bank()]
                for hh in range(8):
                    c.pe.matmul(ps[:, bXs[hh // 4], (hh % 4) * 128:(hh % 4 + 1) * 128], lhsT=xt_of(hh), rhs=V(xcur.ap[:, hh, :], xcur.bufs), start=True, stop=True)
                if lvl < 6:
                    bTs = [self.bank(), self.bank()]
                    for hh in range(8):
                        c.pe.matmul(ps[:, bTs[hh // 4], (hh % 4) * 128:(hh % 4 + 1) * 128], lhsT=V(xcur.ap[:, hh, :], xcur.bufs), rhs=xt_of(hh), start=True, stop=True)
                for q in range(2):
                    src_ = V(ps.ap[:, bXs[q], :].rearrange("p (h t) -> p h t", h=4), ps.bsplit[bXs[q]])
                    if q:
                        c.act.activation(out=V(xn.ap[:, q * 4:(q + 1) * 4, :], xn.bufs), in_=src_, func=AF.Copy)
                    else:
                        c.dve.tensor_copy(out=V(xn.ap[:, q * 4:(q + 1) * 4, :], xn.bufs), in_=src_)
                if lvl < 6:
                    for q in range(2):
                        src_ = V(ps.ap[:, bTs[q], :].rearrange("p (h t) -> p h t", h=4), ps.bsplit[bTs[q]])
                        if q:
                            c.dve.tensor_copy(out=V(xtn.ap[:, q * 4:(q + 1) * 4, :], xtn.bufs), in_=src_)
                        else:
                            c.act.activation(out=V(xtn.ap[:, q * 4:(q + 1) * 4, :], xtn.bufs), in_=src_, func=AF.Copy)
                yield
                bAs = [self.bank(), self.bank()]
                for hh in range(8):
                    c.pe.matmul(ps[:, bAs[hh // 4], (hh % 4) * 128:(hh % 4 + 1) * 128], lhsT=V(xn.ap[:, hh, :], xn.bufs), rhs=V(ttcur.ap[:, hh, :], ttcur.bufs), start=True, stop=True)
                for q in range(2):
                    c.dve.tensor_tensor(out=V(ttn.ap[:, q * 4:(q + 1) * 4, :], ttn.bufs), in0=V(ps.ap[:, bAs[q], :].rearrange("p (h t) -> p h t", h=4), ps.bsplit[bAs[q]]),
                                        in1=V(ttcur.ap[:, q * 4:(q + 1) * 4, :], ttcur.bufs), op=ALU.add)
                xcur, ttcur, xtcur = xn, ttn, xtn
                yield
            S["ttf"] = ttcur

        def seq(ch, half, S):
            cs = slice(ch * 128, (ch + 1) * 128)
            h0 = half * 8
            hk = hkf(h0)
            XT, ttcur = S["XT"], S["ttf"]
            bR = [self.bank(), self.bank()]
            for hh in range(8):
                kc, ba = hk(hh)
                h = h0 + hh
                o = ps[:, bR[hh % 2], (hh // 2) * 64:(hh // 2 + 1) * 64]
                c.pe.matmul(o, lhsT=V(AR.ap[ba:ba + 64, kc, ch, 0:128], AR.bsplit[kc]), rhs=V(STb.ap[ba:ba + 64, kc, :], STb.bufs), start=True, stop=False)
                c.pe.matmul(o, lhsT=V(XT.ap[:, hh, 2, :], XT.bufs), rhs=Vt[:, ch, h * 64:(h + 1) * 64], start=False, stop=True)
            for q in range(2):
                src_ = V(ps.ap[:, bR[q], 0:256].rearrange("p (r v) -> p r v", r=4), ps.bsplit[bR[q]])
                if q:
                    c.act.activation(out=V(RHSb.ap[:, q::2, :], RHSb.bufs), in_=src_, func=AF.Copy)
                else:
                    c.dve.tensor_copy(out=V(RHSb.ap[:, q::2, :], RHSb.bufs), in_=src_)
            yield
            bU = self.bank()
            for hh in range(8):
                c.pe.matmul(ps[:, bU, hh * 64:(hh + 1) * 64], lhsT=V(ttcur.ap[:, hh, :], ttcur.bufs), rhs=V(RHSb.ap[:, hh, :], RHSb.bufs), start=True, stop=True)
            c.dve.tensor_copy(out=V(Ub.ap.rearrange("p h v -> p (h v)"), Ub.bufs), in_=ps[:, bU, :])
            yield
            bYy = [self.bank(), self.bank()]
            for hh in range(8):
                kc, ba = hk(hh)
                h = h0 + hh
                o = ps[:, bYy[hh % 2], (hh // 2) * 64:(hh // 2 + 1) * 64]
                c.pe.matmul(o, lhsT=V(AR.ap[ba:ba + 64, kc, ch, 128:256], AR.bsplit[kc]), rhs=V(STb.ap[ba:ba + 64, kc, :], STb.bufs), start=True, stop=False)
                c.pe.matmul(o, lhsT=V(XT.ap[:, hh, 1, :], XT.bufs), rhs=V(Ub.ap[:, hh, :], Ub.bufs), start=False, stop=False)
                c.pe.matmul(o, lhsT=V(XT.ap[:, hh, 3, :], XT.bufs), rhs=Vt[:, ch, h * 64:(h + 1) * 64], start=False, stop=True)
            for q in range(2):
                c.act.activation(out=V(Ysb.ap[:, q::2, :], Ysb.bufs), in_=V(ps.ap[:, bYy[q], 0:256].rearrange("p (r v) -> p r v", r=4), ps.bsplit[bYy[q]]), func=AF.Copy)
            yield
            bSs = self.bank()
            for hh in range(8):
                kc, ba = hk(hh)
                h = h0 + hh
                o = ps[:, bSs, hh * 64:(hh + 1) * 64]
                c.pe.matmul(o, lhsT=Btok[:, ch, kc * 128:(kc + 1) * 128], rhs=V(Ub.ap[:, hh, :], Ub.bufs), start=True, stop=False)
                c.pe.matmul(o, lhsT=Ktok[:, ch, kc * 128:(kc + 1) * 128], rhs=Vt[:, ch, h * 64:(h + 1) * 64], start=False, stop=True)
            kc0 = h0 // 2
            stv = V(ST.ap[:, kc0:kc0 + 4, :], ST.bufs)
            c.dve.tensor_tensor(out=stv, in0=stv, in1=V(WL.ap[:, ch, kc0:kc0 + 4].unsqueeze(2).to_broadcast([128, 4, 64]), WL.bufs), op=ALU.mult)
            for hh in range(8):
                kc, ba = hk(hh)
                sv = V(ST.ap[ba:ba + 64, kc, :], ST.bufs)
                c.dve.scalar_tensor_tensor(out=sv, in0=V(ps.ap[ba:ba + 64, bSs, hh * 64:(hh + 1) * 64], ps.bsplit[bSs]), scalar=V(WL.ap[ba:ba + 64, ch, kc:kc + 1], WL.bufs), in1=sv,
                                           op0=ALU.mult, op1=ALU.add)
            c.act.activation(out=V(STb.ap[:, kc0:kc0 + 4, :], STb.bufs), in_=stv, func=AF.Copy)
            yield
            c.act.activation(out=Ysq.all(), in_=Ysb.all(), func=AF.Square)
            c.dve.tensor_reduce(out=st8["s1"].all(), in_=Ysb.all(), axis=AX.X, op=ALU.add)
            c.dve.tensor_reduce(out=st8["s2"].all(), in_=Ysq.all(), axis=AX.X, op=ALU.add)
            c.dve.tensor_scalar(out=st8["mean"].all(), in0=st8["s1"].all(), scalar1=float(1.0 / 64.0), scalar2=None, op0=ALU.mult)
            c.dve.tensor_tensor(out=st8["var"].all(), in0=st8["mean"].all(), in1=st8["mean"].all(), op=ALU.mult)
            c.dve.scalar_tensor_tensor(out=st8["var"].all(), in0=st8["s2"].all(), scalar=float(1.0 / 64.0), in1=st8["var"].all(), op0=ALU.mult, op1=ALU.subtract)
            c.act.activation(out=st8["rstd"].all(), in_=st8["var"].all(), func=AF.Sqrt, bias=self.eps_lnx[:, 0:1], scale=1.0)
            c.dve.reciprocal(out=st8["rstd"].all(), in_=st8["rstd"].all())
            yield
            bc8 = lambda t_: V(t_.ap.unsqueeze(2).to_broadcast([128, 8, 64]), t_.bufs)
            c.dve.tensor_tensor(out=Ysb.all(), in0=Ysb.all(), in1=bc8(st8["mean"]), op=ALU.subtract)
            c.dve.tensor_tensor(out=Ysb.all(), in0=Ysb.all(), in1=bc8(st8["rstd"]), op=ALU.mult)
            f0 = h0 * 64
            lw_ = V(self.bc.ap[:, bcol + f0:bcol + f0 + 512].rearrange("p (h v) -> p h v", h=8), self.bc.bufs)
            lb_ = V(self.bc.ap[:, bcol + 1024 + f0:bcol + 1024 + f0 + 512].rearrange("p (h v) -> p h v", h=8), self.bc.bufs)
            c.dve.tensor_tensor(out=Ysb.all(), in0=Ysb.all(), in1=lw_, op=ALU.mult)
            c.dve.tensor_tensor(out=Ysb.all(), in0=Ysb.all(), in1=lb_, op=ALU.add)
            v3 = V(Vt.ap[:, ch, f0:f0 + 512].rearrange("p (h v) -> p h v", h=8), Vt.bsplit[ch])
            c.pool.tensor_tensor(out=Ysq.all(), in0=v3, in1=V(rk_tok.ap[:, ch, h0:h0 + 8].unsqueeze(2).to_broadcast([128, 8, 64]), rk_tok.bufs), op=ALU.mult)
            c.dve.tensor_tensor(out=yv.all(), in0=Ysb.all(), in1=Ysq.all(), op=ALU.add)
            yield
            bT = self.bank()
            pb = ps.ap[:, bT, :].bitcast(BF16)
            yvf = yv.ap.rearrange("p h v -> p (h v)")
            for r in range(4):
                c.pe.transpose(V(pb[:, r * 128:(r + 1) * 128], ps.bsplit[bT]), V(yvf[:, r * 128:(r + 1) * 128], yv.bufs), self.ident_b.all())
            c.dve.tensor_tensor(out=yg[:, kc0:kc0 + 4, cs], in0=V(pb[:, 0:512].rearrange("p (k t) -> p k t", k=4), ps.bsplit[bT]), in1=gT[:, kc0:kc0 + 4, cs], op=ALU.mult)

        def drain(*gens):
            gens = [g for g in gens if g is not None]
            while gens:
                for g in list(gens):
                    try:
                        next(g)
                    except StopIteration:
                        gens.remove(g)

        units = [(ch, half) for ch in range(NCH) for half in range(2)]
        prev = None
        for ui, (ch, half) in enumerate(units):
            S = SETS[ui % 2]
            drain(seq(*prev) if prev is not None else None, chain(ch, half, S))
            prev = (ch, half, S)
        drain(seq(*prev))
        for ob in range(2):
            w = self.weight(f"L{i}.rw_o{ob}")
            for m in range(4):
                mo = ob * 4 + m
                b = self.bank()
                for kc in range(KC):
                    c.pe.matmul(ps[:, b, 0:TM], lhsT=V(w.ap[:, kc, m * 128:(m + 1) * 128], w.bufs), rhs=yg[:, kc, :], start=(kc == 0), stop=(kc == KC - 1))
                c.dve.scalar_tensor_tensor(out=s_t[:, mo, :], in0=x_t[:, mo, :], scalar=float(ALPHA), in1=ps[:, b, 0:TM], op0=ALU.mult, op1=ALU.add)

    def mamba(self, i):
        TM = self.TM
        xb_t, x_t, s_t = self.xbv, self.xv, self.sv
        c = self.c
        ps = self.ps
        NCH = TM // 128
        TW = TM + 4
        st, stb, hist, A_bc = self.ssm_state[i]
        bcol = self.bc_names[f"ssm{i}"]
        c.areset()
        uT = c.carve("uT", [128, 24, TW], BF16, split=1)
        offA = c.ar_off
        zs = c.carve("zs", [128, NCH, 2048], BF16, split=1)
        xs = c.carve("xs", [128, NCH, 2048], BF16, split=1)
        Btok = c.carve("Btok", [128, NCH, 512], BF16, split=1)
        BT = c.carve("BT", [128, 4, TM], BF16, split=1)
        CT = c.carve("CT", [128, 4, TM], BF16, split=1)
        yT = c.carve("yT", [128, 16, TM], BF16, split=1)
        sm = {k: c.carve("sm_" + k, [128, NCH, 32], F32) for k in ("dt", "dA", "cum", "ncum", "ecum", "cl", "ecl", "wx")}
        self.m_cbm = c.carve("m_cbm", [128, 512], BF16)
        cbrow = c.carve("cbrow", [128, 1024], BF16)
        self.m_xdt = c.carve("m_xdt", [128, 8, 64], BF16)
        self.m_xw = c.carve("m_xw", [128, 8, 64], BF16)
        self.m_yc = c.carve("m_yc", [128, 8, 64], F32)
        self.m_t1 = c.carve("m_t1", [128, 8, 64], BF16)
        self.m_yb = c.carve("m_yb", [128, 8, 64], BF16)
        self.m_ssq = c.carve("m_ssq", [128, 2], F32)
        self.m_rs = c.carve("m_rs", [128, 2], F32)
        w = self.weight(f"L{i}.ssm_dt")
        b = self.bank()
        for ch in range(NCH):
            for kc in range(KC):
                c.pe.matmul(ps[:, b, ch * 32:(ch + 1) * 32], lhsT=xb_t[:, kc, ch * 128:(ch + 1) * 128], rhs=V(w.ap[:, kc, :], w.bufs), start=(kc == 0), stop=(kc == KC - 1))
        dtb = V(self.bc.ap[:, bcol + 64:bcol + 96].unsqueeze(1).to_broadcast([128, NCH, 32]), self.bc.bufs)
        c.dve.tensor_tensor(out=sm["dt"].all(), in0=V(ps.ap[:, b, 0:NCH * 32].rearrange("p (c h) -> p c h", c=NCH), ps.bsplit[b]), in1=dtb, op=ALU.add)
        c.act.activation(out=sm["dt"].all(), in_=sm["dt"].all(), func=AF.Exp)
        c.act.activation(out=sm["dt"].all(), in_=sm["dt"].all(), func=AF.Ln, bias=self.one_c[:, 0:1], scale=1.0)
        c.dve.tensor_tensor(out=sm["dA"].all(), in0=sm["dt"].all(), in1=V(A_bc.ap.unsqueeze(1).to_broadcast([128, NCH, 32]), A_bc.bufs), op=ALU.mult)
        b = self.bank()
        flat = lambda t: V(t.ap.rearrange("p c h -> p (c h)"), t.bufs)
        nsm = NCH * 32
        c.pe.matmul(ps[:, b, 0:nsm], lhsT=self.tri.all(), rhs=flat(sm["dA"]), start=True, stop=True)
        c.pe.matmul(ps[:, b, 128:128 + nsm], lhsT=self.ones_f.all(), rhs=flat(sm["dA"]), start=True, stop=True)
        c.dve.tensor_copy(out=flat(sm["cum"]), in_=ps[:, b, 0:nsm])
        c.dve.tensor_copy(out=flat(sm["cl"]), in_=ps[:, b, 128:128 + nsm])
        c.dve.tensor_scalar(out=sm["ncum"].all(), in0=sm["cum"].all(), scalar1=-1.0, scalar2=None, op0=ALU.mult)
        c.act.activation(out=sm["ecum"].all(), in_=sm["cum"].all(), func=AF.Exp)
        c.act.activation(out=sm["ecl"].all(), in_=sm["cl"].all(), func=AF.Exp)
        c.dve.tensor_tensor(out=sm["wx"].all(), in0=sm["cl"].all(), in1=sm["cum"].all(), op=ALU.subtract)
        c.act.activation(out=sm["wx"].all(), in_=sm["wx"].all(), func=AF.Exp)
        c.dve.tensor_tensor(out=sm["wx"].all(), in0=sm["wx"].all(), in1=sm["dt"].all(), op=ALU.mult)
        for zb in range(4):
            w = self.weight(f"L{i}.ssm_z{zb}")
            for ch in range(NCH):
                b = self.bank()
                for kc in range(KC):
                    c.pe.matmul(ps[:, b, :], lhsT=xb_t[:, kc, ch * 128:(ch + 1) * 128], rhs=V(w.ap[:, kc, :], w.bufs), start=(kc == 0), stop=(kc == KC - 1))
                c.act.activation(out=zs[:, ch, zb * 512:(zb + 1) * 512], in_=ps[:, b, :], func=AF.Silu)
        c.pool.tensor_copy(out=V(uT.ap[:, :, 0:4], uT.bufs), in_=hist.all())
        for xb_ in range(6):
            w = self.weight(f"L{i}.ssm_x{xb_}")
            for m in range(4):
                cc = xb_ * 4 + m
                b = self.bank()
                for kc in range(KC):
                    c.pe.matmul(ps[:, b, 0:TM], lhsT=V(w.ap[:, kc, m * 128:(m + 1) * 128], w.bufs), rhs=xb_t[:, kc, :], start=(kc == 0), stop=(kc == KC - 1))
                if cc % 2:
                    c.act.activation(out=uT[:, cc, 4:4 + TM], in_=ps[:, b, 0:TM], func=AF.Copy)
                else:
                    c.dve.tensor_copy(out=uT[:, cc, 4:4 + TM], in_=ps[:, b, 0:TM])
        c.pool.tensor_copy(out=hist.all(), in_=V(uT.ap[:, :, TM:TM + 4], uT.bufs))
        wcb0 = self.weight(f"L{i}.ssm_cb")
        c.act.activation(out=cbrow[0:65, :], in_=V(wcb0.ap[0:65, 0, :], wcb0.bufs), func=AF.Copy)
        for cc in range(24):
            w = self.weight(f"L{i}.ssm_cw{cc}")
            if cc < 20:
                q = cc % 4
                if q == 0:
                    cvb = [self.bank() for _ in range(NCH)]
                for ch in range(NCH):
                    o = ps[:, cvb[ch], q * 128:(q + 1) * 128]
                    for tap in range(4):
                        c.pe.matmul(o, lhsT=uT[:, cc, 1 + tap + ch * 128:1 + tap + (ch + 1) * 128], rhs=V(w.ap[:, 0, tap * 128:(tap + 1) * 128], w.bufs), start=(tap == 0), stop=False)
                    c.pe.matmul(o, lhsT=self.ones1_b[(cc // 8) * 32:(cc // 8) * 32 + 1, :], rhs=cbrow[(cc // 8) * 32:(cc // 8) * 32 + 1, (cc % 8) * 128:(cc % 8 + 1) * 128], start=False, stop=True)
                if q == 3:
                    for ch in range(NCH):
                        if cc < 16:
                            c.act.activation(out=xs[:, ch, (cc - 3) * 128:(cc + 1) * 128], in_=ps[:, cvb[ch], :], func=AF.Silu)
                        else:
                            c.act.activation(out=Btok[:, ch, :], in_=ps[:, cvb[ch], :], func=AF.Silu)
            if cc >= 16:
                b = self.bank()
                for tap in range(4):
                    c.pe.matmul(ps[:, b, 0:TM], lhsT=V(w.ap[:, 0, tap * 128:(tap + 1) * 128], w.bufs), rhs=uT[:, cc, 1 + tap:1 + tap + TM], start=(tap == 0), stop=(tap == 3))
                dst = BT[:, cc - 16, :] if cc < 20 else CT[:, cc - 20, :]
                c.act.activation(out=dst, in_=ps[:, b, 0:TM], func=AF.Silu, bias=self.pvv(f"ssm_cb{i}", cc), scale=1.0)
        hi = c.ar_off
        c.ar_off = 0
        TS0 = dict(dg=c.carve("dg", [128, 8, 128], F32), seg=c.carve("seg", [128, 8, 128], F32), MT=c.carve("MT", [128, 8, 128], BF16),
                   xdt=self.m_xdt, xw=self.m_xw, yc=self.m_yc, t1=self.m_t1, yb=self.m_yb, ssq=self.m_ssq, rs=self.m_rs)
        assert c.ar_off <= offA
        c.ar_off = hi
        TS1 = dict(dg=c.carve("dg1", [128, 8, 128], F32), seg=c.carve("seg1", [128, 8, 128], F32), MT=c.carve("MT1", [128, 8, 128], BF16),
                   xdt=c.carve("m_xdt1", [128, 8, 64], BF16), xw=c.carve("m_xw1", [128, 8, 64], BF16), yc=c.carve("m_yc1", [128, 8, 64], F32),
                   t1=c.carve("m_t11", [128, 8, 64], BF16), yb=c.carve("m_yb1", [128, 8, 64], BF16), ssq=c.carve("m_ssq1", [128, 2], F32), rs=c.carve("m_rs1", [128, 2], F32))
        for ch in range(NCH):
            cs = slice(ch * 128, (ch + 1) * 128)
            bCB = self.bank()
            for g in range(4):
                c.pe.matmul(ps[:, bCB, g * 128:(g + 1) * 128], lhsT=BT[:, g, cs], rhs=CT[:, g, cs], start=True, stop=True)
            c.dve.tensor_tensor(out=self.m_cbm.all(), in0=ps[:, bCB, :], in1=self.mask4.all(), op=ALU.mult)
            def blk_gen(blk, TS, ch=ch, cs=cs):
                dg, seg, MT = TS['dg'], TS['seg'], TS['MT']
                h0 = blk * 8
                g = blk
                xs3 = V(xs.ap[:, ch, h0 * 64:(h0 + 8) * 64].rearrange("p (h q) -> p h q", q=64), xs.bsplit[ch])
                zs3 = V(zs.ap[:, ch, h0 * 64:(h0 + 8) * 64].rearrange("p (h q) -> p h q", q=64), zs.bsplit[ch])

                def hb(t, n):
                    return V(t.ap[:, ch, h0:h0 + 8].unsqueeze(2).to_broadcast([128, 8, n]), t.bufs)
                xdt, xw, yc, t1, yb = TS['xdt'], TS['xw'], TS['yc'], TS['t1'], TS['yb']
                c.dve.tensor_tensor(out=xdt.all(), in0=xs3, in1=hb(sm["dt"], 64), op=ALU.mult)
                c.pool.tensor_tensor(out=xw.all(), in0=xs3, in1=hb(sm["wx"], 64), op=ALU.mult)
                bI = self.bank()
                c.pe.matmul(ps[:, bI, :], lhsT=CT[:, g, cs], rhs=V(stb.ap[:, h0:h0 + 8, :].rearrange("p h q -> p (h q)"), stb.bufs), start=True, stop=True)
                c.dve.tensor_tensor(out=yc.all(), in0=V(ps.ap[:, bI, :].rearrange("p (h q) -> p h q", q=64), ps.bsplit[bI]), in1=hb(sm["ecum"], 64), op=ALU.mult)
                yield
                c.dve.tensor_tensor(out=dg.all(), in0=V(self.ident_f.ap.unsqueeze(1).to_broadcast([128, 8, 128]), self.ident_f.bufs), in1=hb(sm["cum"], 128), op=ALU.mult)
                for q in range(2):
                    bG = self.bank()
                    c.pe.matmul(ps[:, bG, :], lhsT=self.ones_f.all(), rhs=V(dg.ap[:, q * 4:(q + 1) * 4, :].rearrange("p h t -> p (h t)"), dg.bufs), start=True, stop=True)
                    c.dve.tensor_tensor(out=V(seg.ap[:, q * 4:(q + 1) * 4, :], seg.bufs), in0=V(ps.ap[:, bG, :].rearrange("p (h t) -> p h t", t=128), ps.bsplit[bG]),
                                        in1=V(sm["ncum"].ap[:, ch, h0 + q * 4:h0 + (q + 1) * 4].unsqueeze(2).to_broadcast([128, 4, 128]), sm["ncum"].bufs), op=ALU.add)
                yield
                c.dve.tensor_scalar(out=seg.all(), in0=seg.all(), scalar1=0.0, scalar2=None, op0=ALU.min)
                c.act.activation(out=seg.all(), in_=seg.all(), func=AF.Exp)
                cb4 = V(self.m_cbm.ap[:, g * 128:(g + 1) * 128].unsqueeze(1).to_broadcast([128, 8, 128]), self.m_cbm.bufs)
                c.dve.tensor_tensor(out=MT.all(), in0=seg.all(), in1=cb4, op=ALU.mult)
                yield
                bY = self.bank()
                for hh in range(8):
                    c.pe.matmul(ps[:, bY, hh * 64:(hh + 1) * 64], lhsT=V(MT.ap[:, hh, :], MT.bufs), rhs=V(xdt.ap[:, hh, :], xdt.bufs), start=True, stop=True)
                c.dve.tensor_tensor(out=yc.all(), in0=V(ps.ap[:, bY, :].rearrange("p (h q) -> p h q", q=64), ps.bsplit[bY]), in1=yc.all(), op=ALU.add)
                yield
                dbc = V(self.bc.ap[:, bcol + 32 + h0:bcol + 32 + h0 + 8].unsqueeze(2).to_broadcast([128, 8, 64]), self.bc.bufs)
                c.dve.tensor_tensor(out=t1.all(), in0=xs3, in1=dbc, op=ALU.mult)
                c.dve.tensor_tensor(out=yc.all(), in0=yc.all(), in1=t1.all(), op=ALU.add)
                c.dve.tensor_tensor(out=yc.all(), in0=yc.all(), in1=zs3, op=ALU.mult)
                c.act.activation(out=t1.all(), in_=yc.all(), func=AF.Square, accum_out=TS['ssq'][:, 0:1])
                c.act.activation(out=TS['rs'][:, 0:1], in_=TS['ssq'][:, 0:1], func=AF.Sqrt, bias=self.eps_rms[:, 0:1], scale=float(1.0 / 512.0))
                c.dve.reciprocal(out=TS['rs'][:, 0:1], in_=TS['rs'][:, 0:1])
                c.dve.tensor_scalar(out=yb.all(), in0=yc.all(), scalar1=TS['rs'][:, 0:1], scalar2=None, op0=ALU.mult)
                yield
                ybf = yb.ap.rearrange("p h q -> p (h q)")
                bT = self.bank()
                pb = ps.ap[:, bT, :].bitcast(BF16)
                for r in range(4):
                    c.pe.transpose(V(pb[:, r * 128:(r + 1) * 128], ps.bsplit[bT]), V(ybf[:, r * 128:(r + 1) * 128], yb.bufs), self.ident_b.all())
                for r in range(4):
                    fc = blk * 4 + r
                    if r % 2:
                        c.act.activation(out=yT[:, fc, cs], in_=V(pb[:, r * 128:(r + 1) * 128], ps.bsplit[bT]), func=AF.Copy, scale=self.pvv(f"ssm_nw{i}", fc))
                    else:
                        c.dve.tensor_scalar(out=yT[:, fc, cs], in0=V(pb[:, r * 128:(r + 1) * 128], ps.bsplit[bT]), scalar1=self.pvv(f"ssm_nw{i}", fc), scalar2=None, op0=ALU.mult)
                yield
                bS = self.bank()
                c.pe.matmul(ps[:, bS, :], lhsT=Btok[:, ch, g * 128:(g + 1) * 128], rhs=V(xw.ap.rearrange("p h q -> p (h q)"), xw.bufs), start=True, stop=True)
                sv = V(st.ap[:, h0:h0 + 8, :], st.bufs)
                c.dve.tensor_tensor(out=sv, in0=sv, in1=hb(sm["ecl"], 64), op=ALU.mult)
                c.dve.tensor_tensor(out=sv, in0=V(ps.ap[:, bS, :].rearrange("p (h q) -> p h q", q=64), ps.bsplit[bS]), in1=sv, op=ALU.add)
                c.act.activation(out=V(stb.ap[:, h0:h0 + 8, :], stb.bufs), in_=sv, func=AF.Copy)
            for pair in range(2):
                gens = [blk_gen(2 * pair, TS0), blk_gen(2 * pair + 1, TS1)]
                while gens:
                    for g_ in list(gens):
                        try:
                            next(g_)
                        except StopIteration:
                            gens.remove(g_)
        for ob in range(4):
            w = self.weight(f"L{i}.ssm_out{ob}")
            for m in range(2):
                mo = ob * 2 + m
                b = self.bank()
                for kc in range(16):
                    c.pe.matmul(ps[:, b, 0:TM], lhsT=V(w.ap[:, kc, m * 128:(m + 1) * 128], w.bufs), rhs=yT[:, kc, :], start=(kc == 0), stop=(kc == 15))
                c.dve.scalar_tensor_tensor(out=s_t[:, mo, :], in0=x_t[:, mo, :], scalar=float(ALPHA), in1=ps[:, b, 0:TM], op0=ALU.mult, op1=ALU.add)

    def mlstm(self, i):
        TM = self.TM
        xb_t, x_t, s_t = self.xbv, self.xv, self.sv
        cx = self.c
        c = cx
        c.areset()
        self.qT = c.carve("qT", [128, 4, TM], BF16, split=1)
        self.kT = c.carve("kT", [128, 4, TM], BF16, split=1)
        self.ktok = c.carve("ktok", [128, TM // 128, 512], BF16, split=1)
        self.vtok = c.carve("vtok", [128, TM // 128, 1024], BF16, split=1)
        self.sgo = c.carve("sgo", [128, 8, TM], BF16, split=1)
        self.hn = c.carve("hn", [128, 8, TM], BF16, split=1)
        self.mg = {k: c.carve("mg_" + k, [128, n], F32) for k, n in
                   (("graw", 8 * (TM // 128)), ("gi", 4 * (TM // 128)), ("gf", 4 * (TM // 128)), ("bcum", 4 * (TM // 128)), ("blast", 4 * (TM // 128)), ("a_s", 4 * (TM // 128)), ("ws", 4 * (TM // 128)), ("dec", 4 * (TM // 128)))}
        self.diag = c.carve("diag", [128, 512], F32)
        self.scb = c.carve("scb", [128, 512], F32)
        self.dm = c.carve("dm", [128, 512], F32)
        self.pT = c.carve("pT", [128, 512], BF16)
        self.qs = c.carve("qs", [128, 512], BF16)
        self.rden = c.carve("rden", [128, 512], F32)
        self.hd = c.carve("hd", [128, 2, 512], F32, split=1)
        self.sqh = c.carve("sqh", [128, 2, 512], BF16, split=1)
        self.rstd_m = c.carve("rstd_m", [128, 512], F32)
        self.kw = c.carve("kw", [128, 512], BF16)
        st = self.ml_state[i]
        C, Cb, nbc, nbcb = st
        ps = self.ps
        NCH = TM // 128
        w = self.weight(f"L{i}.ml_g")
        bg = self.bank()
        for ch in range(NCH):
            for kc in range(KC):
                cx.pe.matmul(ps[:, bg, ch * 8:(ch + 1) * 8], lhsT=xb_t[:, kc, ch * 128:(ch + 1) * 128], rhs=V(w.ap[:, kc, :], w.bufs), start=(kc == 0), stop=(kc == KC - 1))
        g = self.mg
        bcol = self.bc_names[f"ml_bg{i}"]
        for ch in range(NCH):
            cx.dve.tensor_tensor(out=g["graw"][:, ch * 8:(ch + 1) * 8], in0=ps[:, bg, ch * 8:(ch + 1) * 8], in1=self.bc[:, bcol:bcol + 8], op=ALU.add)
        cx.act.activation(out=g["graw"].all(), in_=g["graw"].all(), func=AF.Tanh, scale=float(1.0 / 15.0))
        gr = g["graw"].ap.rearrange("p (c e) -> p c e", e=8)
        gi3 = g["gi"].ap.rearrange("p (c e) -> p c e", e=4)
        gf3 = g["gf"].ap.rearrange("p (c e) -> p c e", e=4)
        cx.dve.tensor_scalar(out=g["gi"].v(gi3), in0=g["graw"].v(gr[:, :, 0:4]), scalar1=15.0, scalar2=None, op0=ALU.mult)
        cx.act.activation(out=g["gf"].v(gf3), in_=g["graw"].v(gr[:, :, 4:8]), func=AF.Exp, scale=-15.0)
        cx.act.activation(out=g["gf"].all(), in_=g["gf"].all(), func=AF.Ln, bias=self.one_c[:, 0:1], scale=1.0)
        cx.dve.tensor_scalar(out=g["gf"].all(), in0=g["gf"].all(), scalar1=-1.0, scalar2=None, op0=ALU.mult)
        b1 = self.bank()
        ng = 4 * NCH
        cx.pe.matmul(ps[:, b1, 0:ng], lhsT=self.tri.all(), rhs=g["gf"].all(), start=True, stop=True)
        cx.pe.matmul(ps[:, b1, 32:32 + ng], lhsT=self.ones_f.all(), rhs=g["gf"].all(), start=True, stop=True)
        cx.dve.tensor_copy(out=g["bcum"].all(), in_=ps[:, b1, 0:ng])
        cx.dve.tensor_copy(out=g["blast"].all(), in_=ps[:, b1, 32:32 + ng])
        cx.dve.tensor_tensor(out=g["a_s"].all(), in0=g["gi"].all(), in1=g["bcum"].all(), op=ALU.subtract)
        cx.dve.tensor_tensor(out=g["ws"].all(), in0=g["a_s"].all(), in1=g["blast"].all(), op=ALU.add)
        cx.act.activation(out=g["ws"].all(), in_=g["ws"].all(), func=AF.Exp)
        cx.act.activation(out=g["dec"].all(), in_=g["blast"].all(), func=AF.Exp)
        w = self.weight(f"L{i}.ml_in0")
        for h in range(4):
            b = self.bank()
            for kc in range(KC):
                cx.pe.matmul(ps[:, b, 0:TM], lhsT=V(w.ap[:, kc, h * 128:(h + 1) * 128], w.bufs), rhs=xb_t[:, kc, :], start=(kc == 0), stop=(kc == KC - 1))
            cx.act.activation(out=self.qT[:, h, :], in_=ps[:, b, 0:TM], func=AF.Copy, scale=float(128 ** -0.5))
        w = self.weight(f"L{i}.ml_in1")
        for h in range(4):
            b = self.bank()
            for kc in range(KC):
                cx.pe.matmul(ps[:, b, 0:TM], lhsT=V(w.ap[:, kc, h * 128:(h + 1) * 128], w.bufs), rhs=xb_t[:, kc, :], start=(kc == 0), stop=(kc == KC - 1))
            cx.dve.tensor_copy(out=self.kT[:, h, :], in_=ps[:, b, 0:TM])
        for ch in range(NCH):
            b = self.bank()
            for kc in range(KC):
                cx.pe.matmul(ps[:, b, :], lhsT=xb_t[:, kc, ch * 128:(ch + 1) * 128], rhs=V(w.ap[:, kc, :], w.bufs), start=(kc == 0), stop=(kc == KC - 1))
            cx.act.activation(out=self.ktok[:, ch, :], in_=ps[:, b, :], func=AF.Copy)
        for vb in range(2):
            w = self.weight(f"L{i}.ml_in{2 + vb}")
            for ch in range(NCH):
                b = self.bank()
                for kc in range(KC):
                    cx.pe.matmul(ps[:, b, :], lhsT=xb_t[:, kc, ch * 128:(ch + 1) * 128], rhs=V(w.ap[:, kc, :], w.bufs), start=(kc == 0), stop=(kc == KC - 1))
                if (ch + vb) % 2:
                    cx.act.activation(out=self.vtok[:, ch, vb * 512:(vb + 1) * 512], in_=ps[:, b, :], func=AF.Copy)
                else:
                    cx.dve.tensor_copy(out=self.vtok[:, ch, vb * 512:(vb + 1) * 512], in_=ps[:, b, :])
        for ob in range(2):
            w = self.weight(f"L{i}.ml_in{4 + ob}")
            for m in range(4):
                fc = ob * 4 + m
                b = self.bank()
                for kc in range(KC):
                    cx.pe.matmul(ps[:, b, 0:TM], lhsT=V(w.ap[:, kc, m * 128:(m + 1) * 128], w.bufs), rhs=xb_t[:, kc, :], start=(kc == 0), stop=(kc == KC - 1))
                cx.act.activation(out=self.sgo[:, fc, :], in_=ps[:, b, 0:TM], func=AF.Sigmoid)
                cx.dve.tensor_scalar(out=self.sgo[:, fc, :], in0=self.sgo[:, fc, :], scalar1=self.pvv(f"ml_nw{i}", fc), scalar2=None, op0=ALU.mult)
        for ch in range(NCH):
            cs = slice(ch * 128, (ch + 1) * 128)
            bS, bB = self.bank(), self.bank()
            for h in range(4):
                cx.pe.matmul(ps[:, bS, h * 128:(h + 1) * 128], lhsT=self.kT[:, h, cs], rhs=self.qT[:, h, cs], start=True, stop=True)
            cx.dve.tensor_tensor(out=V(self.diag.ap.rearrange("p (h t) -> p h t", h=4), self.diag.bufs), in0=V(self.ident_f.ap.unsqueeze(1).to_broadcast([128, 4, 128]), self.ident_f.bufs),
                                 in1=V(g["bcum"].ap[:, ch * 4:(ch + 1) * 4].unsqueeze(2).to_broadcast([128, 4, 128]), g["bcum"].bufs), op=ALU.mult)
            cx.pe.matmul(ps[:, bB, :], lhsT=self.ones_f.all(), rhs=self.diag.all(), start=True, stop=True)
            cx.act.activation(out=self.scb.all(), in_=ps[:, bB, :], func=AF.Exp)
            for h in range(4):
                cx.dve.tensor_scalar(out=self.dm[:, h * 128:(h + 1) * 128], in0=ps[:, bB, h * 128:(h + 1) * 128], scalar1=g["a_s"][:, ch * 4 + h:ch * 4 + h + 1], scalar2=15.5,
                                     op0=ALU.add, op1=ALU.min)
            cx.act.activation(out=self.dm.all(), in_=self.dm.all(), func=AF.Exp)
            cx.dve.tensor_tensor(out=self.dm.all(), in0=self.dm.all(), in1=self.mask4.all(), op=ALU.mult)
            cx.dve.tensor_tensor(out=self.pT.all(), in0=ps[:, bS, :], in1=self.dm.all(), op=ALU.mult)
            qv = V(self.qT.ap[:, :, cs], self.qT.bufs)
            cx.dve.tensor_tensor(out=self.qs.v(self.qs.ap.rearrange("p (h t) -> p h t", h=4)), in0=qv, in1=self.scb.v(self.scb.ap.rearrange("p (h t) -> p h t", h=4)), op=ALU.mult)
            bD = self.bank()
            cx.pe.matmul(ps[:, bD, :], lhsT=self.ones1_b.all(), rhs=self.pT.all(), start=True, stop=False)
            for h in range(4):
                cx.pe.matmul(ps[:, bD, h * 128:(h + 1) * 128], lhsT=nbcb[:, h, :], rhs=self.qs[:, h * 128:(h + 1) * 128], start=False, stop=(h == 3))
            cx.dve.tensor_scalar(out=self.rden.all(), in0=ps[:, bD, :], scalar1=-1.0, scalar2=1.0, op0=ALU.mult, op1=ALU.max)
            cx.dve.tensor_tensor(out=self.rden.all(), in0=ps[:, bD, :], in1=self.rden.all(), op=ALU.max)
            cx.dve.reciprocal(out=self.rden.all(), in_=self.rden.all())
            bH = [self.bank(), self.bank()]
            for vc in range(2):
                for h in range(4):
                    cx.pe.matmul(ps[:, bH[vc], h * 128:(h + 1) * 128], lhsT=self.vtok[:, ch, h * 256 + vc * 128:h * 256 + (vc + 1) * 128], rhs=self.pT[:, h * 128:(h + 1) * 128],
                                 start=True, stop=False)
                    cx.pe.matmul(ps[:, bH[vc], h * 128:(h + 1) * 128], lhsT=Cb[:, h, vc * 128:(vc + 1) * 128], rhs=self.qs[:, h * 128:(h + 1) * 128], start=False, stop=True)
            for vc in range(2):
                cx.dve.tensor_tensor(out=self.hd[:, vc, :], in0=ps[:, bH[vc], :], in1=self.rden.all(), op=ALU.mult)
                cx.act.activation(out=self.sqh[:, vc, :], in_=self.hd[:, vc, :], func=AF.Square)
            bQ = self.bank()
            for vc in range(2):
                cx.pe.matmul(ps[:, bQ, :], lhsT=self.ones256_b.all(), rhs=self.sqh[:, vc, :], start=(vc == 0), stop=(vc == 1))
            cx.act.activation(out=self.rstd_m.all(), in_=ps[:, bQ, :], func=AF.Sqrt, bias=self.eps_rms[:, 0:1], scale=1.0)
            cx.dve.reciprocal(out=self.rstd_m.all(), in_=self.rstd_m.all())
            for vc in range(2):
                e = cx.dve
                e.tensor_tensor(out=self.hd[:, vc, :], in0=self.hd[:, vc, :], in1=self.rstd_m.all(), op=ALU.mult)
                hv = V(self.hd.ap[:, vc, :].rearrange("p (h t) -> p h t", h=4), self.hd.bsplit[vc])
                e.tensor_tensor(out=self.hn[:, vc::2, cs], in0=hv, in1=self.sgo[:, vc::2, cs], op=ALU.mult)
            cx.dve.tensor_tensor(out=V(self.kw.ap.rearrange("p (h t) -> p h t", h=4), self.kw.bufs), in0=V(self.ktok.ap[:, ch, :].rearrange("p (h t) -> p h t", h=4), self.ktok.bsplit[ch]),
                                 in1=V(g["ws"].ap[:, ch * 4:(ch + 1) * 4].unsqueeze(2).to_broadcast([128, 4, 128]), g["ws"].bufs), op=ALU.mult)
            bC = [self.bank(), self.bank()]
            bN = self.bank()
            for h in range(4):
                cx.pe.matmul(ps[:, bC[h // 2], (h % 2) * 256:(h % 2 + 1) * 256], lhsT=self.kw[:, h * 128:(h + 1) * 128], rhs=self.vtok[:, ch, h * 256:(h + 1) * 256], start=True, stop=True)
            for h in range(4):
                cx.pe.matmul(ps[:, bN, h * 128:(h + 1) * 128], lhsT=self.kw[:, h * 128:(h + 1) * 128], rhs=self.ones1_b.all(), start=True, stop=True)
            for h in range(4):
                dsc = g["dec"][:, ch * 4 + h:ch * 4 + h + 1]
                cx.dve.scalar_tensor_tensor(out=C[:, h, :], in0=C[:, h, :], scalar=dsc, in1=ps[:, bC[h // 2], (h % 2) * 256:(h % 2 + 1) * 256], op0=ALU.mult, op1=ALU.add)
                cx.dve.scalar_tensor_tensor(out=nbc[:, h, :], in0=nbc[:, h, :], scalar=dsc, in1=ps[:, bN, h * 128:(h + 1) * 128], op0=ALU.mult, op1=ALU.add)
            cx.act.activation(out=Cb.all(), in_=C.all(), func=AF.Copy)
            cx.act.activation(out=nbcb.all(), in_=nbc.all(), func=AF.Copy)
        for ob in range(2):
            w = self.weight(f"L{i}.ml_out{ob}")
            for m in range(4):
                mo = ob * 4 + m
                b = self.bank()
                for kc in range(KC):
                    cx.pe.matmul(ps[:, b, 0:TM], lhsT=V(w.ap[:, kc, m * 128:(m + 1) * 128], w.bufs), rhs=self.hn[:, kc, :], start=(kc == 0), stop=(kc == KC - 1))
                cx.dve.scalar_tensor_tensor(out=s_t[:, mo, :], in0=x_t[:, mo, :], scalar=float(ALPHA), in1=ps[:, b, 0:TM], op0=ALU.mult, op1=ALU.add)

    def pack_pv(self, inputs):
        pv = np.zeros((128, self.npv), np.float32)

        def put(name, vec):
            col = self.pv_names[name]
            n = vec.shape[0] // 128
            pv[:, col:col + n] = vec.reshape(n, 128).T
        for i in self.layers:
            for j in range(2):
                put(f"ln_g{i}.{j}", inputs["ln_g"][i, j])
                put(f"ln_b{i}.{j}", inputs["ln_b"][i, j])
            if i % 3 == 0 and self.mixers:
                put(f"ml_nw{i}", inputs["ml_norm_w"][i // 3])
            if i % 3 == 2 and self.mixers:
                j = i // 3
                put(f"rw{i}_mix", inputs["rw_mix"][j].reshape(-1))
                put(f"rw{i}_w0", inputs["rw_w0"][j])
                put(f"rw{i}_a0", inputs["rw_a0"][j])
                put(f"rw{i}_k_k", inputs["rw_k_k"][j])
                put(f"rw{i}_k_a", inputs["rw_k_a"][j])
                put(f"rw{i}_r_k", inputs["rw_r_k"][j].reshape(-1))
            if i % 3 == 1 and self.mixers:
                put(f"ssm_cb{i}", inputs["ssm_conv_b"][i // 3])
                put(f"ssm_nw{i}", inputs["ssm_norm_w"][i // 3])
        return pv

    def pack_bc(self, inputs):
        bc = np.zeros((128, self.nbc), np.float32)
        for i in self.layers:
            if i % 3 == 0 and self.mixers:
                col = self.bc_names[f"ml_bg{i}"]
                bc[:, col:col + 8] = inputs["ml_b_gate"][i // 3][None, :]
            if i % 3 == 2 and self.mixers:
                col = self.bc_names[f"rw{i}"]
                bc[:, col:col + 1024] = inputs["rw_lnx_w"][i // 3][None, :]
                bc[:, col + 1024:col + 2048] = inputs["rw_lnx_b"][i // 3][None, :]
            if i % 3 == 1 and self.mixers:
                col = self.bc_names[f"ssm{i}"]
                j = i // 3
                bc[:, col:col + 32] = inputs["ssm_a_log"][j][None, :]
                bc[:, col + 32:col + 64] = inputs["ssm_d"][j][None, :]
                bc[:, col + 64:col + 96] = inputs["ssm_dt_bias"][j][None, :]
        return bc


_orig_alloc = Prog.alloc


def _alloc2(self):
    _orig_alloc(self)
    c = self.c
    self.eps_ln = c.sbuf("eps_ln", [128, 1], F32)
    self.eps_rms = c.sbuf("eps_rms", [128, 1], F32)
    self.one_c = c.sbuf("one_c", [128, 1], F32)
    self.bc = c.sbuf("bc", [128, self.nbc], F32)
    self.tri = c.sbuf("tri", [128, 128], F32)
    self.ones_f = c.sbuf("ones_f", [128, 128], F32)
    self.ident_f = c.sbuf("ident_f", [128, 128], F32)
    self.mask4 = c.sbuf("mask4", [128, 512], F32)
    self.ones1_b = c.sbuf("ones1_b", [128, 128], BF16)
    self.ones256_b = c.sbuf("ones256_b", [128, 128], BF16)
    self.ident_b = c.sbuf("ident_b", [128, 128], BF16)
    self.maskX = c.sbuf("maskX", [128, 512], BF16)
    self.lowS = c.sbuf("lowS", [128, 128], BF16)
    self.blockones = c.sbuf("blockones", [128, 128], BF16)
    self.sel2 = c.sbuf("sel2", [128, 2], BF16)
    self.rmask = c.sbuf("rmask", [128, T], F32)
    self.eps_lnx = c.sbuf("eps_lnx", [128, 1], F32)
    self.rw_state = {}
    self.rw_omka = {}
    if self.mixers:
        for i in self.layers:
            if i % 3 == 2:
                self.rw_state[i] = (c.sbuf(f"rwS{i}", [128, 8, 64], F32), c.sbuf(f"rwSb{i}", [128, 8, 64], BF16), c.sbuf(f"rwX{i}", [128, 8, 1], F32))
                self.rw_omka[i] = c.sbuf(f"rwOmka{i}", [128, 8], F32)
    self.ssm_state = {}
    if self.mixers:
        for i in self.layers:
            if i % 3 == 1:
                self.ssm_state[i] = (c.sbuf(f"ssmS{i}", [128, 32, 64], F32), c.sbuf(f"ssmSb{i}", [128, 32, 64], BF16),
                                     c.sbuf(f"ssmH{i}", [128, 24, 4], BF16), c.sbuf(f"ssmA{i}", [128, 32], F32))
    if any(i % 3 == 0 for i in self.layers) and self.mixers:
        self.ml_state = {}
        for i in self.layers:
            if i % 3 == 0:
                self.ml_state[i] = (c.sbuf(f"mlC{i}", [128, 4, 256], F32, split=1), c.sbuf(f"mlCb{i}", [128, 4, 256], BF16),
                                    c.sbuf(f"mln{i}", [128, 4, 128], F32, split=1), c.sbuf(f"mlnb{i}", [128, 4, 128], BF16))


Prog.alloc = _alloc2
_orig_consts = Prog.consts


def _consts2(self):
    _orig_consts(self)
    c = self.c
    c.dve.memset(self.eps_ln.all(), LN_EPS)
    c.dve.memset(self.eps_rms.all(), RMS_EPS)
    c.dve.memset(self.one_c.all(), 1.0)
    c.dve.memset(self.ones_f.all(), 1.0)
    c.dve.memset(self.ones1_b.all(), 1.0)
    c.dve.memset(self.ones256_b.all(), 1.0 / 256.0)
    c.pool.memset(self.tri.all(), 1.0)
    c.pool.affine_select(out=self.tri.all(), in_=self.tri.all(), pattern=[[1, 128]], compare_op=ALU.is_ge, fill=0.0, base=0, channel_multiplier=-1)
    c.pool.memset(self.ident_f.all(), 1.0)
    c.pool.affine_select(out=self.ident_f.all(), in_=self.ident_f.all(), pattern=[[1, 128]], compare_op=ALU.is_equal, fill=0.0, base=0, channel_multiplier=-1)
    for h in range(4):
        c.pool.tensor_copy(out=self.mask4[:, h * 128:(h + 1) * 128], in_=self.tri.all())
    c.pool.tensor_copy(out=self.ident_b.all(), in_=self.ident_f.all())
    c.pool.memset(self.maskX.all(), 1.0)
    for q in range(4):
        c.pool.affine_select(out=self.maskX[:, q * 128:(q + 1) * 128], in_=self.maskX[:, q * 128:(q + 1) * 128], pattern=[[1, 128]],
                             compare_op=(ALU.is_ge if q % 2 else ALU.is_gt), fill=0.0, base=0, channel_multiplier=-1)
    c.pool.memset(self.lowS.all(), 1.0)
    c.pool.affine_select(out=self.lowS.all(), in_=self.lowS.all(), pattern=[[-1, 128]], compare_op=ALU.is_gt, fill=0.0, base=0, channel_multiplier=1)
    c.pool.memset(self.blockones.all(), 0.0)
    c.pool.memset(self.blockones[0:64, 0:64], 1.0)
    c.pool.memset(self.blockones[64:128, 64:128], 1.0)
    c.pool.memset(self.sel2.all(), 0.0)
    c.pool.memset(self.sel2[0:64, 0:1], 1.0)
    c.pool.memset(self.sel2[64:128, 1:2], 1.0)
    c.pool.memset(self.rmask.all(), 1.0)
    for q in range(T // 128):
        c.pool.memset(self.rmask[:, q * 128:q * 128 + 1], 0.0)
    c.pool.memset(self.eps_lnx.all(), 64e-5)
    for i, t_ in self.rw_omka.items():
        col = self.pv_names[f"rw{i}_k_a"]
        c.dve.tensor_scalar(out=t_.all(), in0=self.pv[:, col:col + 8], scalar1=-1.0, scalar2=1.0, op0=ALU.mult, op1=ALU.add)
    for i, st in self.ssm_state.items():
        bcol = self.bc_names[f"ssm{i}"]
        c.act.activation(out=st[3].all(), in_=self.bc[:, bcol:bcol + 32], func=AF.Exp)
        c.dve.tensor_scalar(out=st[3].all(), in0=st[3].all(), scalar1=-1.0, scalar2=None, op0=ALU.mult)


def _reset2(self):
    c = self.c
    if self.mixers:
        for i, st in getattr(self, "ml_state", {}).items():
            for t in st:
                c.pool.memset(t.all(), 0.0)
        for i, st in self.ssm_state.items():
            for t in st[:3]:
                c.pool.memset(t.all(), 0.0)
        for i, st in self.rw_state.items():
            for t in st:
                c.pool.memset(t.all(), 0.0)


Prog.reset_state = _reset2


Prog.consts = _consts2


def run(inputs, nseq_per_core, seqlen, layers, ncores, mixers=True):
    inputs = {k: np.asarray(v) for k, v in inputs.items()}
    prog = Prog(nseq_per_core, seqlen, layers, mixers)
    nc = prog.build()
    wf = prog.wp.pack(inputs)
    pv = prog.pack_pv(inputs)
    bcv = prog.pack_bc(inputs)
    x = inputs["x"]
    in_maps = []
    for cidx in range(ncores):
        xs = x[cidx * nseq_per_core:(cidx + 1) * nseq_per_core]
        in_maps.append({"xT": np.ascontiguousarray(xs.transpose(0, 2, 1)), "wf": wf, "pv": pv, "bc": bcv})
    res = run_bass_kernel_spmd(nc, in_maps, core_ids=list(range(ncores)))
    outs = [np.asarray(r["yT"]).transpose(0, 2, 1) for r in res.results]
    return np.ascontiguousarray(np.concatenate(outs, axis=0)).astype(np.float32)


def kernel(**inputs):
    return run(inputs, 2, 4096, list(range(DEPTH)), 8)
```
